# Optimizing a Trainium2 kernel written in Bass

```python
import jax
import jax.numpy as jnp
from jax import lax
import numpy as np


D_MODEL = 1024
BATCH = 4
SEQ = 4096
DEPTH = 1

ML_HEADS = 4
ML_HEAD_DIM = 128
ML_WIDTH = ML_HEADS * ML_HEAD_DIM
ML_CONV = 4
ML_CHUNK = 64
NSA_HEADS = 8
NSA_KV_GROUPS = 2
NSA_HEAD_DIM = 64
NSA_WIDTH = NSA_HEADS * NSA_HEAD_DIM
NSA_KV_WIDTH = NSA_KV_GROUPS * NSA_HEAD_DIM
NSA_N_BRANCH = 3
CMP_BLOCK = 32
CMP_STRIDE = 16
CMP_HIDDEN = 128
SEL_BLOCK = 64
SEL_COUNT = 16
SEL_FORCE = 1e4
WINDOW = 512
NSA_QBLOCK = 64
N_MIXERS = 2
MEM_TOKENS = 256
XA_HEADS = 4
XA_HEAD_DIM = D_MODEL // XA_HEADS
XA_WIDTH = XA_HEADS * XA_HEAD_DIM
MOE_GROUPS = 4
MOE_EXPERTS_PER_GROUP = 4
MOE_EXPERTS = MOE_GROUPS * MOE_EXPERTS_PER_GROUP
MOE_TOPK = 2
MOE_HIDDEN = 512

RMS_EPS = 1e-6
NEG_INF = -1e30
IN_SPLITS = (ML_WIDTH,) * 4 + (ML_HEADS,) * 2 + (NSA_WIDTH,) + (NSA_KV_WIDTH,) * 6 + (NSA_N_BRANCH * NSA_HEADS, N_MIXERS * D_MODEL)
D_IN = 4 * ML_WIDTH + 2 * ML_HEADS + NSA_WIDTH + 6 * NSA_KV_WIDTH + NSA_N_BRANCH * NSA_HEADS + N_MIXERS * D_MODEL

kernel_name = 'hybrid_mlstm_nsa_hmoe_block'


def rms_norm(x, g):
    xf = x.astype(jnp.float32)
    y = xf * lax.rsqrt(jnp.mean(xf * xf, axis=-1, keepdims=True) + RMS_EPS)
    return (y * g.astype(jnp.float32)).astype(x.dtype)


def alibi_slopes(n_heads):
    return jnp.exp2(-8.0 * jnp.arange(1, n_heads + 1, dtype=jnp.float32) / n_heads)


def causal_depthwise_conv(x, w):
    width, channels = w.shape
    return lax.conv_general_dilated(x, w[:, None, :].astype(x.dtype), window_strides=(1,), padding=[(width - 1, 0)], dimension_numbers=('NWC', 'WIO', 'NWC'), feature_group_count=channels)


def mlstm_chunkwise(q, k, v, i_pre, log_f):
    B, H, S, d = q.shape
    L = ML_CHUNK
    nc = S // L

    def to_chunks(a):
        return jnp.moveaxis(a.reshape(B, H, nc, L, *a.shape[3:]), 2, 0)

    tri = jnp.tril(jnp.ones((L, L), dtype=bool))

    def step(carry, xs):
        C, n, m = carry
        qc, kc, vc, ic, fc = xs
        b = jnp.cumsum(fc, axis=-1)
        dlog = jnp.where(tri, b[..., :, None] - b[..., None, :] + ic[..., None, :], -jnp.inf)
        inter = b + m[..., None]
        mt = jnp.maximum(jnp.max(dlog, axis=-1), inter)
        w_intra = jnp.exp(dlog - mt[..., None])
        w_inter = jnp.exp(inter - mt)
        s = jnp.einsum('bhtd,bhsd->bhts', qc, kc) * w_intra
        num = jnp.einsum('bhts,bhsd->bhtd', s, vc) + w_inter[..., None] * jnp.einsum('bhvk,bhtk->bhtv', C, qc)
        den = jnp.sum(s, axis=-1) + w_inter * jnp.einsum('bhk,bhtk->bht', n, qc)
        h = num / jnp.maximum(jnp.abs(den), jnp.exp(-mt))[..., None]
        bl = b[..., -1]
        logw = bl[..., None] - b + ic
        m_new = jnp.maximum(bl + m, jnp.max(logw, axis=-1))
        decay = jnp.exp(bl + m - m_new)
        ws = jnp.exp(logw - m_new[..., None])
        C_new = decay[..., None, None] * C + jnp.einsum('bhsv,bhsk->bhvk', vc * ws[..., None], kc)
        n_new = decay[..., None] * n + jnp.einsum('bhs,bhsk->bhk', ws, kc)
        return (C_new, n_new, m_new), h

    init = (jnp.zeros((B, H, d, d), jnp.float32), jnp.zeros((B, H, d), jnp.float32), jnp.zeros((B, H), jnp.float32))
    _, h = lax.scan(step, init, (to_chunks(q), to_chunks(k), to_chunks(v), to_chunks(i_pre), to_chunks(log_f)))
    return jnp.moveaxis(h, 0, 2).reshape(B, H, S, d)


def nsa_attention(q, k_cmp, v_cmp, k_slc, v_slc, k_win, v_win, gates, cmp_pos_k, cmp_pos_v, cmp_k_w1, cmp_k_w2, cmp_v_w1, cmp_v_w2):
    B, S, _ = q.shape
    H, G, dh = NSA_HEADS, NSA_KV_GROUPS, NSA_HEAD_DIM
    HG = H // G
    QB = NSA_QBLOCK
    nqb = S // QB
    n_cmp = S // CMP_STRIDE - 1
    n_slc = S // SEL_BLOCK
    n_sel = min(SEL_COUNT, n_slc)
    f32 = jnp.float32
    slopes = alibi_slopes(H).reshape(G, HG)

    qh = (q.reshape(B, S, H, dh) * dh ** -0.5).transpose(0, 2, 1, 3)
    q_blocks = jnp.moveaxis(qh.reshape(B, H, nqb, QB, dh), 2, 0)
    g_blocks = jnp.moveaxis(jax.nn.sigmoid(gates.astype(f32)).reshape(B, nqb, QB, H, NSA_N_BRANCH), 1, 0)

    def kv(a):
        return a.reshape(B, S, G, dh)

    def compress(a, pos, w1, w2):
        ch = kv(a).reshape(B, S // CMP_STRIDE, CMP_STRIDE, G, dh)
        blk = jnp.concatenate([ch[:, :-1], ch[:, 1:]], axis=2) + pos[None, None, :, None, :]
        blk = blk.transpose(0, 3, 1, 2, 4).reshape(B, G, n_cmp, CMP_BLOCK * dh)
        return jax.nn.gelu(blk @ w1) @ w2

    Kc = compress(k_cmp, cmp_pos_k, cmp_k_w1, cmp_k_w2)
    Vc = compress(v_cmp, cmp_pos_v, cmp_v_w1, cmp_v_w2)
    cmp_start = jnp.arange(n_cmp) * CMP_STRIDE
    cmp_end = cmp_start + CMP_BLOCK - 1
    cmp_center = cmp_start.astype(f32) + (CMP_BLOCK - 1) / 2
    cs = cmp_start[:, None]
    ss = jnp.arange(n_slc)[None, :] * SEL_BLOCK
    overlap = jnp.clip(jnp.minimum(cs + CMP_BLOCK, ss + SEL_BLOCK) - jnp.maximum(cs, ss), 0, None).astype(f32) / CMP_BLOCK

    Ks = kv(k_slc).reshape(B, n_slc, SEL_BLOCK, G, dh).transpose(0, 3, 1, 2, 4)
    Vs = kv(v_slc).reshape(B, n_slc, SEL_BLOCK, G, dh).transpose(0, 3, 1, 2, 4)
    Kw = jnp.pad(kv(k_win).transpose(0, 2, 1, 3), ((0, 0), (0, 0), (WINDOW, 0), (0, 0)))
    Vw = jnp.pad(kv(v_win).transpose(0, 2, 1, 3), ((0, 0), (0, 0), (WINDOW, 0), (0, 0)))
    bi = jnp.arange(B)[:, None, None, None]
    gi = jnp.arange(G)[None, :, None, None]
    sel_off = jnp.arange(SEL_BLOCK)
    s_idx = jnp.arange(n_slc)

    def query_block(args):
        i, qb, gb = args
        t = i * QB + jnp.arange(QB)
        tf = t.astype(f32)
        qg = qb.reshape(B, G, HG, QB, dh)
        sc = jnp.einsum('bgxqd,bgjd->bgxqj', qg, Kc).astype(f32)
        valid_c = cmp_end[None, :] <= t[:, None]
        sc = jnp.where(valid_c, sc - slopes[None, :, :, None, None] * (tf[:, None] - cmp_center[None, :]), NEG_INF)
        p_cmp = jax.nn.softmax(sc, axis=-1) * valid_c
        o_cmp = jnp.einsum('bgxqj,bgjd->bgxqd', p_cmp.astype(Vc.dtype), Vc)
        p_slc = jnp.einsum('bgxqj,js->bgqs', p_cmp, overlap)
        cur = t // SEL_BLOCK
        forced = (s_idx[None, :] == 0) | (s_idx[None, :] == cur[:, None]) | (s_idx[None, :] == cur[:, None] - 1)
        future = s_idx[None, :] > cur[:, None]
        score = jnp.where(forced, SEL_FORCE, jnp.where(future, -SEL_FORCE, p_slc))
        _, idx = lax.top_k(score, n_sel)
        Kg = Ks[bi, gi, idx]
        Vg = Vs[bi, gi, idx]
        dist = t[None, None, :, None, None] - (idx[..., None] * SEL_BLOCK + sel_off)
        ssc = jnp.einsum('bgxqd,bgqnsd->bgxqns', qg, Kg).astype(f32)
        ssc = jnp.where((dist >= 0)[:, :, None], ssc - slopes[None, :, :, None, None, None] * dist[:, :, None].astype(f32), NEG_INF)
        p_s = jax.nn.softmax(ssc.reshape(B, G, HG, QB, n_sel * SEL_BLOCK), axis=-1).reshape(ssc.shape)
        o_slc = jnp.einsum('bgxqns,bgqnsd->bgxqd', p_s.astype(Vg.dtype), Vg)
        Kwb = lax.dynamic_slice_in_dim(Kw, i * QB, QB + WINDOW, axis=2)
        Vwb = lax.dynamic_slice_in_dim(Vw, i * QB, QB + WINDOW, axis=2)
        kp = i * QB - WINDOW + jnp.arange(QB + WINDOW)
        dw = t[:, None] - kp[None, :]
        valid_w = (dw >= 0) & (dw < WINDOW) & (kp[None, :] >= 0)
        sw = jnp.einsum('bgxqd,bgkd->bgxqk', qg, Kwb).astype(f32)
        sw = jnp.where(valid_w, sw - slopes[None, :, :, None, None] * dw.astype(f32), NEG_INF)
        o_win = jnp.einsum('bgxqk,bgkd->bgxqd', jax.nn.softmax(sw, axis=-1).astype(Vwb.dtype), Vwb)
        gb = gb.transpose(0, 2, 1, 3).reshape(B, G, HG, QB, NSA_N_BRANCH)
        o = gb[..., 0:1] * o_cmp + gb[..., 1:2] * o_slc + gb[..., 2:3] * o_win
        return o.reshape(B, H, QB, dh).astype(qb.dtype)

    out = lax.map(query_block, (jnp.arange(nqb), q_blocks, g_blocks))
    return out.transpose(1, 0, 3, 2, 4).reshape(B, S, H * dh)


def hybrid_mixer(h, w_in, conv_qk, b_igate, b_fgate, mlstm_norm, cmp_pos_k, cmp_pos_v, cmp_k_w1, cmp_k_w2, cmp_v_w1, cmp_v_w2, w_br_mlstm, w_br_nsa, w_mix_out):
    B, S, _ = h.shape
    f32 = jnp.float32
    offs = [int(o) for o in np.cumsum(IN_SPLITS)[:-1]]
    (ml_q, ml_k, ml_v, ml_o, ml_i, ml_f, ns_q, k_cmp, v_cmp, k_slc, v_slc, k_win, v_win, ns_gates, merge_pre) = jnp.split(h @ w_in, offs, axis=-1)
    qk = jax.nn.silu(causal_depthwise_conv(jnp.concatenate([ml_q, ml_k], axis=-1), conv_qk))
    ml_q, ml_k = jnp.split(qk, 2, axis=-1)

    def heads(a):
        return a.reshape(B, S, ML_HEADS, ML_HEAD_DIM).transpose(0, 2, 1, 3).astype(f32)

    i_pre = (ml_i.astype(f32) + b_igate.astype(f32)).transpose(0, 2, 1)
    log_f = jax.nn.log_sigmoid(ml_f.astype(f32) + b_fgate.astype(f32)).transpose(0, 2, 1)
    h_cell = mlstm_chunkwise(heads(ml_q), heads(ml_k) * ML_HEAD_DIM ** -0.5, heads(ml_v), i_pre, log_f)
    h_cell = rms_norm(h_cell.transpose(0, 2, 1, 3), mlstm_norm.reshape(ML_HEADS, ML_HEAD_DIM)).reshape(B, S, ML_WIDTH)
    y_ml = (jax.nn.sigmoid(ml_o.astype(f32)) * h_cell).astype(h.dtype) @ w_br_mlstm
    y_ns = nsa_attention(ns_q, k_cmp, v_cmp, k_slc, v_slc, k_win, v_win, ns_gates, cmp_pos_k, cmp_pos_v, cmp_k_w1, cmp_k_w2, cmp_v_w1, cmp_v_w2) @ w_br_nsa
    g_ml, g_ns = jnp.split(jax.nn.sigmoid(merge_pre), 2, axis=-1)
    return (g_ml * y_ml + g_ns * y_ns) @ w_mix_out


def memory_cross_attention(h, m, wq, wkv, wo):
    B, S, _ = h.shape
    q = (h @ wq).reshape(B, S, XA_HEADS, XA_HEAD_DIM)
    k, v = jnp.split(m @ wkv, 2, axis=-1)
    k = k.reshape(B, -1, XA_HEADS, XA_HEAD_DIM)
    v = v.reshape(B, -1, XA_HEADS, XA_HEAD_DIM)
    s = jnp.einsum('bshd,bmhd->bhsm', q, k).astype(jnp.float32) * XA_HEAD_DIM ** -0.5
    p = jax.nn.softmax(s, axis=-1).astype(v.dtype)
    o = jnp.einsum('bhsm,bmhd->bshd', p, v).reshape(B, S, XA_WIDTH)
    return o @ wo


def hierarchical_moe(h, wg, bg, we, be, w1, w3, w2):
    B, S, D = h.shape
    f32 = jnp.float32
    hf = h.reshape(B * S, D)
    n_tok = B * S
    p_group = jax.nn.softmax((hf @ wg).astype(f32) + bg.astype(f32), axis=-1)
    g_val, g_idx = lax.top_k(p_group, 1)
    e_logits = ((hf @ we).astype(f32) + be.astype(f32)).reshape(n_tok, MOE_GROUPS, MOE_EXPERTS_PER_GROUP)
    e_in_group = e_logits[jnp.arange(n_tok), g_idx[:, 0]]
    e_val, e_idx = lax.top_k(e_in_group, MOE_TOPK)
    combine = g_val * jax.nn.softmax(e_val, axis=-1)
    expert_id = g_idx * MOE_EXPERTS_PER_GROUP + e_idx
    weights = jnp.sum(jax.nn.one_hot(expert_id, MOE_EXPERTS, dtype=f32) * combine[..., None], axis=1)
    y = jnp.zeros((n_tok, D), f32)
    for e in range(MOE_EXPERTS):
        a = jax.nn.silu(hf @ w1[e]) * (hf @ w3[e])
        y = y + weights[:, e:e + 1] * (a @ w2[e])
    return y.astype(h.dtype).reshape(B, S, D)


def setup_inputs(seed: int = 0) -> dict:
    key = jax.random.key(seed)
    ks = jax.random.split(key, 32)
    f32 = jnp.float32
    L = DEPTH
    dh = NSA_HEAD_DIM

    def nrm(k, shape, scale):
        return scale * jax.random.normal(k, shape, f32)

    def gain(k, shape):
        return 1.0 + 0.02 * jax.random.normal(k, shape, f32)

    return {
        'x': nrm(ks[0], (BATCH, SEQ, D_MODEL), 1.0),
        'mem': nrm(ks[1], (BATCH, MEM_TOKENS, D_MODEL), 1.0),
        'norm_mix': gain(ks[2], (L, D_MODEL)),
        'w_in': nrm(ks[3], (L, D_MODEL, D_IN), D_MODEL ** -0.5),
        'conv_qk': nrm(ks[4], (L, ML_CONV, 2 * ML_WIDTH), ML_CONV ** -0.5),
        'b_igate': nrm(ks[5], (L, ML_HEADS), 0.1),
        'b_fgate': jnp.linspace(3.0, 6.0, ML_HEADS, dtype=f32)[None, :] + nrm(ks[6], (L, ML_HEADS), 0.1),
        'mlstm_norm': gain(ks[7], (L, ML_WIDTH)),
        'cmp_pos_k': nrm(ks[8], (L, CMP_BLOCK, dh), 0.1),
        'cmp_pos_v': nrm(ks[9], (L, CMP_BLOCK, dh), 0.1),
        'cmp_k_w1': nrm(ks[10], (L, CMP_BLOCK * dh, CMP_HIDDEN), (CMP_BLOCK * dh) ** -0.5),
        'cmp_k_w2': nrm(ks[11], (L, CMP_HIDDEN, dh), CMP_HIDDEN ** -0.5),
        'cmp_v_w1': nrm(ks[12], (L, CMP_BLOCK * dh, CMP_HIDDEN), (CMP_BLOCK * dh) ** -0.5),
        'cmp_v_w2': nrm(ks[13], (L, CMP_HIDDEN, dh), CMP_HIDDEN ** -0.5),
        'w_br_mlstm': nrm(ks[14], (L, ML_WIDTH, D_MODEL), ML_WIDTH ** -0.5),
        'w_br_nsa': nrm(ks[15], (L, NSA_WIDTH, D_MODEL), NSA_WIDTH ** -0.5),
        'w_mix_out': nrm(ks[16], (L, D_MODEL, D_MODEL), D_MODEL ** -0.5),
        'norm_xattn': gain(ks[17], (L, D_MODEL)),
        'norm_mem': gain(ks[18], (L, D_MODEL)),
        'xa_wq': nrm(ks[19], (L, D_MODEL, XA_WIDTH), D_MODEL ** -0.5),
        'xa_wkv': nrm(ks[20], (L, D_MODEL, 2 * XA_WIDTH), D_MODEL ** -0.5),
        'xa_wo': nrm(ks[21], (L, XA_WIDTH, D_MODEL), XA_WIDTH ** -0.5),
        'norm_ffn': gain(ks[22], (L, D_MODEL)),
        'router_group_w': nrm(ks[23], (L, D_MODEL, MOE_GROUPS), D_MODEL ** -0.5),
        'router_group_b': nrm(ks[24], (L, MOE_GROUPS), 0.01),
        'router_expert_w': nrm(ks[25], (L, D_MODEL, MOE_EXPERTS), D_MODEL ** -0.5),
        'router_expert_b': nrm(ks[26], (L, MOE_EXPERTS), 0.01),
        'moe_w1': nrm(ks[27], (L, MOE_EXPERTS, D_MODEL, MOE_HIDDEN), D_MODEL ** -0.5),
        'moe_w3': nrm(ks[28], (L, MOE_EXPERTS, D_MODEL, MOE_HIDDEN), D_MODEL ** -0.5),
        'moe_w2': nrm(ks[29], (L, MOE_EXPERTS, MOE_HIDDEN, D_MODEL), MOE_HIDDEN ** -0.5),
        'norm_final': gain(ks[30], (D_MODEL,)),
    }


def reference(x, mem, norm_mix, w_in, conv_qk, b_igate, b_fgate, mlstm_norm, cmp_pos_k, cmp_pos_v, cmp_k_w1, cmp_k_w2, cmp_v_w1, cmp_v_w2, w_br_mlstm, w_br_nsa, w_mix_out, norm_xattn, norm_mem, xa_wq, xa_wkv, xa_wo, norm_ffn, router_group_w, router_group_b, router_expert_w, router_expert_b, moe_w1, moe_w3, moe_w2, norm_final):
    for layer in range(DEPTH):
        x = x + hybrid_mixer(rms_norm(x, norm_mix[layer]), w_in[layer], conv_qk[layer], b_igate[layer], b_fgate[layer], mlstm_norm[layer], cmp_pos_k[layer], cmp_pos_v[layer], cmp_k_w1[layer], cmp_k_w2[layer], cmp_v_w1[layer], cmp_v_w2[layer], w_br_mlstm[layer], w_br_nsa[layer], w_mix_out[layer])
        x = x + memory_cross_attention(rms_norm(x, norm_xattn[layer]), rms_norm(mem, norm_mem[layer]), xa_wq[layer], xa_wkv[layer], xa_wo[layer])
        x = x + hierarchical_moe(rms_norm(x, norm_ffn[layer]), router_group_w[layer], router_group_b[layer], router_expert_w[layer], router_expert_b[layer], moe_w1[layer], moe_w3[layer], moe_w2[layer])
    return rms_norm(x, norm_final)
```

```python
import numpy as np
import ml_dtypes
from contextlib import ExitStack
import concourse.bass as bass
import concourse.mybir as mybir
from concourse.bass_utils import run_bass_kernel_spmd

F32 = mybir.dt.float32
BF16 = mybir.dt.bfloat16
ALU = mybir.AluOpType
AF = mybir.ActivationFunctionType
AX = mybir.AxisListType
BF = ml_dtypes.bfloat16

D = 1024
S = 4096
NT = 32
NJ = 16
KC = 8
NEG = -30000.0
C_MLQ, C_MLK, C_MLV, C_MLO, C_MLI, C_MLF = 0, 512, 1024, 1536, 2048, 2052
C_NSQ, C_KCMP, C_VCMP, C_KSLC, C_VSLC, C_KWIN, C_VWIN, C_NSG, C_MRG = 2056, 2568, 2696, 2824, 2952, 3080, 3208, 3336, 3360
D_IN = 5408


class Buf:
    def __init__(self, t, name=""):
        self.t = t
        self.name = name
        self.w = None
        self.r = []
        self.parts = {}

    def __getitem__(self, idx):
        return self.t[idx]

    def _deps(self, key, is_write):
        ev = []
        if key is None:
            recs = [(self.w, self.r)] + [(p[0], p[1]) for p in self.parts.values()]
        else:
            p = self.parts.setdefault(key, [None, []])
            recs = [(self.w, self.r), (p[0], p[1])]
        for w, r in recs:
            if w is not None:
                ev.append(w)
            if is_write:
                ev.extend(r)
        return ev

    def _note(self, key, is_write, e):
        if key is None:
            if is_write:
                self.w = e
                self.r = []
                self.parts = {}
            else:
                self.r.append(e)
        else:
            p = self.parts.setdefault(key, [None, []])
            if is_write:
                p[0] = e
                p[1] = []
            else:
                p[1].append(e)


class BufView(Buf):
    def __init__(self, parent, ap):
        self.p = parent
        self.t = ap
        self.name = parent.name + "_v"

    def _deps(self, key, is_write):
        return self.p._deps(key, is_write)

    def _note(self, key, is_write, e):
        return self.p._note(key, is_write, e)


class FW:
    def __init__(self, nc, n_dma_sems=16):
        self.nc = nc
        self.es = ExitStack()
        self.engs = {}
        for name, h in (("pe", nc.tensor), ("act", nc.scalar), ("dve", nc.vector),
                        ("pool", nc.gpsimd), ("sp", nc.sync)):
            sem = self.es.enter_context(nc.semaphore("s_" + name))
            self.engs[name] = dict(h=h, sem=sem, cnt=0, seen={}, name=name)
        self.dma_sems = []
        self.dma_pools = {"hw": [], "sw": []}
        for kind, n in (("hw", n_dma_sems), ("sw", 8)):
            for i in range(n):
                sem = self.es.enter_context(nc.semaphore("s_dma_%s%d" % (kind, i)))
                d = dict(sem=sem, val=0)
                self.dma_sems.append(d)
                self.dma_pools[kind].append(d)
        self.dma_rr = {"hw": 0, "sw": 0}
        self.n_wait = 0
        self.n_ins = 0

    def _wait(self, eng, events):
        e = self.engs[eng]
        need = {}
        for (sem, val, src) in events:
            if src == eng and eng == "pe":
                continue
            k = sem.num
            if val > need.get(k, (None, 0))[1]:
                need[k] = (sem, val)
        for k, (sem, val) in need.items():
            if e["seen"].get(k, 0) >= val:
                continue
            e["h"].wait_ge(sem, val)
            e["seen"][k] = val
            self.n_wait += 1

    @staticmethod
    def _norm(lst):
        return [(b, None) if isinstance(b, Buf) else b for b in lst]

    def op(self, eng, fn, reads=(), writes=()):
        e = self.engs[eng]
        rs = self._norm(reads)
        ws = self._norm(writes)
        ev = []
        for b, k in rs:
            ev.extend(b._deps(k, False))
        for b, k in ws:
            ev.extend(b._deps(k, True))
        self._wait(eng, ev)
        ins = fn()
        e["cnt"] += 1
        ins.then_inc(e["sem"], 1)
        me = (e["sem"], e["cnt"], eng)
        for b, k in rs:
            b._note(k, False, me)
        for b, k in ws:
            b._note(k, True, me)
        self.n_ins += 1
        return ins

    def dma(self, eng, out, in_, reads=(), writes=(), **kw):
        e = self.engs[eng]
        rs = self._norm(reads)
        ws = self._norm(writes)
        ev = []
        for b, k in rs:
            ev.extend(b._deps(k, False))
        for b, k in ws:
            ev.extend(b._deps(k, True))
        kind = "sw" if eng == "pool" else "hw"
        pool_ = self.dma_pools[kind]
        d = pool_[self.dma_rr[kind]]
        self.dma_rr[kind] = (self.dma_rr[kind] + 1) % len(pool_)
        if d["val"] > 0:
            ev.append((d["sem"], d["val"], "dma"))
        self._wait(eng, ev)
        ins = e["h"].dma_start(out=out, in_=in_, **kw)
        d["val"] += 16
        ins.then_inc(d["sem"], 16)
        me = (d["sem"], d["val"], "dma")
        for b, k in rs:
            b._note(k, False, me)
        for b, k in ws:
            b._note(k, True, me)
        self.n_ins += 1
        return me

    def wait_all_dma(self, eng):
        for d in self.dma_sems:
            if d["val"]:
                self._wait(eng, [(d["sem"], d["val"], "dma")])

    def close(self):
        self.es.close()


class Rot:
    def __init__(self, items):
        self.items = items
        self.i = 0

    def next(self):
        x = self.items[self.i]
        self.i = (self.i + 1) % len(self.items)
        return x


class Arena:
    def __init__(self, nc, es, nbytes):
        self.t = es.enter_context(nc.sbuf_tensor("arena", [128, nbytes // 4], F32))
        self.free = [[0, nbytes]]
        self.pending = []
        self.peak = 0
        self.nbytes = nbytes

    def alloc(self, name, shape, dt, top=False):
        shape = list(shape)
        esz = 2 if dt == BF16 else 4
        nel = int(np.prod(shape[1:]))
        n = (nel * esz + 31) // 32 * 32
        off = None
        for fr in (reversed(self.free) if top else self.free):
            if fr[1] - fr[0] >= n:
                if top:
                    fr[1] -= n
                    off = fr[1]
                else:
                    off = fr[0]
                    fr[0] += n
                break
        if off is None:
            raise RuntimeError("arena full allocating %s (%d B); free=%s" % (name, n, self.free))
        self.free = [f for f in self.free if f[1] > f[0]]
        self.peak = max(self.peak, off + n)
        base = self.t[:, off // 4:(off + n) // 4]
        ap = base.bitcast(BF16)[:, 0:nel] if dt == BF16 else base[:, 0:nel]
        if len(shape) > 2:
            names = ["a%d" % i for i in range(len(shape) - 1)]
            kw = {nm: sz for nm, sz in zip(names, shape[1:])}
            ap = ap.rearrange("p (%s) -> p %s" % (" ".join(names), " ".join(names)), **kw)
        if shape[0] < 128:
            ap = ap[0:shape[0]]
        buf = Buf(ap, name)
        buf.region = (off, off + n)
        ev = []
        keep = []
        for (s0, e0, evs) in self.pending:
            if s0 < off + n and e0 > off:
                ev.extend(evs)
                if s0 >= off and e0 <= off + n:
                    continue
            keep.append((s0, e0, evs))
        self.pending = keep
        buf.r = ev
        return buf

    def release(self, buf):
        evs = []
        if buf.w is not None:
            evs.append(buf.w)
        evs.extend(buf.r)
        for p in buf.parts.values():
            if p[0] is not None:
                evs.append(p[0])
            evs.extend(p[1])
        best = {}
        for (sem, val, src) in evs:
            if val > best.get(sem.num, (None, 0, None))[1]:
                best[sem.num] = (sem, val, src)
        s0, e0 = buf.region
        self.pending.append((s0, e0, list(best.values())))
        self.free.append([s0, e0])
        self.free.sort()
        merged = []
        for f in self.free:
            if merged and merged[-1][1] == f[0]:
                merged[-1][1] = f[1]
            else:
                merged.append(list(f))
        self.free = merged


class Scope:
    def __init__(self, arena):
        self.arena = arena
        self.bufs = []

    def close(self):
        for b in reversed(self.bufs):
            self.arena.release(b)
        self.bufs = []


def host_consts(p):
    c = {}
    c["par"] = np.full((128, 1), float(p), np.float32)
    c["npar"] = np.full((128, 1), 1.0 - float(p), np.float32)
    s = np.arange(S)
    posk = np.stack([s % 128, np.ones(S), np.ones(S), (s // 128) * 128]).astype(np.float32)
    c["posk"] = posk.astype(BF)
    j = np.arange(256)
    c["poskc"] = np.stack([16.0 * j, np.ones(256), np.ones(256), np.full(256, 15.5)]).astype(BF)
    slopes = np.exp2(-8.0 * np.arange(1, 9) / 8.0)
    posq = np.zeros((2, 4, NJ, 4, 128), np.float32)
    tl = np.arange(128, dtype=np.float32)
    for g in range(2):
        for J in range(NJ):
            t0 = 128.0 * (2 * J + p)
            for hh in range(4):
                sl = slopes[g * 4 + hh]
                posq[g, 0, J, hh] = sl
                posq[g, 1, J, hh] = -sl * tl
                posq[g, 2, J, hh] = -sl * t0
                posq[g, 3, J, hh] = sl
    c["posq"] = posq.reshape(2, 4, NJ * 512).astype(BF)
    c["eall"] = (np.arange(64)[:, None] == (s[None, :] // 64)).astype(np.float32).astype(BF)
    sl_ = np.arange(128)[:, None]
    tl_ = np.arange(128)[None, :]
    wm = np.zeros((128, 6, 128), np.float32)
    for r in range(6):
        dw = (4 + p - r) * 128 + tl_ - sl_
        wm[:, r, :] = np.where((dw >= 0) & (dw < 512), 0.0, NEG)
    c["wmask"] = wm.astype(BF)
    cm = np.zeros((128, NJ, 2, 128), np.float32)
    for J in range(NJ):
        t = 128 * (2 * J + p) + tl_
        for ch in range(2):
            jj = ch * 128 + sl_
            valid = (16 * jj + 31 <= t) & (jj < 255)
            cm[:, J, ch, :] = np.where(valid, 0.0, NEG)
    c["cmask"] = cm.astype(BF)
    cs = (np.arange(256) * 16)[:, None]
    ss = (np.arange(64) * 64)[None, :]
    ov = np.clip(np.minimum(cs + 32, ss + 64) - np.maximum(cs, ss), 0, None).astype(np.float32) / 32.0
    ov[255] = 0.0
    c["ov"] = ov.reshape(2, 128, 64).transpose(1, 0, 2).copy().astype(BF)
    selm = np.zeros((128, NJ, 64), np.float32)
    selc = np.zeros((128, NJ, 64), np.float32)
    blk = np.arange(64)
    for J in range(NJ):
        t = 128 * (2 * J + p) + np.arange(128)
        cur = t // 64
        for i in range(128):
            for bI in range(64):
                if bI == 0 or bI == cur[i] or bI == cur[i] - 1:
                    selc[i, J, bI] = 1e4 + bI
                elif bI > cur[i]:
                    selc[i, J, bI] = -1e4 - bI
                else:
                    selm[i, J, bI] = 1.0
                    selc[i, J, bI] = -1e-30 * bI
    c["selm"] = selm
    c["selc"] = selc
    mm = np.zeros((128, 2, 128), np.float32)
    caus = (sl_ <= tl_).astype(np.float32)
    if p == 0:
        mm[:, 0, :] = caus
    else:
        mm[:, 0, :] = 1.0
        mm[:, 1, :] = caus
    c["mlmask"] = mm.astype(BF)
    return c


def build_nc(debug=None):
    try:
        return _build_nc(debug)
    except _StopBuild as e:
        return e.args[0]


class _StopBuild(Exception):
    pass


def _build_nc(debug=None):
    nc = bass.Bass("TRN2", target_bir_lowering=False)
    V, A, G, T = nc.vector, nc.scalar, nc.gpsimd, nc.tensor
    dram = {}

    def din(name, shape, dt=F32):
        dram[name] = nc.dram_tensor(name, list(shape), dt, kind="ExternalInput").ap()
        return dram[name]

    x_all = din("x_all", [S, D])
    x_own = din("x_own", [NJ * 128, D])
    mem = din("mem", [256, D])
    for nm in ("norm_mix", "norm_xattn", "norm_mem", "norm_ffn", "norm_final"):
        din(nm, [D])
    w_in = din("w_in", [D, D_IN])
    conv_qk = din("conv_qk", [128, 8, 4])
    din("bi_rep", [128]); din("bf_rep", [128])
    din("mlstm_norm", [512])
    din("cmp_pos_kT", [64, 32]); din("cmp_pos_vT", [64, 32])
    din("cmp_k_w1", [2048, 128]); din("cmp_k_w2", [128, 64])
    din("cmp_v_w1", [2048, 128]); din("cmp_v_w2", [128, 64])
    din("w_br_mlstm", [512, D]); din("w_br_nsa", [512, D]); din("w_mix_out", [D, D])
    din("xa_wq", [D, D]); din("xa_wkv", [D, 2 * D]); din("xa_wo", [D, D])
    din("router_w", [D, 20]); din("router_b_rep", [20])
    din("moe_w1", [16, D, 512]); din("moe_w3", [16, D, 512]); din("moe_w2", [16, 512, D])
    din("par", [128, 1]); din("npar", [128, 1])
    din("posk", [4, S], BF16); din("poskc", [4, 256], BF16); din("posq", [2, 4, NJ * 512], BF16)
    din("eall", [64, S], BF16); din("wmask", [128, 6, 128], BF16); din("cmask", [128, NJ, 2, 128], BF16)
    din("ov", [128, 2, 64], BF16); din("selm", [128, NJ, 64]); din("selc", [128, NJ, 64])
    din("mlmask", [128, 2, 128], BF16)
    y_out = nc.dram_tensor("y", [NJ * 128, D], F32, kind="ExternalOutput").ap()
    dbg = {}
    if debug:
        for nm, shp in debug.items():
            dbg[nm] = nc.dram_tensor("dbg_" + nm, list(shp), F32, kind="ExternalOutput").ap()

    fw = FW(nc)
    pes = ExitStack()
    arena = Arena(nc, pes, 206 * 1024)
    gs = Scope(arena)

    def sb(sc, name, shape, dt, top=False):
        b = arena.alloc(name, shape, dt, top=top)
        sc.bufs.append(b)
        return b

    def pst(es, name, shape, dt):
        return Buf(pes.enter_context(nc.psum_tensor("ps_" + name, list(shape), dt)), name)

    def dve(fn, r=(), w=()):
        return fw.op("dve", fn, reads=r, writes=w)

    def act(fn, r=(), w=()):
        return fw.op("act", fn, reads=r, writes=w)

    def pool(fn, r=(), w=()):
        return fw.op("pool", fn, reads=r, writes=w)

    def pe(fn, r=(), w=()):
        return fw.op("pe", fn, reads=r, writes=w)

    pbs = [pst(gs, "pb%d" % i, [128, 512], F32) for i in range(8)]
    ptbs = [BufView(pbs[6], pbs[6][:, :].bitcast(BF16)), BufView(pbs[7], pbs[7][:, :].bitcast(BF16))]
    pb_rot = Rot(pbs[0:6])
    ptb_rot = Rot(ptbs)

    ident_b = sb(gs, "ident_b", [128, 128], BF16)
    ident_f = sb(gs, "ident_f", [128, 128], F32)
    ones_f = sb(gs, "ones_f", [128, 128], F32)
    tri_f = sb(gs, "tri_f", [128, 128], F32)
    cst = sb(gs, "cst", [128, 4], F32)
    par = sb(gs, "par", [128, 1], F32)
    npar = sb(gs, "npar", [128, 1], F32)
    pool(lambda: G.memset(ident_f[:], 0.0), w=[ident_f])
    pool(lambda: G.affine_select(out=ident_f[:], in_=ident_f[:], pattern=[[-1, 128]], compare_op=ALU.not_equal,
                                 fill=1.0, base=0, channel_multiplier=1), r=[ident_f], w=[ident_f])
    pool(lambda: G.tensor_copy(out=ident_b[:], in_=ident_f[:]), r=[ident_f], w=[ident_b])
    pool(lambda: G.memset(ones_f[:], 1.0), w=[ones_f])
    pool(lambda: G.memset(tri_f[:], 1.0), w=[tri_f])
    pool(lambda: G.affine_select(out=tri_f[:], in_=tri_f[:], pattern=[[1, 128]], compare_op=ALU.is_ge,
                                 fill=0.0, base=0, channel_multiplier=-1), r=[tri_f], w=[tri_f])
    pool(lambda: G.memset(cst[:, 0:1], 1e-6), w=[cst])
    pool(lambda: G.memset(cst[:, 1:2], 1.0), w=[cst])
    pool(lambda: G.memset(cst[:, 2:3], 0.0), w=[cst])
    fw.dma("sp", par[:], dram["par"], writes=[par])
    fw.dma("sp", npar[:], dram["npar"], writes=[npar])

    gb = sb(gs, "gb", [128, D], F32)
    nsc = Scope(arena)
    xs_rot = Rot([sb(nsc, "xs%d" % i, [128, D], F32) for i in range(3)])
    xn_rot = Rot([sb(nsc, "xn%d" % i, [128, D], BF16) for i in range(3)])
    sq_junk = sb(gs, "sq_junk", [128, D], BF16)
    st_rot = Rot([sb(gs, "st%d" % i, [128, 4], F32) for i in range(6)])
    wst_rot = Rot([sb(gs, "wst%d" % i, [128, 1024], F32) for i in range(2)])
    cast_rot = Rot(["act"])

    def blend(eng, out_ap, even_ap, odd_ap, tmp_ap, r, w):
        h = {"dve": V, "pool": G}[eng]
        r = list(r)
        tmpbuf = r.pop()
        fw.op(eng, lambda: h.tensor_scalar(out=tmp_ap, in0=even_ap, scalar1=npar[:, 0:1], scalar2=None, op0=ALU.mult),
              reads=r + [npar], writes=[tmpbuf])
        if eng == "dve":
            fw.op(eng, lambda: h.scalar_tensor_tensor(out=out_ap, in0=odd_ap, scalar=par[:, 0:1], in1=tmp_ap,
                                                      op0=ALU.mult, op1=ALU.add), reads=r + [par, tmpbuf], writes=list(w))
        else:
            fw.op(eng, lambda: h.tensor_scalar(out=out_ap, in0=odd_ap, scalar1=par[:, 0:1], scalar2=None, op0=ALU.mult),
                  reads=r + [par], writes=list(w))
            fw.op(eng, lambda: h.tensor_tensor(out=out_ap, in0=out_ap, in1=tmp_ap, op=ALU.add), reads=[tmpbuf] + list(w), writes=list(w))

    def load_gain(name):
        fw.dma("sp", gb[:], dram[name].partition_broadcast(128), writes=[gb])

    def load_w(dst_ap, src_ap, n_free, dst_bufs, eng=None):
        wst = wst_rot.next()
        stv = wst[:, 0:n_free]
        if len(src_ap.shape) == 3:
            stv = stv.rearrange("p (a n) -> p a n", a=src_ap.shape[1])
        fw.dma("sp", stv, src_ap, writes=[wst])
        e = eng or cast_rot.next()
        if e == "act":
            act(lambda: A.copy(out=dst_ap, in_=stv), r=[wst], w=dst_bufs)
        elif e == "pool":
            pool(lambda: G.tensor_copy(out=dst_ap, in_=stv), r=[wst], w=dst_bufs)
        else:
            dve(lambda: V.tensor_copy(out=dst_ap, in_=stv), r=[wst], w=dst_bufs)

    def load_w_cols(dst, col_dst0, src2d, col0, ncols, kc=KC, key=None):
        step = max(1, 1024 // (kc * 1)) if False else None
        per = max(1, 1024 // kc)
        c = 0
        while c < ncols:
            n = min(per, ncols - c)
            src = src2d[:, col0 + c: col0 + c + n].rearrange("(k p) n -> p k n", p=128)
            load_w(dst[:, :, col_dst0 + c: col_dst0 + c + n], src, kc * n, [(dst, key)] if key is not None else [dst])
            c += n

    def load_x(src_tile_ap):
        xs_buf = xs_rot.next()
        fw.dma("pool", xs_buf[:], src_tile_ap, writes=[xs_buf])
        return (xs_buf[:], xs_buf)

    def rmsnorm_front(src_tile_ap):
        if isinstance(src_tile_ap, tuple):
            xs_ap, xs_buf = src_tile_ap
        else:
            xs_ap, xs_buf = load_x(src_tile_ap)
        st = st_rot.next()
        act(lambda: A.activation(out=sq_junk[:], in_=xs_ap, func=AF.Square, accum_out=st[:, 0:1]), r=[xs_buf], w=[sq_junk, st])
        act(lambda: A.activation(out=st[:, 1:2], in_=st[:, 0:1], func=AF.Ln, scale=1.0 / D, bias=cst[:, 0:1]), r=[st, cst], w=[st])
        act(lambda: A.activation(out=st[:, 2:3], in_=st[:, 1:2], func=AF.Exp, scale=-0.5), r=[st], w=[st])
        xn = xn_rot.next()
        dve(lambda: V.scalar_tensor_tensor(out=xn[:], in0=xs_ap, scalar=st[:, 2:3], in1=gb[:], op0=ALU.mult, op1=ALU.mult),
            r=[xs_buf, st, gb], w=[xn])
        return xn

    def rmsnorm_back(xn, dstT, col0, key=None, evac_eng="act"):
        ptb = ptb_rot.next()
        for k in range(KC):
            pe(lambda: T.transpose(out=ptb[:, k * 128:(k + 1) * 128], in_=xn[:, k * 128:(k + 1) * 128], identity=ident_b[:]),
               r=[xn, ident_b], w=[ptb])
        dst_ap = dstT[:, :, col0:col0 + 128]
        src_ap = ptb[:].rearrange("p (k t) -> p k t", k=KC)
        wl = [(dstT, key)] if key is not None else [dstT]
        if evac_eng == "act":
            act(lambda: A.copy(out=dst_ap, in_=src_ap), r=[ptb], w=wl)
        else:
            dve(lambda: V.tensor_copy(out=dst_ap, in_=src_ap), r=[ptb], w=wl)

    def rmsnorm_T(src_tile_ap, dstT, col0, key=None, evac_eng="act"):
        xn = rmsnorm_front(src_tile_ap)
        rmsnorm_back(xn, dstT, col0, key=key, evac_eng=evac_eng)

    def norm_loop(srcs, dstT, keyfn, la=2):
        q_ = []
        n = len(srcs)
        for t in range(n + la):
            if t < n:
                q_.append(rmsnorm_front(srcs[t]))
            if t >= la:
                tt = t - la
                rmsnorm_back(q_[tt], dstT, tt * 128, key=keyfn(tt), evac_eng="act" if tt % 2 else "dve")

    def final_wait():
        fw.wait_all_dma("sp")


    def maybe_stop(tag):
        if tag in dbg:
            fw.dma("sp", dbg[tag], cst[:], reads=[cst])
            final_wait()
            pes.close(); fw.close()
            raise _StopBuild(nc)

    def dump(name, src_ap, bufs, dst_ap=None):
        if name in dbg:
            fw.dma("sp", dst_ap if dst_ap is not None else dbg[name], src_ap, reads=bufs)

    ps1 = Scope(arena)
    uT_own = sb(ps1, "uT_own", [128, 4, NJ * 128], BF16, top=True)

    pa = Scope(arena)
    hT_all = sb(pa, "hT_all", [128, KC, S], BF16)
    load_gain("norm_mix")
    xn_q = []
    for t in range(NT + 2):
        if t < NT:
            xn_q.append(rmsnorm_front(x_all[t * 128:(t + 1) * 128, :]))
        if t >= 2:
            tt = t - 2
            rmsnorm_back(xn_q[tt], hT_all, tt * 128, key=tt // 4, evac_eng="act" if tt % 2 else "dve")

    nsc.close()
    pm = Scope(arena)
    wg = sb(pm, "wg", [128, KC, 8], BF16)
    load_w_cols(wg, 0, w_in, C_MLI, 8)
    gp = pb_rot.next()
    for t in range(NT):
        for k in range(KC):
            pe(lambda: T.matmul(gp[:, t * 8:(t + 1) * 8], lhsT=hT_all[:, k, t * 128:(t + 1) * 128], rhs=wg[:, k, :],
                                start=(k == 0), stop=(k == KC - 1)), r=[(hT_all, t // 4), wg], w=[gp])
    NG = 14
    pgt = Scope(arena)
    gt = [sb(pgt, "gt%d" % i, [128, 128], F32) for i in range(NG)]
    bi_b, bf_b = gt[0], gt[1]
    fw.dma("sp", bi_b[:], dram["bi_rep"].partition_broadcast(128), writes=[bi_b])
    fw.dma("sp", bf_b[:], dram["bf_rep"].partition_broadcast(128), writes=[bf_b])
    gpv = gp[:, 0:256].rearrange("p (t c) -> p t c", c=8)
    ipre, fz, t_a, t_b = gt[2], gt[3], gt[4], gt[5]
    v3 = lambda b_: b_[:].rearrange("p (t h) -> p t h", h=4)
    dve(lambda: V.tensor_tensor(out=v3(ipre), in0=gpv[:, :, 0:4], in1=v3(bi_b), op=ALU.add), r=[gp, bi_b], w=[ipre])
    dve(lambda: V.tensor_tensor(out=v3(fz), in0=gpv[:, :, 4:8], in1=v3(bf_b), op=ALU.add), r=[gp, bf_b], w=[fz])
    dve(lambda: V.tensor_scalar(out=t_a[:], in0=fz[:], scalar1=-1.0, scalar2=None, op0=ALU.mult), r=[fz], w=[t_a])
    dve(lambda: V.tensor_tensor(out=t_a[:], in0=t_a[:], in1=fz[:], op=ALU.max), r=[fz, t_a], w=[t_a])
    act(lambda: A.activation(out=t_a[:], in_=t_a[:], func=AF.Exp, scale=-1.0), r=[t_a], w=[t_a])
    act(lambda: A.activation(out=t_a[:], in_=t_a[:], func=AF.Ln, bias=cst[:, 1:2]), r=[t_a, cst], w=[t_a])
    dve(lambda: V.tensor_scalar_min(out=t_b[:], in0=fz[:], scalar1=0.0), r=[fz], w=[t_b])
    logf = gt[6]
    dve(lambda: V.tensor_sub(out=logf[:], in0=t_b[:], in1=t_a[:]), r=[t_a, t_b], w=[logf])
    pc = pb_rot.next()
    pe(lambda: T.matmul(pc[:, 0:128], lhsT=tri_f[:], rhs=logf[:], start=True, stop=True), r=[tri_f, logf], w=[pc])
    pe(lambda: T.matmul(pc[:, 128:256], lhsT=ones_f[:], rhs=logf[:], start=True, stop=True), r=[ones_f, logf], w=[pc])
    blocal, tot = gt[7], gt[8]
    dve(lambda: V.tensor_copy(out=blocal[:], in_=pc[:, 0:128]), r=[pc], w=[blocal])
    dve(lambda: V.tensor_copy(out=tot[:], in_=pc[:, 128:256]), r=[pc], w=[tot])

    def scan(src, op, tmp1, tmp2):
        cur = src
        bufs = [tmp1, tmp2]
        i = 0
        for d in (1, 2, 4, 8, 16):
            nxt = bufs[i % 2]
            i += 1
            dve(lambda: V.tensor_copy(out=nxt[:, 0:4 * d], in_=cur[:, 0:4 * d]), r=[cur], w=[nxt])
            dve(lambda: V.tensor_tensor(out=nxt[:, 4 * d:128], in0=cur[:, 4 * d:128], in1=cur[:, 0:128 - 4 * d], op=op), r=[cur], w=[nxt])
            cur = nxt
        return cur

    incl = scan(tot, ALU.add, gt[9], gt[10])
    Bg = gt[11]
    dve(lambda: V.tensor_sub(out=Bg[:], in0=incl[:], in1=tot[:]), r=[incl, tot], w=[Bg])
    dve(lambda: V.tensor_add(out=Bg[:], in0=Bg[:], in1=blocal[:]), r=[Bg, blocal], w=[Bg])
    a_t = gt[12]
    dve(lambda: V.tensor_sub(out=a_t[:], in0=ipre[:], in1=Bg[:]), r=[ipre, Bg], w=[a_t])
    pe(lambda: T.transpose(out=pc[:, 256:384], in_=a_t[:], identity=ident_f[:]), r=[a_t, ident_f], w=[pc])
    amax = gt[13]
    dve(lambda: V.tensor_reduce(out=amax[:, 0:1], in_=pc[:, 256:384], axis=AX.X, op=ALU.max), r=[pc], w=[amax])
    dgm = gt[9]
    dve(lambda: V.tensor_scalar(out=dgm[:], in0=ident_f[:], scalar1=amax[:, 0:1], scalar2=None, op0=ALU.mult), r=[ident_f, amax, incl], w=[dgm])
    pe(lambda: T.matmul(pc[:, 384:512], lhsT=ones_f[:], rhs=dgm[:], start=True, stop=True), r=[ones_f, dgm], w=[pc])
    amb = gt[10]
    dve(lambda: V.tensor_scalar_max(out=amb[:], in0=pc[:, 384:512], scalar1=0.0), r=[pc], w=[amb])
    ainc = scan(amb, ALU.max, gt[3], gt[4])
    ref = gt[5]
    v4 = lambda b_: b_[:].rearrange("p (j r h) -> p j r h", r=2, h=4)
    dve(lambda: V.memset(ref[:], 0.0), w=[ref])
    for r_ in range(2):
        dve(lambda: V.tensor_copy(out=v4(ref)[:, 1:16, r_, :], in_=v4(ainc)[:, 0:15, 1, :]), r=[ainc], w=[ref])
    decay = sb(pm, "decay", [128, NJ, 4], F32)
    dve(lambda: V.tensor_sub(out=decay[:], in0=v4(ref)[:, :, 0, :], in1=v4(ainc)[:, :, 1, :]), r=[ref, ainc], w=[decay])
    act(lambda: A.activation(out=decay[:], in_=decay[:], func=AF.Exp), r=[decay], w=[decay])
    ws = sb(pm, "ws", [128, NT, 4], F32)
    thr = sb(pm, "thr", [128, NT, 4], F32)
    wsf = ws[:].rearrange("p t h -> p (t h)")
    thf = thr[:].rearrange("p t h -> p (t h)")
    dve(lambda: V.tensor_sub(out=wsf, in0=a_t[:], in1=ref[:]), r=[a_t, ref], w=[ws])
    act(lambda: A.activation(out=wsf, in_=wsf, func=AF.Exp), r=[ws], w=[ws])
    dve(lambda: V.tensor_scalar(out=wsf, in0=wsf, scalar1=128.0 ** -0.5, scalar2=None, op0=ALU.mult), r=[ws], w=[ws])
    dve(lambda: V.tensor_add(out=thf, in0=Bg[:], in1=ref[:]), r=[Bg, ref], w=[thr])
    act(lambda: A.activation(out=thf, in_=thf, func=AF.Exp, scale=-1.0), r=[thr], w=[thr])
    thr_own = sb(pm, "thr_own", [128, NJ, 4], F32)
    thr4 = thr[:].rearrange("p (j r) h -> p j r h", r=2)
    blend("dve", thr_own[:], thr4[:, :, 0, :], thr4[:, :, 1, :], gt[6][:, 0:64].rearrange("p (j h) -> p j h", h=4), [thr, gt[6]], [thr_own])

    pgt.close()
    og = sb(pm, "og", [128, NJ, 512], BF16)
    pog = Scope(arena)
    wo_b = sb(pog, "wo_b", [128, KC, 512], BF16)
    load_w_cols(wo_b, 0, w_in, C_MLO, 512)
    gain_ml = sb(pog, "gain_ml", [128, 512], F32)
    fw.dma("sp", gain_ml[:], dram["mlstm_norm"].partition_broadcast(128), writes=[gain_ml])
    hown_rot = Rot([sb(pog, "hown%d" % i, [128, KC, 128], BF16) for i in range(2)])
    htmp = sb(pog, "htmp", [128, KC, 128], BF16)
    sig_t = sb(pog, "sig_t", [128, 512], F32)
    def og_blend(J):
        ho = hown_rot.next()
        blend("dve", ho[:], hT_all[:, :, (2 * J) * 128:(2 * J + 1) * 128], hT_all[:, :, (2 * J + 1) * 128:(2 * J + 2) * 128], htmp[:],
              [(hT_all, (2 * J) // 4), htmp], [ho])
        return ho
    ho_next = og_blend(0)
    for J in range(NJ):
        ho = ho_next
        po = pb_rot.next()
        for k in range(KC):
            pe(lambda: T.matmul(po[:, :], lhsT=ho[:, k, :], rhs=wo_b[:, k, :], start=(k == 0), stop=(k == KC - 1)), r=[ho, wo_b], w=[po])
        if J + 1 < NJ:
            ho_next = og_blend(J + 1)
        act(lambda: A.activation(out=sig_t[:], in_=po[:], func=AF.Sigmoid), r=[po], w=[sig_t])
        dve(lambda: V.tensor_mul(out=og[:, J, :], in0=sig_t[:], in1=gain_ml[:]), r=[sig_t, gain_ml], w=[(og, J)])
    pog.close()
    convw = sb(pm, "convw", [128, KC, 4], F32)
    fw.dma("sp", convw[:], conv_qk, writes=[convw])
    mlmask = sb(pm, "mlmask", [128, 2, 128], BF16)
    fw.dma("sp", mlmask[:], dram["mlmask"], writes=[mlmask])
    wq_rot = Rot([sb(pm, "wq%d" % i, [128, KC, 128], BF16) for i in range(2)])
    wk_rot = Rot([sb(pm, "wk%d" % i, [128, KC, 128], BF16) for i in range(2)])
    wv_rot = Rot([sb(pm, "wv%d" % i, [128, KC, 128], BF16) for i in range(2)])
    pre_q = sb(pm, "pre_q", [128, 3 + 512], F32)
    pre_k = sb(pm, "pre_k", [128, 3 + 512], F32)
    acc_rot = Rot([sb(pm, "acc%d" % i, [128, 512], F32) for i in range(2)])
    qsil = sb(pm, "qsil", [128, 512], BF16)
    qtmp = sb(pm, "qtmp", [128, 2, 128], BF16)
    hsets = [dict(QT_own=sb(pm, "QT_own%d" % i, [128, NJ, 128], BF16), KT=sb(pm, "KT%d" % i, [128, S], BF16),
                  Ktok=sb(pm, "Ktok%d" % i, [128, NT, 128], BF16), Vp=sb(pm, "Vp%d" % i, [128, NT, 129], BF16)) for i in range(2)]
    Cst = sb(pm, "Cst", [128, 129], F32)
    Cbf_rot = Rot([sb(pm, "Cbf%d" % i, [128, 129], BF16) for i in range(2)])
    stm_rot = Rot([sb(pm, "stm%d" % i, [128, 2, 128], BF16) for i in range(2)])
    hc_t = sb(pm, "hc_t", [128, 128], F32)
    sm_rot = Rot([sb(pm, "sm%d" % i, [128, 8], F32) for i in range(2)])
    u_rot = Rot([sb(pm, "u%d" % i, [128, 128], BF16) for i in range(2)])
    hc_junk = sb(pm, "hc_junk", [128, 128], BF16)

    def head_items(h):
        hs = hsets[h % 2]
        wq, wk, wv = wq_rot.next(), wk_rot.next(), wv_rot.next()
        items = []

        def ld():
            load_w_cols(wq, 0, w_in, C_MLQ + h * 128, 128)
            load_w_cols(wk, 0, w_in, C_MLK + h * 128, 128)
            load_w_cols(wv, 0, w_in, C_MLV + h * 128, 128)

        def qk_chunk(which, c):
            wsel = wq if which == "q" else wk
            cch = h if which == "q" else 4 + h
            pre = pre_q if which == "q" else pre_k
            st_ = {}

            def A_():
                ps = pb_rot.next()
                for k in range(KC):
                    pe(lambda: T.matmul(ps[:, :], lhsT=wsel[:, k, :], rhs=hT_all[:, k, c * 512:(c + 1) * 512], start=(k == 0), stop=(k == KC - 1)),
                       r=[wsel, (hT_all, c)], w=[ps])
                if c == 0:
                    dve(lambda: V.memset(pre[:, 0:3], 0.0), w=[pre])
                else:
                    dve(lambda: V.tensor_copy(out=pre[:, 0:3], in_=pre[:, 512:515]), r=[pre], w=[pre])
                act(lambda: A.copy(out=pre[:, 3:515], in_=ps[:, :]), r=[ps], w=[pre])
                acc = acc_rot.next()
                st_["acc"] = acc
                act(lambda: A.activation(out=acc[:], in_=ps[:, :], func=AF.Copy, scale=convw[:, cch, 3:4]), r=[ps, convw], w=[acc])

            def B_():
                acc = st_["acc"]
                for j in range(3):
                    dve(lambda: V.scalar_tensor_tensor(out=acc[:], in0=pre[:, j:j + 512], scalar=convw[:, cch, j:j + 1], in1=acc[:],
                                                       op0=ALU.mult, op1=ALU.add), r=[pre, convw, acc], w=[acc])
                if which == "q":
                    qs = qsil_rot.next()
                    st_["qs"] = qs
                    act(lambda: A.activation(out=qs[:], in_=acc[:], func=AF.Silu), r=[acc], w=[qs])
                else:
                    act(lambda: A.activation(out=hs["KT"][:, c * 512:(c + 1) * 512], in_=acc[:], func=AF.Silu), r=[acc], w=[(hs["KT"], c)])

            def C_():
                qs = st_["qs"]
                q4 = qs[:].rearrange("p (j r t) -> p j r t", r=2, t=128)
                blend("dve", hs["QT_own"][:, 2 * c:2 * c + 2, :], q4[:, :, 0, :], q4[:, :, 1, :], qtmp[:], [qs, qtmp], [(hs["QT_own"], c)])
            return [A_, B_, C_] if which == "q" else [A_, B_]

        def ktok_group(c):
            st_ = {}

            def A_():
                ptb = ptb_rot.next()
                st_["ptb"] = ptb
                for i in range(8):
                    t = c * 8 + i
                    pe(lambda: T.transpose(out=ptb[:, i * 128:(i + 1) * 128], in_=hs["KT"][:, t * 128:(t + 1) * 128], identity=ident_b[:]),
                       r=[(hs["KT"], t // 4), ident_b], w=[ptb])

            def B_():
                ptb = st_["ptb"]
                dve(lambda: V.tensor_copy(out=hs["Ktok"][:, c * 8:(c + 1) * 8, :], in_=ptb[:].rearrange("p (i d) -> p i d", i=8)), r=[ptb], w=[(hs["Ktok"], c)])
            return [A_, B_]

        def v_group(c):
            st_ = {}

            def A_():
                ps = pb_rot.next()
                st_["ps"] = ps
                for i in range(4):
                    t = c * 4 + i
                    for k in range(KC):
                        pe(lambda: T.matmul(ps[:, i * 128:(i + 1) * 128], lhsT=hT_all[:, k, t * 128:(t + 1) * 128], rhs=wv[:, k, :],
                                            start=(k == 0), stop=(k == KC - 1)), r=[(hT_all, c), wv], w=[ps])

            def B_():
                ps = st_["ps"]
                wsv = ws[:, c * 4:(c + 1) * 4, h:h + 1]
                dve(lambda: V.tensor_tensor(out=hs["Vp"][:, c * 4:(c + 1) * 4, 0:128], in0=ps[:].rearrange("p (i d) -> p i d", i=4),
                                            in1=wsv.to_broadcast([128, 4, 128]), op=ALU.mult), r=[ps, ws], w=[(hs["Vp"], c)])
                dve(lambda: V.tensor_copy(out=hs["Vp"][:, c * 4:(c + 1) * 4, 128:129], in_=ws[:, c * 4:(c + 1) * 4, h:h + 1]), r=[ws], w=[(hs["Vp"], c)])
            return [A_, B_]

        items.append([ld])
        late = None
        for c in range(8):
            items.append(qk_chunk("q", c))
            items.append(qk_chunk("k", c))
            items.append(v_group(c))
            if late is not None and c % 2 == 0:
                items.append(ktok_group(late))
                late = None
            if c % 2 == 1:
                late = c // 2
        items.append([lambda: None])
        items.append(ktok_group(late))
        return items

    class ItemSched:
        def __init__(self):
            self.queue = []
            self.inflight = []

        def add(self, items):
            self.queue += items

        def step(self, n_new):
            nxt = []
            for it in self.inflight:
                it.pop(0)()
                if it:
                    nxt.append(it)
            self.inflight = nxt
            for _ in range(n_new):
                if self.queue:
                    it = list(self.queue.pop(0))
                    it.pop(0)()
                    if it:
                        self.inflight.append(it)

        def drain(self):
            while self.queue or self.inflight:
                self.step(2)

    qsil_rot = Rot([qsil, sb(pm, "qsil2", [128, 512], BF16)])
    sched = ItemSched()
    u_pending = []
    u_pending_dve = []
    po_rot = Rot([pbs[2], pbs[3]])
    pb_rot = Rot(pbs[4:6])
    sched.add(head_items(0))
    for h in range(4):
        sched.drain()
        if h + 1 < 4:
            sched.add(head_items(h + 1))
        hs = hsets[h % 2]
        QT_own, KT, Ktok, Vp = hs["QT_own"], hs["KT"], hs["Ktok"], hs["Vp"]
        pool(lambda: G.memset(Cst[:], 0.0), w=[Cst])
        Cbf = Cbf_rot.next()
        pool(lambda: G.memset(Cbf[:], 0.0), w=[Cbf])
        for J in range(NJ):
            pst_ = pbs[0]
            for r_ in range(2):
                t = 2 * J + r_
                pe(lambda: T.matmul(pst_[:, r_ * 128:(r_ + 1) * 128], lhsT=KT[:, t * 128:(t + 1) * 128], rhs=QT_own[:, J, :], start=True, stop=True),
                   r=[(KT, t // 4), (QT_own, J // 2)], w=[pst_])
            stm = stm_rot.next()
            dve(lambda: V.tensor_tensor(out=stm[:], in0=pst_[:, 0:256].rearrange("p (r t) -> p r t", r=2), in1=mlmask[:], op=ALU.mult),
                r=[pst_, mlmask], w=[stm])
            Cbf_next = None
            if J < NJ - 1:
                pcs = pbs[1]
                for r_ in range(2):
                    t = 2 * J + r_
                    pe(lambda: T.matmul(pcs[:, 0:129], lhsT=Ktok[:, t, :], rhs=Vp[:, t, :], start=(r_ == 0), stop=(r_ == 1)),
                       r=[(Ktok, t // 8), (Vp, t // 4)], w=[pcs])
                jm = max(J - 1, 0)
                dve(lambda: V.scalar_tensor_tensor(out=Cst[:], in0=Cst[:], scalar=decay[:, jm, h:h + 1], in1=pcs[:, 0:129], op0=ALU.mult, op1=ALU.add),
                    r=[pcs, decay, Cst], w=[Cst])
                Cbf_next = Cbf_rot.next()
                act(lambda: A.activation(out=Cbf_next[:], in_=Cst[:], func=AF.Copy, scale=decay[:, J, h:h + 1]), r=[Cst, decay], w=[Cbf_next])
            sched.step(2)
            po = po_rot.next()
            pe(lambda: T.matmul(po[:, 0:129], lhsT=QT_own[:, J, :], rhs=Cbf[:], start=True, stop=False), r=[(QT_own, J // 2), Cbf], w=[po])
            for r_ in range(2):
                t = 2 * J + r_
                pe(lambda: T.matmul(po[:, 0:129], lhsT=stm[:, r_, :], rhs=Vp[:, t, :], start=False, stop=(r_ == 1)), r=[stm, (Vp, t // 4)], w=[po])
            for f_ in u_pending_dve:
                f_()
            u_pending_dve[:] = []
            while u_pending:
                u_pending.pop(0)()
            sm = sm_rot.next()
            dve(lambda: V.tensor_scalar(out=sm[:, 0:1], in0=po[:, 128:129], scalar1=-1.0, scalar2=thr_own[:, J, h:h + 1], op0=ALU.mult, op1=ALU.max), r=[po, thr_own], w=[sm])
            dve(lambda: V.tensor_tensor(out=sm[:, 1:2], in0=sm[:, 0:1], in1=po[:, 128:129], op=ALU.max), r=[sm, po], w=[sm])
            dve(lambda: V.reciprocal(out=sm[:, 2:3], in_=sm[:, 1:2]), r=[sm], w=[sm])
            dve(lambda: V.tensor_scalar(out=hc_t[:], in0=po[:, 0:128], scalar1=sm[:, 2:3], scalar2=None, op0=ALU.mult), r=[po, sm], w=[hc_t])
            if "h_cell" in dbg:
                dump("h_cell", hc_t[:], [hc_t], dbg["h_cell"][h, J * 128:(J + 1) * 128, :])
            act(lambda: A.activation(out=hc_junk[:], in_=hc_t[:], func=AF.Square, accum_out=sm[:, 3:4]), r=[hc_t], w=[hc_junk, sm])
            act(lambda: A.activation(out=sm[:, 4:5], in_=sm[:, 3:4], func=AF.Ln, scale=1.0 / 128, bias=cst[:, 0:1]), r=[sm, cst], w=[sm])
            act(lambda: A.activation(out=sm[:, 5:6], in_=sm[:, 4:5], func=AF.Exp, scale=-0.5), r=[sm], w=[sm])
            u = u_rot.next()

            def u_dve(u=u, sm=sm, h=h, J=J):
                dve(lambda: V.scalar_tensor_tensor(out=u[:], in0=hc_t[:], scalar=sm[:, 5:6], in1=og[:, J, h * 128:(h + 1) * 128], op0=ALU.mult, op1=ALU.mult),
                    r=[hc_t, sm, (og, J)], w=[u])
            u_pending_dve.append(u_dve)

            def u_fin(u=u, h=h, J=J):
                ptb = ptb_rot.next()
                pe(lambda: T.transpose(out=ptb[:, 0:128], in_=u[:], identity=ident_b[:]), r=[u, ident_b], w=[ptb])
                act(lambda: A.copy(out=uT_own[:, h, J * 128:(J + 1) * 128], in_=ptb[:, 0:128]), r=[ptb], w=[(uT_own, (h, J))])
            u_pending.append(u_fin)
            if Cbf_next is not None:
                Cbf = Cbf_next
    for f_ in u_pending_dve:
        f_()
    while u_pending:
        u_pending.pop(0)()
    pb_rot = Rot(pbs[0:6])
    pm.close()
    if "uT" in dbg:
        uf = sb(pa, "uf_dbg", [128, 4, NJ * 128], F32)
        dve(lambda: V.tensor_copy(out=uf[:], in_=uT_own[:]), r=[uT_own], w=[uf])
        dump("uT", uf[:], [uf], dbg["uT"].rearrange("(h p) t -> p h t", p=128))
        if "oT" not in dbg:
            final_wait()
            pes.close(); fw.close()
            return nc

    pn = Scope(arena)
    KcT = [sb(pn, "KcT%d" % g, [68, 256], BF16) for g in range(2)]
    Vc = [sb(pn, "Vc%d" % g, [128, 2, 64], BF16) for g in range(2)]
    OV = sb(pn, "OV", [128, 2, 64], BF16)
    fw.dma("sp", OV[:], dram["ov"], writes=[OV])
    pcm = Scope(arena)
    cmpT = sb(pcm, "cmpT", [128, S], BF16)
    w1sb = sb(pcm, "w1sb", [128, 32, 128], BF16)
    wcm = sb(pcm, "wcm", [128, KC, 128], BF16)
    w2b = sb(pcm, "w2b", [128, 64], BF16)
    posT = sb(pcm, "posT", [128, 32], BF16)
    posTf = sb(pcm, "posTf", [128, 32], F32)
    b1 = sb(pcm, "b1", [128, 1], F32)
    xb = sb(pcm, "xb", [128, 256], F32)
    tg = sb(pcm, "tg", [128, 256], F32)
    hidb = sb(pcm, "hidb", [128, 256], BF16)
    for which in ("k", "v"):
        w1d = dram["cmp_%s_w1" % which].rearrange("(pos d) h -> d pos h", d=64)
        for q8 in range(4):
            wst = wst_rot.next()
            stv = wst[:, 0:1024].rearrange("p (a n) -> p a n", a=8)
            fw.dma("sp", stv[0:64], w1d[:, q8 * 8:(q8 + 1) * 8, :], writes=[wst])
            fw.dma("sp", stv[64:128], w1d[:, q8 * 8:(q8 + 1) * 8, :], writes=[wst])
            act(lambda: A.copy(out=w1sb[:, q8 * 8:(q8 + 1) * 8, :], in_=stv), r=[wst], w=[w1sb])
        load_w(w2b[:], dram["cmp_%s_w2" % which], 64, [w2b])
        fw.dma("sp", posTf[0:64], dram["cmp_pos_%sT" % which], writes=[posTf])
        fw.dma("sp", posTf[64:128], dram["cmp_pos_%sT" % which], writes=[posTf])
        dve(lambda: V.tensor_copy(out=posT[:], in_=posTf[:]), r=[posTf], w=[posT])
        load_w_cols(wcm, 0, w_in, C_KCMP if which == "k" else C_VCMP, 128)
        for c in range(8):
            ps = pb_rot.next()
            for k in range(KC):
                pe(lambda: T.matmul(ps[:, :], lhsT=wcm[:, k, :], rhs=hT_all[:, k, c * 512:(c + 1) * 512], start=(k == 0), stop=(k == KC - 1)),
                   r=[wcm, (hT_all, c)], w=[ps])
            if c % 2:
                act(lambda: A.copy(out=cmpT[:, c * 512:(c + 1) * 512], in_=ps[:, :]), r=[ps], w=[(cmpT, c)])
            else:
                dve(lambda: V.tensor_copy(out=cmpT[:, c * 512:(c + 1) * 512], in_=ps[:, :]), r=[ps], w=[(cmpT, c)])
        maybe_stop("s0" + which)
        pb1 = pb_rot.next()
        for pos in range(32):
            pe(lambda: T.matmul(pb1[:, 0:1], lhsT=w1sb[0:64, pos, :], rhs=posT[0:64, pos:pos + 1], start=(pos == 0), stop=(pos == 31)),
               r=[w1sb, posT], w=[pb1])
        dve(lambda: V.tensor_copy(out=b1[:], in_=pb1[:, 0:1]), r=[pb1], w=[b1])
        maybe_stop("s1" + which)
        c16 = cmpT[:].rearrange("p (j s) -> p j s", s=16)
        for g in range(2):
            ph = pb_rot.next()
            for pos in range(32):
                rhs = c16[g * 64:(g + 1) * 64, 0:255, pos] if pos < 16 else c16[g * 64:(g + 1) * 64, 1:256, pos - 16]
                pe(lambda: T.matmul(ph[:, 0:255], lhsT=w1sb[g * 64:(g + 1) * 64, pos, :], rhs=rhs, start=(pos == 0), stop=(pos == 31)),
                   r=[w1sb, cmpT], w=[ph])
            act(lambda: A.activation(out=xb[:, 0:255], in_=ph[:, 0:255], func=AF.Identity, bias=b1[:, 0:1]), r=[ph, b1], w=[xb])
            dve(lambda: V.tensor_mul(out=tg[:, 0:255], in0=xb[:, 0:255], in1=xb[:, 0:255]), r=[xb], w=[tg])
            dve(lambda: V.tensor_scalar(out=tg[:, 0:255], in0=tg[:, 0:255], scalar1=0.044715, scalar2=1.0, op0=ALU.mult, op1=ALU.add), r=[tg], w=[tg])
            dve(lambda: V.tensor_mul(out=tg[:, 0:255], in0=tg[:, 0:255], in1=xb[:, 0:255]), r=[tg, xb], w=[tg])
            act(lambda: A.activation(out=tg[:, 0:255], in_=tg[:, 0:255], func=AF.Sigmoid, scale=1.5957691216057308), r=[tg], w=[tg])
            dve(lambda: V.memset(hidb[:], 0.0), w=[hidb])
            dve(lambda: V.tensor_mul(out=hidb[:, 0:255], in0=xb[:, 0:255], in1=tg[:, 0:255]), r=[xb, tg], w=[hidb])
            pk = pb_rot.next()
            if which == "k":
                pe(lambda: T.matmul(pk[0:64, 0:255], lhsT=w2b[:], rhs=hidb[:, 0:255], start=True, stop=True), r=[w2b, hidb], w=[pk])
                dve(lambda: V.memset(KcT[g][0:64, :], 0.0), w=[KcT[g]])
                dve(lambda: V.tensor_copy(out=KcT[g][0:64, 0:255], in_=pk[0:64, 0:255]), r=[pk], w=[KcT[g]])
                fw.dma("sp", KcT[g][64:68, :], dram["poskc"], writes=[KcT[g]])
            else:
                for c in range(2):
                    pe(lambda: T.matmul(pk[:, c * 64:(c + 1) * 64], lhsT=hidb[:, c * 128:(c + 1) * 128], rhs=w2b[:], start=True, stop=True),
                       r=[w2b, hidb], w=[pk])
                dve(lambda: V.tensor_copy(out=Vc[g][:], in_=pk[:, 0:128].rearrange("p (c d) -> p c d", c=2)), r=[pk], w=[Vc[g]])
    maybe_stop("s2")
    pcm.close()
    KTn = {}
    for b_ in ("slc", "win"):
        for g in range(2):
            KTn[(b_, g)] = sb(pn, "KT_%s%d" % (b_, g), [68, S], BF16)
    Vn = {b_: sb(pn, "V_%s" % b_, [128, NT, 2, 65], BF16) for b_ in ("slc", "win")}
    pkv = Scope(arena)
    wkn = sb(pkv, "wkn", [128, KC, 256], BF16)
    wvn = sb(pkv, "wvn", [128, KC, 256], BF16)
    load_w_cols(wkn, 0, w_in, C_KSLC, 128)
    load_w_cols(wkn, 128, w_in, C_KWIN, 128)
    load_w_cols(wvn, 0, w_in, C_VSLC, 128)
    load_w_cols(wvn, 128, w_in, C_VWIN, 128)
    i_ev = 0
    for bi_, b_ in enumerate(("slc", "win")):
        for g in range(2):
            kt_ = KTn[(b_, g)]
            fw.dma("sp", kt_[64:68, :], dram["posk"], writes=[(kt_, "pos")])
            for c in range(8):
                ps = pb_rot.next()
                for k in range(KC):
                    pe(lambda: T.matmul(ps[0:64, :], lhsT=wkn[:, k, bi_ * 128 + g * 64: bi_ * 128 + (g + 1) * 64], rhs=hT_all[:, k, c * 512:(c + 1) * 512],
                                        start=(k == 0), stop=(k == KC - 1)), r=[wkn, (hT_all, c)], w=[ps])
                i_ev += 1
                if i_ev % 2:
                    act(lambda: A.copy(out=kt_[0:64, c * 512:(c + 1) * 512], in_=ps[0:64, :]), r=[ps], w=[(kt_, c)])
                else:
                    dve(lambda: V.tensor_copy(out=kt_[0:64, c * 512:(c + 1) * 512], in_=ps[0:64, :]), r=[ps], w=[(kt_, c)])
    maybe_stop("s3")
    for b_ in ("slc", "win"):
        pool(lambda: G.memset(Vn[b_][:], 1.0), w=[Vn[b_]])
    maybe_stop("s4")
    for t2 in range(NT // 2):
        ps = pb_rot.next()
        for i in range(2):
            t = t2 * 2 + i
            for k in range(KC):
                pe(lambda: T.matmul(ps[:, i * 256:(i + 1) * 256], lhsT=hT_all[:, k, t * 128:(t + 1) * 128], rhs=wvn[:, k, :], start=(k == 0), stop=(k == KC - 1)),
                   r=[(hT_all, t // 4), wvn], w=[ps])
        for i in range(2):
            t = t2 * 2 + i
            for bi_, b_ in enumerate(("slc", "win")):
                src = ps[:, i * 256 + bi_ * 128: i * 256 + (bi_ + 1) * 128].rearrange("p (g d) -> p g d", g=2)
                dve(lambda: V.tensor_copy(out=Vn[b_][:, t, :, 0:64], in_=src), r=[ps], w=[(Vn[b_], t2)])
    maybe_stop("s5")
    pkv.close()
    if "stopB" in dbg:
        uf = sb(pa, "kc_dbg", [128, 128], F32)
        dve(lambda: V.tensor_copy(out=uf[:], in_=Vc[0][:].rearrange("p c d -> p (c d)")), r=[Vc[0]], w=[uf])
        dump("stopB", uf[:], [uf])
        final_wait()
        pes.close(); fw.close()
        return nc
    pa.close()

    oT_own = sb(ps1, "oT_own", [128, 4, NJ * 128], BF16, top=True)
    xs_rot = Rot([sb(gs, "xs%d" % i, [128, D], F32) for i in range(3)])
    xn_rot = Rot([sb(gs, "xn%d" % i, [128, D], BF16) for i in range(3)])
    pq_ = Scope(arena)
    eall = sb(pq_, "eall", [64, S], BF16)
    fw.dma("sp", eall[:], dram["eall"], writes=[eall])
    cmask = sb(pq_, "cmask", [128, NJ, 2, 128], BF16)
    fw.dma("sp", cmask[:], dram["cmask"], writes=[cmask])
    wmask = sb(pq_, "wmask", [128, 6, 128], BF16)
    fw.dma("sp", wmask[:], dram["wmask"], writes=[wmask])
    selm = sb(pq_, "selm", [128, NJ, 64], F32)
    fw.dma("sp", selm[:], dram["selm"], writes=[selm])
    selc = sb(pq_, "selc", [128, NJ, 64], F32)
    fw.dma("sp", selc[:], dram["selc"], writes=[selc])
    wnq = sb(pq_, "wnq", [128, KC, 512], BF16)
    load_w_cols(wnq, 0, w_in, C_NSQ, 512)
    wng = sb(pq_, "wng", [128, KC, 24], BF16)
    load_w_cols(wng, 0, w_in, C_NSG, 24)
    hJ_rot = Rot([sb(pq_, "hJ%d" % i, [128, KC, 128], BF16) for i in range(2)])
    QTg_rot = [Rot([sb(pq_, "QTg%d_%d" % (g, i), [68, 512], BF16) for i in range(2)]) for g in range(2)]
    gsig_rot = Rot([sb(pq_, "gsig%d" % i, [128, 24], F32) for i in range(3)])
    P_rot = Rot([sb(pq_, "P%d" % i, [128, 512], BF16) for i in range(4)])
    selT_g = [sb(pq_, "selT%d" % g, [64, 128], BF16) for g in range(2)]
    cu_sb_g = [sb(pq_, "cu_sb%d" % g, [128, 512], F32) for g in range(2)]
    Msb_rot = Rot([sb(pq_, "Msb%d" % i, [128, NT, 128], BF16) for i in range(2)])
    sc_ = sb(pq_, "sc", [128, 64], F32)
    scw = sb(pq_, "scw", [128, 64], F32)
    psl = sb(pq_, "psl", [128, 64], F32)
    nsb_g = [sb(pq_, "nsb%d" % g, [128, 64], BF16) for g in range(2)]
    m16 = sb(pq_, "m16", [128, 16], F32)
    dd_g = [sb(pq_, "dd%d" % g, [128, 3, 4], F32) for g in range(2)]
    gr = sb(pq_, "gr", [128, 3, 4], F32)
    o_f = sb(pq_, "o_f", [128, 4, 64], F32)
    o_t = sb(pq_, "o_t", [128, 4, 64], F32)
    o_b = sb(pq_, "o_b", [128, 256], BF16)
    S_rot = Rot(pbs[0:3])
    MB = pbs[6]
    CU, OW, OS, PQ = pbs[3], pbs[4], pbs[5], pbs[6]
    PT = ptbs[1]
    load_gain("norm_mix")
    bc4 = lambda ap2: ap2.unsqueeze(1).to_broadcast([128, 4, 128])

    oc_sb_g = [sb(pq_, "oc_sb%d" % g, [128, 256], F32) for g in range(2)]
    ow_sb_g = [sb(pq_, "ow_sb%d" % g, [128, 260], F32) for g in range(2)]
    os_sb = sb(pq_, "os_sb", [128, 260], F32)
    ob_rot = Rot([sb(pq_, "o_b%d" % i, [128, 256], BF16) for i in range(2)])

    def emit_S(tl):
        Sp = S_rot.next()
        tl["Sp"] = Sp
        extra = tl["extra"]
        pe(lambda: T.matmul(Sp[:, :], lhsT=tl["kT"], rhs=tl["QT"][0:68, :], start=True, stop=(len(extra) == 0)), r=tl["kbufs"] + [tl["QT"]], w=[Sp])
        for i, (l_ap, r_ap, bufs) in enumerate(extra):
            pe(lambda: T.matmul(Sp[:, :], lhsT=l_ap, rhs=r_ap, start=False, stop=(i == len(extra) - 1)), r=bufs, w=[Sp])
        Pb = P_rot.next()
        tl["Pb"] = Pb
        act(lambda: A.activation(out=Pb[:], in_=Sp[:, :], func=AF.Exp), r=[Sp], w=[Pb])

    def emit_PV(tl):
        Pb = tl["Pb"]
        if tl["kind"] == "slc":
            Msb = state[("Msb", tl["g"])]
            kt_ = tl["kt"]
            dve(lambda: V.tensor_tensor(out=Pb[:].rearrange("p (h t) -> p h t", h=4), in0=Pb[:].rearrange("p (h t) -> p h t", h=4),
                                        in1=Msb[:, kt_, :].unsqueeze(1).to_broadcast([128, 4, 128]), op=ALU.mult), r=[Pb, (Msb, kt_ // 4)], w=[Pb])
        Oacc, vrhs, vbufs, first, last, also = tl["Oacc"], tl["vrhs"], tl["vbufs"], tl["first"], tl["last"], tl.get("also")
        W = vrhs.shape[-1]
        for hh in range(4):
            pe(lambda: T.matmul(Oacc[0][:, Oacc[1] + hh * W: Oacc[1] + (hh + 1) * W], lhsT=Pb[:, hh * 128:(hh + 1) * 128], rhs=vrhs,
                                start=(first and hh == 0), stop=last, skip_group_check=True),
               r=[Pb] + vbufs, w=[Oacc[0]])
            if also is not None:
                a_ap, a_bufs, a_off = also
                pe(lambda: T.matmul(Oacc[0][:, a_off + hh * 64: a_off + (hh + 1) * 64], lhsT=Pb[:, hh * 128:(hh + 1) * 128], rhs=a_ap,
                                    start=False, stop=last, skip_group_check=True),
                   r=[Pb] + a_bufs, w=[Oacc[0]])

    def prepDMA(J):
        if J not in state["xs"]:
            state["xs"][J] = load_x(x_own[J * 128:(J + 1) * 128, :])
        return state["xs"][J]

    def prepA(J):
        if J not in state["prepA"]:
            state["prepA"][J] = rmsnorm_front(prepDMA(J))
        return state["prepA"][J]

    def prepB1(J):
        if J not in state["hJ"]:
            hJ = hJ_rot.next()
            rmsnorm_back(prepA(J), hJ, 0)
            state["hJ"][J] = hJ
        return state["hJ"][J]

    def prepB2(J):
        if J not in state["gsig"]:
            hJ = prepB1(J)
            gsig = gsig_rot.next()
            for k in range(KC):
                pe(lambda: T.matmul(PQ[:, 0:24], lhsT=hJ[:, k, :], rhs=wng[:, k, :], start=(k == 0), stop=(k == KC - 1)), r=[hJ, wng], w=[PQ])
            act(lambda: A.activation(out=gsig[:], in_=PQ[:, 0:24], func=AF.Sigmoid), r=[PQ], w=[gsig])
            state["gsig"][J] = gsig
        return state["gsig"][J]

    def prepQ(J, g):
        if (J, g) not in state["QT"]:
            hJ = prepB1(J)
            prepB2(J)
            QT = QTg_rot[g].next()
            for hh in range(4):
                hd = g * 4 + hh
                for k in range(KC):
                    pe(lambda: T.matmul(PQ[0:64, hh * 128:(hh + 1) * 128], lhsT=wnq[:, k, hd * 64:(hd + 1) * 64], rhs=hJ[:, k, :], start=(k == 0), stop=(k == KC - 1)),
                       r=[hJ, wnq], w=[PQ])
            dve(lambda: V.tensor_scalar(out=QT[0:64, :], in0=PQ[0:64, :], scalar1=0.125, scalar2=None, op0=ALU.mult), r=[PQ], w=[QT])
            fw.dma("pool", QT[64:68, :], dram["posq"][g, :, J * 512:(J + 1) * 512], writes=[QT])
            state["QT"][(J, g)] = QT
        return state["QT"][(J, g)]

    def prep(J):
        return dict(gsig=prepB2(J), QT=[prepQ(J, 0), prepQ(J, 1)])

    def topk_dve(J, g):
        dd, nsb, oc_sb, cu_sb = dd_g[g], nsb_g[g], oc_sb_g[g], cu_sb_g[g]
        act(lambda: A.copy(out=cu_sb[:], in_=CU[:, :]), r=[CU], w=[cu_sb])
        U3 = cu_sb[:, 256:512].rearrange("p (h b) -> p h b", h=4)
        dve(lambda: V.tensor_reduce(out=dd[:, 0, :], in_=U3, axis=AX.X, op=ALU.add), r=[cu_sb], w=[dd])
        dve(lambda: V.tensor_scalar_max(out=dd[:, 0, :], in0=dd[:, 0, :], scalar1=1e-30), r=[dd], w=[dd])
        dve(lambda: V.reciprocal(out=dd[:, 0, :], in_=dd[:, 0, :]), r=[dd], w=[dd])
        dve(lambda: V.tensor_scalar(out=psl[:], in0=U3[:, 0, :], scalar1=dd[:, 0, 0:1], scalar2=None, op0=ALU.mult), r=[cu_sb, dd], w=[psl])
        for hh in range(1, 4):
            dve(lambda: V.scalar_tensor_tensor(out=psl[:], in0=U3[:, hh, :], scalar=dd[:, 0, hh:hh + 1], in1=psl[:], op0=ALU.mult, op1=ALU.add),
                r=[cu_sb, dd, psl], w=[psl])
        dve(lambda: V.tensor_mul(out=sc_[:], in0=psl[:], in1=selm[:, J, :]), r=[psl, selm], w=[sc_])
        dve(lambda: V.tensor_add(out=sc_[:], in0=sc_[:], in1=selc[:, J, :]), r=[sc_, selc], w=[sc_])
        dve(lambda: V.max(out=m16[:, 0:8], in_=sc_[:]), r=[sc_], w=[m16])
        dve(lambda: V.match_replace(out=scw[:], in_to_replace=m16[:, 0:8], in_values=sc_[:], imm_value=-1e9), r=[sc_, m16], w=[scw])
        dve(lambda: V.max(out=m16[:, 8:16], in_=scw[:]), r=[scw], w=[m16])
        dve(lambda: V.tensor_tensor(out=nsb[:], in0=sc_[:], in1=m16[:, 15:16].to_broadcast([128, 64]), op=ALU.is_ge), r=[sc_, m16], w=[nsb])
        dve(lambda: V.tensor_copy(out=oc_sb[:], in_=cu_sb[:, 0:256]), r=[cu_sb], w=[oc_sb])

    def topk_pe(g):
        nsb, selT = nsb_g[g], selT_g[g]
        pe(lambda: T.transpose(out=PT[0:64, 0:128], in_=nsb[:], identity=ident_b[:]), r=[nsb, ident_b], w=[PT])
        dve(lambda: V.tensor_copy(out=selT[:], in_=PT[0:64, 0:128]), r=[PT], w=[selT])

    def mask_group(J, g, k4):
        if k4 == 0:
            state[("Msb", g)] = Msb_rot.next()
        Msb = state[("Msb", g)]
        selT = selT_g[g]
        nk = 2 * J + 2
        n4 = min(4, nk - k4)
        for i in range(n4):
            kt = k4 + i
            pe(lambda: T.matmul(MB[:, i * 128:(i + 1) * 128], lhsT=eall[:, kt * 128:(kt + 1) * 128], rhs=selT[:], start=True, stop=True),
               r=[eall, selT], w=[MB])
        act(lambda: A.copy(out=Msb[:, k4:k4 + n4, :], in_=MB[:, 0:n4 * 128].rearrange("p (k t) -> p k t", k=n4)), r=[MB], w=[(Msb, k4 // 4)])

    def evac_ow(g):
        act(lambda: A.copy(out=ow_sb_g[g][:], in_=OW[:, 0:260]), r=[OW], w=[ow_sb_g[g]])

    def combine(J, g, gsig):
        dd, oc_sb, ow_sb = dd_g[g], oc_sb_g[g], ow_sb_g[g]
        act(lambda: A.copy(out=os_sb[:], in_=OS[:, 0:260]), r=[OS], w=[os_sb])
        g3 = gsig[:, g * 12:(g + 1) * 12].rearrange("p (h b) -> p h b", b=3)
        OW3 = ow_sb[:].rearrange("p (h w) -> p h w", h=4)
        OS3 = os_sb[:].rearrange("p (h w) -> p h w", h=4)
        OC3 = oc_sb[:].rearrange("p (h w) -> p h w", h=4)
        dve(lambda: V.tensor_copy(out=dd[:, 1, :], in_=OS3[:, :, 64]), r=[os_sb], w=[dd])
        dve(lambda: V.tensor_copy(out=dd[:, 2, :], in_=OW3[:, :, 64]), r=[ow_sb], w=[dd])
        dve(lambda: V.reciprocal(out=dd[:, 1:3, :], in_=dd[:, 1:3, :]), r=[dd], w=[dd])
        dve(lambda: V.tensor_mul(out=gr[:], in0=dd[:], in1=g3.rearrange("p h b -> p b h")), r=[dd, gsig], w=[gr])
        dve(lambda: V.tensor_tensor(out=o_f[:], in0=OC3, in1=gr[:, 0, :].unsqueeze(2).to_broadcast([128, 4, 64]), op=ALU.mult), r=[oc_sb, gr], w=[o_f])
        dve(lambda: V.tensor_tensor(out=o_t[:], in0=OS3[:, :, 0:64], in1=gr[:, 1, :].unsqueeze(2).to_broadcast([128, 4, 64]), op=ALU.mult), r=[os_sb, gr], w=[o_t])
        dve(lambda: V.tensor_add(out=o_f[:], in0=o_f[:], in1=o_t[:]), r=[o_f, o_t], w=[o_f])
        dve(lambda: V.tensor_tensor(out=o_t[:], in0=OW3[:, :, 0:64], in1=gr[:, 2, :].unsqueeze(2).to_broadcast([128, 4, 64]), op=ALU.mult), r=[ow_sb, gr, o_t], w=[o_t])
        ob = ob_rot.next()
        dve(lambda: V.tensor_add(out=ob[:].rearrange("p (h d) -> p h d", h=4), in0=o_f[:], in1=o_t[:]), r=[o_f, o_t], w=[ob])

        def fin():
            for i in range(2):
                pe(lambda: T.transpose(out=PT[:, 128 + i * 128:256 + i * 128], in_=ob[:, i * 128:(i + 1) * 128], identity=ident_b[:]), r=[ob, ident_b], w=[PT])
            act(lambda: A.copy(out=oT_own[:, g * 2:g * 2 + 2, J * 128:(J + 1) * 128], in_=PT[:, 128:384].rearrange("p (i t) -> p i t", i=2)), r=[PT], w=[(oT_own, (g, J))])
        return fin

    seq = []
    state = {"prep": {}, "prepA": {}, "xs": {}, "hJ": {}, "gsig": {}, "QT": {}, "deferred": []}

    def get_prep(J):
        if J not in state["prep"]:
            state["prep"][J] = prep(J)
        return state["prep"][J]

    def add_post(tl, f_):
        tl.setdefault("post", []).append(f_)

    for J in range(NJ):
        cmp_t, win_t, slc_t = [[], []], [[], []], [[], []]
        for g in range(2):
            chunks = [0] if J <= 7 else [0, 1]
            for ci, c in enumerate(chunks):
                cmp_t[g].append(dict(J=J, g=g, kind="cmp", kT=KcT[g][0:68, c * 128:(c + 1) * 128], kbufs=[KcT[g]],
                                     extra_fn=(lambda J=J, c=c: [(ident_b[:], bc4(cmask[:, J, c, :]), [ident_b, cmask])]),
                                     Oacc=(CU, 0), vrhs=Vc[g][:, c, :], vbufs=[Vc[g]], first=(ci == 0), last=(ci == len(chunks) - 1),
                                     also=(OV[:, c, :], [OV], 256)))
            add_post(cmp_t[g][-1], lambda J=J, g=g: topk_dve(J, g))
            wl = [(r_, 2 * J - 4 + r_) for r_ in range(6) if 2 * J - 4 + r_ >= 0]
            kbw = KTn[("win", g)]
            for wi, (r_, kt) in enumerate(wl):
                win_t[g].append(dict(J=J, g=g, kind="win", kT=kbw[0:68, kt * 128:(kt + 1) * 128], kbufs=[(kbw, kt // 4), (kbw, "pos")],
                                     extra_fn=(lambda r_=r_: [(ident_b[:], bc4(wmask[:, r_, :]), [ident_b, wmask])] if r_ in (0, 1, 4, 5) else []),
                                     Oacc=(OW, 0), vrhs=Vn["win"][:, kt, g, :], vbufs=[(Vn["win"], kt // 2)], first=(wi == 0), last=(wi == len(wl) - 1)))
            add_post(win_t[g][-1], lambda g=g: evac_ow(g))
            nk = 2 * J + 2
            kbs = KTn[("slc", g)]
            for kt in range(nk):
                def ex(kt=kt, J=J):
                    e_ = []
                    if kt >= 2 * J:
                        e_.append((ident_b[:], bc4(wmask[:, 4 + kt - 2 * J, :]), [ident_b, wmask]))
                    return e_
                slc_t[g].append(dict(J=J, g=g, kind="slc", kt=kt, kT=kbs[0:68, kt * 128:(kt + 1) * 128], kbufs=[(kbs, kt // 4), (kbs, "pos")], extra_fn=ex,
                                     Oacc=(OS, 0), vrhs=Vn["slc"][:, kt, g, :], vbufs=[(Vn["slc"], kt // 2)], first=(kt == 0), last=(kt == nk - 1)))

            def comb_hook(J=J, g=g):
                state["deferred"].append(combine(J, g, get_prep(J)["gsig"]))
            add_post(slc_t[g][-1], comb_hook)
        nw = len(win_t[0])
        add_post(win_t[0][nw - 1], lambda: topk_pe(0))
        add_post(win_t[1][nw - 1], lambda: topk_pe(1))
        slot_list = cmp_t[0] + cmp_t[1] + win_t[0] + win_t[1] + slc_t[0] + slc_t[1]
        idx0 = slot_list.index(win_t[1][min(1, nw - 1)])
        idx1 = slot_list.index(slc_t[0][min(1, len(slc_t[0]) - 1)])
        for gi, ib in ((0, idx0), (1, idx1)):
            for i, k4 in enumerate(range(0, 2 * J + 2, 4)):
                add_post(slot_list[ib + i], lambda J=J, gi=gi, k4=k4: mask_group(J, gi, k4))

        def flush():
            for f_ in state["deferred"]:
                f_()
            state["deferred"] = []
        add_post(win_t[1][min(2, nw - 1)], flush)
        cmp_t[0][0]["pre"] = [lambda J=J: (get_prep(J), prepA(J + 1) if J + 1 < NJ else None, prepDMA(J + 2) if J + 2 < NJ else None)]
        if J + 1 < NJ:
            nw0 = len(win_t[0])
            add_post(win_t[0][0], lambda J=J: prepB1(J + 1))
            add_post(win_t[0][min(1, nw0 - 1)], lambda J=J: prepB2(J + 1))
            add_post(win_t[0][min(2, nw0 - 1)], lambda J=J: prepQ(J + 1, 0))
            add_post(win_t[0][min(3, nw0 - 1)], lambda J=J: prepQ(J + 1, 1))
        seq += slot_list

    def do_S(tl):
        for f_ in tl.get("pre", []):
            f_()
        tl["QT"] = get_prep(tl["J"])["QT"][tl["g"]]
        tl["extra"] = tl["extra_fn"]()
        emit_S(tl)

    LA = 2
    for i in range(min(LA, len(seq))):
        do_S(seq[i])
    for i in range(len(seq)):
        if i + LA < len(seq):
            do_S(seq[i + LA])
        emit_PV(seq[i])
        for f_ in seq[i].get("post", []):
            f_()
    for f_ in state["deferred"]:
        f_()
    pq_.close()
    pn.close()
    if "oT" in dbg:
        uf = sb(ps1, "of_dbg", [128, 4, NJ * 128], F32)
        dve(lambda: V.tensor_copy(out=uf[:], in_=oT_own[:]), r=[oT_own], w=[uf])
        dump("oT", uf[:], [uf], dbg["oT"].rearrange("(h p) t -> p h t", p=128))
        if "x1" not in dbg:
            final_wait()
            pes.close(); fw.close()
            return nc

    pd = Scope(arena)
    hT_own = sb(pd, "hT_own", [128, KC, NJ * 128], BF16)
    zT = sb(pd, "zT", [128, KC, NJ * 128], BF16)
    wbm = sb(pd, "wbm", [128, 4, D], BF16)
    wbn = sb(pd, "wbn", [128, 4, D], BF16)
    load_w_cols(wbm, 0, dram["w_br_mlstm"], 0, D, kc=4)
    load_w_cols(wbn, 0, dram["w_br_nsa"], 0, D, kc=4)
    load_gain("norm_mix")
    norm_loop([x_own[J * 128:(J + 1) * 128, :] for J in range(NJ)], hT_own, lambda J: J // 4)
    wmg_rot = Rot([sb(pd, "wmg%d" % i, [128, KC, 128], BF16) for i in range(4)])
    sg_rot = Rot([sb(pd, "sg%d" % i, [128, 512], F32) for i in range(4)])
    for cc in range(8):
        wA, wB = wmg_rot.next(), wmg_rot.next()
        load_w_cols(wA, 0, w_in, C_MRG + cc * 128, 128)
        load_w_cols(wB, 0, w_in, C_MRG + 1024 + cc * 128, 128)
        for tc in range(4):
            tsl = slice(tc * 512, (tc + 1) * 512)
            pA, pB, pC, pD = pb_rot.next(), pb_rot.next(), pb_rot.next(), pb_rot.next()
            for k in range(KC):
                pe(lambda: T.matmul(pA[:, :], lhsT=wA[:, k, :], rhs=hT_own[:, k, tsl], start=(k == 0), stop=(k == KC - 1)), r=[wA, (hT_own, tc)], w=[pA])
            for k in range(KC):
                pe(lambda: T.matmul(pB[:, :], lhsT=wB[:, k, :], rhs=hT_own[:, k, tsl], start=(k == 0), stop=(k == KC - 1)), r=[wB, (hT_own, tc)], w=[pB])
            for k in range(4):
                pe(lambda: T.matmul(pC[:, :], lhsT=wbm[:, k, cc * 128:(cc + 1) * 128], rhs=uT_own[:, k, tsl], start=(k == 0), stop=(k == 3)), r=[wbm, uT_own], w=[pC])
            for k in range(4):
                pe(lambda: T.matmul(pD[:, :], lhsT=wbn[:, k, cc * 128:(cc + 1) * 128], rhs=oT_own[:, k, tsl], start=(k == 0), stop=(k == 3)), r=[wbn, oT_own], w=[pD])
            sA, sB = sg_rot.next(), sg_rot.next()
            act(lambda: A.activation(out=sA[:], in_=pA[:, :], func=AF.Sigmoid), r=[pA], w=[sA])
            act(lambda: A.activation(out=sB[:], in_=pB[:, :], func=AF.Sigmoid), r=[pB], w=[sB])
            dve(lambda: V.tensor_mul(out=sA[:], in0=sA[:], in1=pC[:, :]), r=[sA, pC], w=[sA])
            dve(lambda: V.tensor_mul(out=sB[:], in0=sB[:], in1=pD[:, :]), r=[sB, pD], w=[sB])
            dve(lambda: V.tensor_add(out=zT[:, cc, tsl], in0=sA[:], in1=sB[:]), r=[sA, sB], w=[(zT, tc)])
    pd_keep = [zT]
    for b_ in pd.bufs:
        if b_ is not zT:
            arena.release(b_)
    pd.bufs = [zT]
    ps1.close()
    px = Scope(arena)
    xr = sb(px, "xr", [128, NJ, D], F32)
    wmo = sb(pd, "wmo", [128, KC, D], BF16)
    load_w_cols(wmo, 0, dram["w_mix_out"], 0, D)
    for J in range(NJ):
        xs_buf = xs_rot.next()
        fw.dma("sp", xs_buf[:], x_own[J * 128:(J + 1) * 128, :], writes=[xs_buf])
        for hf in range(2):
            po = pb_rot.next()
            for k in range(KC):
                pe(lambda: T.matmul(po[:, :], lhsT=zT[:, k, J * 128:(J + 1) * 128], rhs=wmo[:, k, hf * 512:(hf + 1) * 512], start=(k == 0), stop=(k == KC - 1)),
                   r=[(zT, J // 4), wmo], w=[po])
            dve(lambda: V.tensor_add(out=xr[:, J, hf * 512:(hf + 1) * 512], in0=po[:, :], in1=xs_buf[:, hf * 512:(hf + 1) * 512]), r=[po, xs_buf], w=[(xr, J)])
    pd.close()
    if "x1" in dbg:
        for J in range(NJ):
            dump("x1", xr[:, J, :], [(xr, J)], dbg["x1"][J * 128:(J + 1) * 128, :])
        if "x2" not in dbg:
            final_wait()
            pes.close(); fw.close()
            return nc

    pe_ = Scope(arena)
    ph = Scope(arena)
    qT = sb(pe_, "qT", [128, KC, NJ * 128], BF16)
    hxT = sb(ph, "hxT", [128, KC, NJ * 128], BF16)
    ones_b = sb(pe_, "ones_b", [128, 128], BF16)
    pool(lambda: G.memset(ones_b[:], 1.0), w=[ones_b])
    memT = sb(pe_, "memT", [128, KC, 256], BF16)
    load_gain("norm_mem")
    for t in range(2):
        rmsnorm_T(mem[t * 128:(t + 1) * 128, :], memT, t * 128)
    KxT = sb(pe_, "KxT", [128, KC, 256], BF16)
    Vx = sb(pe_, "Vx", [128, 2, D], BF16)
    wch_rot = Rot([sb(pe_, "wch%d" % i, [128, KC, 128], BF16) for i in range(3)])
    for j in range(8):
        wc = wch_rot.next()
        load_w_cols(wc, 0, dram["xa_wkv"], j * 128, 128)
        ps = pb_rot.next()
        for k in range(KC):
            pe(lambda: T.matmul(ps[:, 0:256], lhsT=wc[:, k, :], rhs=memT[:, k, :], start=(k == 0), stop=(k == KC - 1)), r=[wc, memT], w=[ps])
        dve(lambda: V.tensor_copy(out=KxT[:, j, :], in_=ps[:, 0:256]), r=[ps], w=[KxT])
    for j in range(8):
        wc = wch_rot.next()
        load_w_cols(wc, 0, dram["xa_wkv"], D + j * 128, 128)
        ps = pb_rot.next()
        for mc in range(2):
            for k in range(KC):
                pe(lambda: T.matmul(ps[:, mc * 128:(mc + 1) * 128], lhsT=memT[:, k, mc * 128:(mc + 1) * 128], rhs=wc[:, k, :], start=(k == 0), stop=(k == KC - 1)),
                   r=[wc, memT], w=[ps])
        dve(lambda: V.tensor_copy(out=Vx[:, :, j * 128:(j + 1) * 128], in_=ps[:, 0:256].rearrange("p (m d) -> p m d", m=2)), r=[ps], w=[Vx])
    load_gain("norm_xattn")
    norm_loop([(xr[:, J, :], xr) for J in range(NJ)], hxT, lambda J: J // 4)
    for j in range(8):
        wc = wch_rot.next()
        load_w_cols(wc, 0, dram["xa_wq"], j * 128, 128)
        for tc in range(4):
            ps = pb_rot.next()
            for k in range(KC):
                pe(lambda: T.matmul(ps[:, :], lhsT=wc[:, k, :], rhs=hxT[:, k, tc * 512:(tc + 1) * 512], start=(k == 0), stop=(k == KC - 1)), r=[wc, (hxT, tc)], w=[ps])
            if tc % 2:
                act(lambda: A.copy(out=qT[:, j, tc * 512:(tc + 1) * 512], in_=ps[:, :]), r=[ps], w=[(qT, j)])
            else:
                dve(lambda: V.tensor_copy(out=qT[:, j, tc * 512:(tc + 1) * 512], in_=ps[:, :]), r=[ps], w=[(qT, j)])
    ph.close()
    oTx = sb(pe_, "oTx", [128, KC, NJ * 128], BF16)
    Px_rot = Rot([sb(pe_, "Px%d" % i, [128, 512], BF16) for i in range(4)])
    rdx_rot = Rot([sb(pe_, "rdx%d" % i, [128, 512], F32) for i in range(2)])
    def xa_S(h, tc):
        tsl = slice(tc * 512, (tc + 1) * 512)
        Pm = []
        for mc in range(2):
            Sp = pb_rot.next()
            for c in range(2):
                pe(lambda: T.matmul(Sp[:, :], lhsT=KxT[:, 2 * h + c, mc * 128:(mc + 1) * 128], rhs=qT[:, 2 * h + c, tsl], start=(c == 0), stop=(c == 1)),
                   r=[KxT, (qT, 2 * h + c)], w=[Sp])
            Pb = Px_rot.next()
            act(lambda: A.activation(out=Pb[:], in_=Sp[:, :], func=AF.Exp, scale=1.0 / 16.0), r=[Sp], w=[Pb])
            Pm.append(Pb)
        return Pm

    def xa_P(h, tc, Pm):
        tsl = slice(tc * 512, (tc + 1) * 512)
        pden = pb_rot.next()
        for mc in range(2):
            pe(lambda: T.matmul(pden[:, :], lhsT=ones_b[:], rhs=Pm[mc][:], start=(mc == 0), stop=(mc == 1)), r=[ones_b, Pm[mc]], w=[pden])
        rdx = rdx_rot.next()
        dve(lambda: V.reciprocal(out=rdx[:], in_=pden[:, :]), r=[pden], w=[rdx])
        for c in range(2):
            pov = pb_rot.next()
            for mc in range(2):
                pe(lambda: T.matmul(pov[:, :], lhsT=Vx[:, mc, (2 * h + c) * 128:(2 * h + c + 1) * 128], rhs=Pm[mc][:], start=(mc == 0), stop=(mc == 1)),
                   r=[Vx, Pm[mc]], w=[pov])
            dve(lambda: V.tensor_mul(out=oTx[:, 2 * h + c, tsl], in0=pov[:, :], in1=rdx[:]), r=[pov, rdx], w=[(oTx, tc)])

    units = [(h, tc) for h in range(4) for tc in range(4)]
    pm_next = xa_S(*units[0])
    for i, (h, tc) in enumerate(units):
        pm_cur = pm_next
        if i + 1 < len(units):
            pm_next = xa_S(*units[i + 1])
        xa_P(h, tc, pm_cur)
    wxo = sb(pe_, "wxo", [128, KC, D], BF16)
    load_w_cols(wxo, 0, dram["xa_wo"], 0, D)
    for J in range(NJ):
        for hf in range(2):
            po = pb_rot.next()
            for k in range(KC):
                pe(lambda: T.matmul(po[:, :], lhsT=oTx[:, k, J * 128:(J + 1) * 128], rhs=wxo[:, k, hf * 512:(hf + 1) * 512], start=(k == 0), stop=(k == KC - 1)),
                   r=[(oTx, J // 4), wxo], w=[po])
            dve(lambda: V.tensor_add(out=xr[:, J, hf * 512:(hf + 1) * 512], in0=po[:, :], in1=xr[:, J, hf * 512:(hf + 1) * 512]), r=[po, (xr, J)], w=[(xr, J)])
    pe_.close()
    if "x2" in dbg:
        for J in range(NJ):
            dump("x2", xr[:, J, :], [(xr, J)], dbg["x2"][J * 128:(J + 1) * 128, :])
        if "x3" not in dbg:
            final_wait()
            pes.close(); fw.close()
            return nc

    pf = Scope(arena)
    hmT = sb(pf, "hmT", [128, KC, NJ * 128], BF16)
    load_gain("norm_ffn")
    norm_loop([(xr[:, J, :], xr) for J in range(NJ)], hmT, lambda J: J // 4)
    wr = sb(pf, "wr", [128, KC, 20], BF16)
    load_w_cols(wr, 0, dram["router_w"], 0, 20)
    rb = sb(pf, "rb", [128, 20], F32)
    fw.dma("sp", rb[:], dram["router_b_rep"].partition_broadcast(128), writes=[rb])
    wts = sb(pf, "wts", [128, NJ, 16], F32)
    lg = sb(pf, "lg", [128, 20], F32)
    rt = sb(pf, "rt", [128, 64], F32)
    t44 = sb(pf, "t44", [128, 4, 4], F32)
    def route_tile(J):
        pr = pb_rot.next()
        for k in range(KC):
            pe(lambda: T.matmul(pr[:, 0:20], lhsT=hmT[:, k, J * 128:(J + 1) * 128], rhs=wr[:, k, :], start=(k == 0), stop=(k == KC - 1)), r=[(hmT, J // 4), wr], w=[pr])
        dve(lambda: V.tensor_add(out=lg[:], in0=pr[:, 0:20], in1=rb[:]), r=[pr, rb], w=[lg])
        gmx, ngm, gsum, gval = rt[:, 0:1], rt[:, 1:2], rt[:, 2:3], rt[:, 3:4]
        gm, eg, ein, mk1, e2, mk2, cw = rt[:, 4:8], rt[:, 8:12], rt[:, 12:16], rt[:, 16:20], rt[:, 20:24], rt[:, 24:28], rt[:, 28:32]
        m1, m2, dlt, wa, wb_ = rt[:, 32:33], rt[:, 33:34], rt[:, 34:35], rt[:, 35:36], rt[:, 36:37]
        dve(lambda: V.tensor_reduce(out=gmx, in_=lg[:, 0:4], axis=AX.X, op=ALU.max), r=[lg], w=[rt])
        dve(lambda: V.tensor_tensor(out=gm, in0=lg[:, 0:4], in1=gmx.to_broadcast([128, 4]), op=ALU.is_equal), r=[lg, rt], w=[rt])
        dve(lambda: V.tensor_scalar(out=ngm, in0=gmx, scalar1=-1.0, scalar2=None, op0=ALU.mult), r=[rt], w=[rt])
        act(lambda: A.activation(out=eg, in_=lg[:, 0:4], func=AF.Exp, bias=ngm, accum_out=gsum), r=[lg, rt], w=[rt])
        dve(lambda: V.reciprocal(out=gval, in_=gsum), r=[rt], w=[rt])
        dve(lambda: V.tensor_tensor(out=t44[:], in0=lg[:, 4:20].rearrange("p (g e) -> p g e", g=4), in1=gm.unsqueeze(2).to_broadcast([128, 4, 4]), op=ALU.mult), r=[lg, rt], w=[t44])
        dve(lambda: V.tensor_reduce(out=ein, in_=t44[:].rearrange("p g e -> p e g"), axis=AX.X, op=ALU.add), r=[t44], w=[rt])
        dve(lambda: V.tensor_reduce(out=m1, in_=ein, axis=AX.X, op=ALU.max), r=[rt], w=[rt])
        dve(lambda: V.tensor_tensor(out=mk1, in0=ein, in1=m1.to_broadcast([128, 4]), op=ALU.is_equal), r=[rt], w=[rt])
        dve(lambda: V.scalar_tensor_tensor(out=e2, in0=mk1, scalar=-1e9, in1=ein, op0=ALU.mult, op1=ALU.add), r=[rt], w=[rt])
        dve(lambda: V.tensor_reduce(out=m2, in_=e2, axis=AX.X, op=ALU.max), r=[rt], w=[rt])
        dve(lambda: V.tensor_tensor(out=mk2, in0=e2, in1=m2.to_broadcast([128, 4]), op=ALU.is_equal), r=[rt], w=[rt])
        dve(lambda: V.tensor_sub(out=dlt, in0=m2, in1=m1), r=[rt], w=[rt])
        act(lambda: A.activation(out=dlt, in_=dlt, func=AF.Exp), r=[rt], w=[rt])
        dve(lambda: V.tensor_scalar(out=dlt, in0=dlt, scalar1=1.0, scalar2=None, op0=ALU.add), r=[rt], w=[rt])
        dve(lambda: V.reciprocal(out=dlt, in_=dlt), r=[rt], w=[rt])
        dve(lambda: V.tensor_mul(out=wa, in0=dlt, in1=gval), r=[rt], w=[rt])
        dve(lambda: V.tensor_sub(out=wb_, in0=gval, in1=wa), r=[rt], w=[rt])
        dve(lambda: V.tensor_scalar(out=cw, in0=mk1, scalar1=wa, scalar2=None, op0=ALU.mult), r=[rt], w=[rt])
        dve(lambda: V.scalar_tensor_tensor(out=cw, in0=mk2, scalar=wb_, in1=cw, op0=ALU.mult, op1=ALU.add), r=[rt], w=[rt])
        dve(lambda: V.tensor_tensor(out=wts[:, J, :].rearrange("p (g e) -> p g e", g=4), in0=gm.unsqueeze(2).to_broadcast([128, 4, 4]),
                                    in1=cw.unsqueeze(1).to_broadcast([128, 4, 4]), op=ALU.mult), r=[rt], w=[wts])
    w1_rot = Rot([sb(pf, "w1e%d" % i, [128, KC, 512], BF16) for i in range(2)])
    w3_rot = Rot([sb(pf, "w3e%d" % i, [128, KC, 512], BF16) for i in range(2)])
    w2_rot = Rot([sb(pf, "w2e%d" % i, [128, 4, D], BF16) for i in range(2)])
    actT = sb(pf, "actT", [128, 4, NJ * 128], BF16)
    s1_rot = Rot([sb(pf, "s1_%d" % i, [128, 512], F32) for i in range(2)])
    for e in range(16):
        w1e, w3e, w2e = w1_rot.next(), w3_rot.next(), w2_rot.next()
        load_w_cols(w1e, 0, dram["moe_w1"][e], 0, 512)
        load_w_cols(w3e, 0, dram["moe_w3"][e], 0, 512)
        load_w_cols(w2e, 0, dram["moe_w2"][e], 0, D, kc=4)
        for hc in range(4):
            for tc in range(4):
                tsl = slice(tc * 512, (tc + 1) * 512)
                p1, p3 = pb_rot.next(), pb_rot.next()
                for k in range(KC):
                    pe(lambda: T.matmul(p1[:, :], lhsT=w1e[:, k, hc * 128:(hc + 1) * 128], rhs=hmT[:, k, tsl], start=(k == 0), stop=(k == KC - 1)), r=[w1e, (hmT, tc)], w=[p1])
                for k in range(KC):
                    pe(lambda: T.matmul(p3[:, :], lhsT=w3e[:, k, hc * 128:(hc + 1) * 128], rhs=hmT[:, k, tsl], start=(k == 0), stop=(k == KC - 1)), r=[w3e, (hmT, tc)], w=[p3])
                s1 = s1_rot.next()
                act(lambda: A.activation(out=s1[:], in_=p1[:, :], func=AF.Silu), r=[p1], w=[s1])
                dve(lambda: V.tensor_mul(out=actT[:, hc, tsl], in0=s1[:], in1=p3[:, :]), r=[s1, p3], w=[(actT, tc)])
                if e == 0:
                    route_tile(hc * 4 + tc)
        for J in range(NJ):
            for hf in range(2):
                py = pb_rot.next()
                for hc in range(4):
                    pe(lambda: T.matmul(py[:, :], lhsT=actT[:, hc, J * 128:(J + 1) * 128], rhs=w2e[:, hc, hf * 512:(hf + 1) * 512], start=(hc == 0), stop=(hc == 3)),
                       r=[(actT, J // 4), w2e], w=[py])
                dve(lambda: V.scalar_tensor_tensor(out=xr[:, J, hf * 512:(hf + 1) * 512], in0=py[:, :], scalar=wts[:, J, e:e + 1], in1=xr[:, J, hf * 512:(hf + 1) * 512],
                                                   op0=ALU.mult, op1=ALU.add), r=[py, wts, (xr, J)], w=[(xr, J)])
    pf.close()
    if "x3" in dbg:
        for J in range(NJ):
            dump("x3", xr[:, J, :], [(xr, J)], dbg["x3"][J * 128:(J + 1) * 128, :])

    load_gain("norm_final")
    yo_rot = Rot([sb(px, "yo%d" % i, [128, D], F32) for i in range(2)])
    for J in range(NJ):
        st = st_rot.next()
        act(lambda: A.activation(out=sq_junk[:], in_=xr[:, J, :], func=AF.Square, accum_out=st[:, 0:1]), r=[(xr, J)], w=[sq_junk, st])
        act(lambda: A.activation(out=st[:, 1:2], in_=st[:, 0:1], func=AF.Ln, scale=1.0 / D, bias=cst[:, 0:1]), r=[st, cst], w=[st])
        act(lambda: A.activation(out=st[:, 2:3], in_=st[:, 1:2], func=AF.Exp, scale=-0.5), r=[st], w=[st])
        yo = yo_rot.next()
        dve(lambda: V.scalar_tensor_tensor(out=yo[:], in0=xr[:, J, :], scalar=st[:, 2:3], in1=gb[:], op0=ALU.mult, op1=ALU.mult), r=[(xr, J), st, gb], w=[yo])
        fw.dma("pool", y_out[J * 128:(J + 1) * 128, :], yo[:], reads=[yo])
    final_wait()
    pes.close(); fw.close()
    return nc


_CACHE = {}


def make_in_maps(inputs):
    f = lambda a: np.ascontiguousarray(np.asarray(a, dtype=np.float32))
    x = f(inputs["x"]); mem = f(inputs["mem"])
    shared = {}
    for nm in ("norm_mix", "norm_xattn", "norm_mem", "norm_ffn"):
        shared[nm] = f(inputs[nm][0])
    shared["norm_final"] = f(inputs["norm_final"])
    shared["w_in"] = f(inputs["w_in"][0])
    shared["conv_qk"] = np.ascontiguousarray(f(inputs["conv_qk"][0]).reshape(4, 8, 128).transpose(2, 1, 0))
    shared["bi_rep"] = np.ascontiguousarray(np.tile(f(inputs["b_igate"][0]), 32))
    shared["bf_rep"] = np.ascontiguousarray(np.tile(f(inputs["b_fgate"][0]), 32))
    shared["mlstm_norm"] = f(inputs["mlstm_norm"][0])
    shared["cmp_pos_kT"] = np.ascontiguousarray(f(inputs["cmp_pos_k"][0]).T)
    shared["cmp_pos_vT"] = np.ascontiguousarray(f(inputs["cmp_pos_v"][0]).T)
    for nm in ("cmp_k_w1", "cmp_k_w2", "cmp_v_w1", "cmp_v_w2", "w_br_mlstm", "w_br_nsa", "w_mix_out",
               "xa_wq", "xa_wkv", "xa_wo", "moe_w1", "moe_w3", "moe_w2"):
        shared[nm] = f(inputs[nm][0])
    shared["router_w"] = np.ascontiguousarray(np.concatenate([f(inputs["router_group_w"][0]), f(inputs["router_expert_w"][0])], axis=1))
    shared["router_b_rep"] = np.ascontiguousarray(np.concatenate([f(inputs["router_group_b"][0]), f(inputs["router_expert_b"][0])]))
    hc = [host_consts(0), host_consts(1)]
    maps = []
    for core in range(8):
        b, p = core // 2, core % 2
        m = dict(shared)
        m["x_all"] = x[b]
        m["x_own"] = np.ascontiguousarray(x[b].reshape(NJ, 2, 128, D)[:, p].reshape(NJ * 128, D))
        m["mem"] = mem[b]
        m.update(hc[p])
        maps.append(m)
    return maps


def kernel(**inputs):
    if "nc" not in _CACHE:
        _CACHE["nc"] = build_nc()
    nc = _CACHE["nc"]
    maps = make_in_maps(inputs)
    res = run_bass_kernel_spmd(nc, maps, core_ids=list(range(8)))
    out = np.zeros((4, S, D), np.float32)
    for core in range(8):
        b, p = core // 2, core % 2
        y = np.asarray(res.results[core]["y"]).reshape(NJ, 128, D)
        out[b].reshape(NJ, 2, 128, D)[:, p] = y
    return out
```

```python
import numpy as np
import ml_dtypes
from contextlib import ExitStack
import concourse.bass as bass
import concourse.mybir as mybir
from concourse.bass_utils import run_bass_kernel_spmd

F32 = mybir.dt.float32
BF16 = mybir.dt.bfloat16
ALU = mybir.AluOpType
AF = mybir.ActivationFunctionType
AX = mybir.AxisListType
BF = ml_dtypes.bfloat16

D = 1024
S = 4096
NT = 32
NJ = 16
KC = 8
NEG = -30000.0
C_MLQ, C_MLK, C_MLV, C_MLO, C_MLI, C_MLF = 0, 512, 1024, 1536, 2048, 2052
C_NSQ, C_KCMP, C_VCMP, C_KSLC, C_VSLC, C_KWIN, C_VWIN, C_NSG, C_MRG = 2056, 2568, 2696, 2824, 2952, 3080, 3208, 3336, 3360
D_IN = 5408


class Buf:
    def __init__(self, t, name=""):
        self.t = t
        self.name = name
        self.w = None
        self.r = []
        self.parts = {}

    def __getitem__(self, idx):
        return self.t[idx]

    def _deps(self, key, is_write):
        ev = []
        if key is None:
            recs = [(self.w, self.r)] + [(p[0], p[1]) for p in self.parts.values()]
        else:
            p = self.parts.setdefault(key, [None, []])
            recs = [(self.w, self.r), (p[0], p[1])]
        for w, r in recs:
            if w is not None:
                ev.append(w)
            if is_write:
                ev.extend(r)
        return ev

    def _note(self, key, is_write, e):
        if key is None:
            if is_write:
                self.w = e
                self.r = []
                self.parts = {}
            else:
                self.r.append(e)
        else:
            p = self.parts.setdefault(key, [None, []])
            if is_write:
                p[0] = e
                p[1] = []
            else:
                p[1].append(e)


class BufView(Buf):
    def __init__(self, parent, ap):
        self.p = parent
        self.t = ap
        self.name = parent.name + "_v"

    def _deps(self, key, is_write):
        return self.p._deps(key, is_write)

    def _note(self, key, is_write, e):
        return self.p._note(key, is_write, e)


class FW:
    def __init__(self, nc, n_dma_sems=16):
        self.nc = nc
        self.es = ExitStack()
        self.engs = {}
        for name, h in (("pe", nc.tensor), ("act", nc.scalar), ("dve", nc.vector),
                        ("pool", nc.gpsimd), ("sp", nc.sync)):
            sem = self.es.enter_context(nc.semaphore("s_" + name))
            self.engs[name] = dict(h=h, sem=sem, cnt=0, seen={}, name=name)
        self.dma_sems = []
        self.dma_pools = {"hw": [], "sw": []}
        for kind, n in (("hw", n_dma_sems), ("sw", 8)):
            for i in range(n):
                sem = self.es.enter_context(nc.semaphore("s_dma_%s%d" % (kind, i)))
                d = dict(sem=sem, val=0)
                self.dma_sems.append(d)
                self.dma_pools[kind].append(d)
        self.dma_rr = {"hw": 0, "sw": 0}
        self.n_wait = 0
        self.n_ins = 0

    def _wait(self, eng, events):
        e = self.engs[eng]
        need = {}
        for (sem, val, src) in events:
            if src == eng and eng == "pe":
                continue
            k = sem.num
            if val > need.get(k, (None, 0))[1]:
                need[k] = (sem, val)
        for k, (sem, val) in need.items():
            if e["seen"].get(k, 0) >= val:
                continue
            e["h"].wait_ge(sem, val)
            e["seen"][k] = val
            self.n_wait += 1

    @staticmethod
    def _norm(lst):
        return [(b, None) if isinstance(b, Buf) else b for b in lst]

    def op(self, eng, fn, reads=(), writes=()):
        e = self.engs[eng]
        rs = self._norm(reads)
        ws = self._norm(writes)
        ev = []
        for b, k in rs:
            ev.extend(b._deps(k, False))
        for b, k in ws:
            ev.extend(b._deps(k, True))
        self._wait(eng, ev)
        ins = fn()
        e["cnt"] += 1
        ins.then_inc(e["sem"], 1)
        me = (e["sem"], e["cnt"], eng)
        for b, k in rs:
            b._note(k, False, me)
        for b, k in ws:
            b._note(k, True, me)
        self.n_ins += 1
        return ins

    def dma(self, eng, out, in_, reads=(), writes=(), **kw):
        e = self.engs[eng]
        rs = self._norm(reads)
        ws = self._norm(writes)
        ev = []
        for b, k in rs:
            ev.extend(b._deps(k, False))
        for b, k in ws:
            ev.extend(b._deps(k, True))
        kind = "sw" if eng == "pool" else "hw"
        pool_ = self.dma_pools[kind]
        d = pool_[self.dma_rr[kind]]
        self.dma_rr[kind] = (self.dma_rr[kind] + 1) % len(pool_)
        if d["val"] > 0:
            ev.append((d["sem"], d["val"], "dma"))
        self._wait(eng, ev)
        ins = e["h"].dma_start(out=out, in_=in_, **kw)
        d["val"] += 16
        ins.then_inc(d["sem"], 16)
        me = (d["sem"], d["val"], "dma")
        for b, k in rs:
            b._note(k, False, me)
        for b, k in ws:
            b._note(k, True, me)
        self.n_ins += 1
        return me

    def wait_all_dma(self, eng):
        for d in self.dma_sems:
            if d["val"]:
                self._wait(eng, [(d["sem"], d["val"], "dma")])

    def close(self):
        self.es.close()


class Rot:
    def __init__(self, items):
        self.items = items
        self.i = 0

    def next(self):
        x = self.items[self.i]
        self.i = (self.i + 1) % len(self.items)
        return x


class Arena:
    def __init__(self, nc, es, nbytes):
        self.t = es.enter_context(nc.sbuf_tensor("arena", [128, nbytes // 4], F32))
        self.free = [[0, nbytes]]
        self.pending = []
        self.peak = 0
        self.nbytes = nbytes

    def alloc(self, name, shape, dt, top=False):
        shape = list(shape)
        esz = 2 if dt == BF16 else 4
        nel = int(np.prod(shape[1:]))
        n = (nel * esz + 31) // 32 * 32
        off = None
        for fr in (reversed(self.free) if top else self.free):
            if fr[1] - fr[0] >= n:
                if top:
                    fr[1] -= n
                    off = fr[1]
                else:
                    off = fr[0]
                    fr[0] += n
                break
        if off is None:
            raise RuntimeError("arena full allocating %s (%d B); free=%s" % (name, n, self.free))
        self.free = [f for f in self.free if f[1] > f[0]]
        self.peak = max(self.peak, off + n)
        base = self.t[:, off // 4:(off + n) // 4]
        ap = base.bitcast(BF16)[:, 0:nel] if dt == BF16 else base[:, 0:nel]
        if len(shape) > 2:
            names = ["a%d" % i for i in range(len(shape) - 1)]
            kw = {nm: sz for nm, sz in zip(names, shape[1:])}
            ap = ap.rearrange("p (%s) -> p %s" % (" ".join(names), " ".join(names)), **kw)
        if shape[0] < 128:
            ap = ap[0:shape[0]]
        buf = Buf(ap, name)
        buf.region = (off, off + n)
        ev = []
        keep = []
        for (s0, e0, evs) in self.pending:
            if s0 < off + n and e0 > off:
                ev.extend(evs)
                if s0 >= off and e0 <= off + n:
                    continue
            keep.append((s0, e0, evs))
        self.pending = keep
        buf.r = ev
        return buf

    def release(self, buf):
        evs = []
        if buf.w is not None:
            evs.append(buf.w)
        evs.extend(buf.r)
        for p in buf.parts.values():
            if p[0] is not None:
                evs.append(p[0])
            evs.extend(p[1])
        best = {}
        for (sem, val, src) in evs:
            if val > best.get(sem.num, (None, 0, None))[1]:
                best[sem.num] = (sem, val, src)
        s0, e0 = buf.region
        self.pending.append((s0, e0, list(best.values())))
        self.free.append([s0, e0])
        self.free.sort()
        merged = []
        for f in self.free:
            if merged and merged[-1][1] == f[0]:
                merged[-1][1] = f[1]
            else:
                merged.append(list(f))
        self.free = merged


class Scope:
    def __init__(self, arena):
        self.arena = arena
        self.bufs = []

    def close(self):
        for b in reversed(self.bufs):
            self.arena.release(b)
        self.bufs = []


def host_consts(p):
    c = {}
    c["par"] = np.full((128, 1), float(p), np.float32)
    c["npar"] = np.full((128, 1), 1.0 - float(p), np.float32)
    s = np.arange(S)
    posk = np.stack([s % 128, np.ones(S), np.ones(S), (s // 128) * 128]).astype(np.float32)
    c["posk"] = posk.astype(BF)
    j = np.arange(256)
    c["poskc"] = np.stack([16.0 * j, np.ones(256), np.ones(256), np.full(256, 15.5)]).astype(BF)
    slopes = np.exp2(-8.0 * np.arange(1, 9) / 8.0)
    posq = np.zeros((2, 4, NJ, 4, 128), np.float32)
    tl = np.arange(128, dtype=np.float32)
    for g in range(2):
        for J in range(NJ):
            t0 = 128.0 * (2 * J + p)
            for hh in range(4):
                sl = slopes[g * 4 + hh]
                posq[g, 0, J, hh] = sl
                posq[g, 1, J, hh] = -sl * tl
                posq[g, 2, J, hh] = -sl * t0
                posq[g, 3, J, hh] = sl
    c["posq"] = posq.reshape(2, 4, NJ * 512).astype(BF)
    c["eall"] = (np.arange(64)[:, None] == (s[None, :] // 64)).astype(np.float32).astype(BF)
    sl_ = np.arange(128)[:, None]
    tl_ = np.arange(128)[None, :]
    wm = np.zeros((128, 6, 128), np.float32)
    for r in range(6):
        dw = (4 + p - r) * 128 + tl_ - sl_
        wm[:, r, :] = np.where((dw >= 0) & (dw < 512), 0.0, NEG)
    c["wmask"] = wm.astype(BF)
    cm = np.zeros((128, NJ, 2, 128), np.float32)
    for J in range(NJ):
        t = 128 * (2 * J + p) + tl_
        for ch in range(2):
            jj = ch * 128 + sl_
            valid = (16 * jj + 31 <= t) & (jj < 255)
            cm[:, J, ch, :] = np.where(valid, 0.0, NEG)
    c["cmask"] = cm.astype(BF)
    cs = (np.arange(256) * 16)[:, None]
    ss = (np.arange(64) * 64)[None, :]
    ov = np.clip(np.minimum(cs + 32, ss + 64) - np.maximum(cs, ss), 0, None).astype(np.float32) / 32.0
    ov[255] = 0.0
    c["ov"] = ov.reshape(2, 128, 64).transpose(1, 0, 2).copy().astype(BF)
    selm = np.zeros((128, NJ, 64), np.float32)
    selc = np.zeros((128, NJ, 64), np.float32)
    blk = np.arange(64)
    for J in range(NJ):
        t = 128 * (2 * J + p) + np.arange(128)
        cur = t // 64
        for i in range(128):
            for bI in range(64):
                if bI == 0 or bI == cur[i] or bI == cur[i] - 1:
                    selc[i, J, bI] = 1e4 + bI
                elif bI > cur[i]:
                    selc[i, J, bI] = -1e4 - bI
                else:
                    selm[i, J, bI] = 1.0
                    selc[i, J, bI] = -1e-30 * bI
    c["selm"] = selm
    c["selc"] = selc
    mm = np.zeros((128, 2, 128), np.float32)
    caus = (sl_ <= tl_).astype(np.float32)
    if p == 0:
        mm[:, 0, :] = caus
    else:
        mm[:, 0, :] = 1.0
        mm[:, 1, :] = caus
    c["mlmask"] = mm.astype(BF)
    return c


def build_nc(debug=None):
    try:
        return _build_nc(debug)
    except _StopBuild as e:
        return e.args[0]


class _StopBuild(Exception):
    pass


def _build_nc(debug=None):
    nc = bass.Bass("TRN2", target_bir_lowering=False)
    V, A, G, T = nc.vector, nc.scalar, nc.gpsimd, nc.tensor
    dram = {}

    def din(name, shape, dt=F32):
        dram[name] = nc.dram_tensor(name, list(shape), dt, kind="ExternalInput").ap()
        return dram[name]

    x_all = din("x_all", [S, D])
    x_own = din("x_own", [NJ * 128, D])
    mem = din("mem", [256, D])
    for nm in ("norm_mix", "norm_xattn", "norm_mem", "norm_ffn", "norm_final"):
        din(nm, [D])
    w_in = din("w_in", [D, D_IN])
    conv_qk = din("conv_qk", [128, 8, 4])
    din("bi_rep", [128]); din("bf_rep", [128])
    din("mlstm_norm", [512])
    din("cmp_pos_kT", [64, 32]); din("cmp_pos_vT", [64, 32])
    din("cmp_k_w1", [2048, 128]); din("cmp_k_w2", [128, 64])
    din("cmp_v_w1", [2048, 128]); din("cmp_v_w2", [128, 64])
    din("w_br_mlstm", [512, D]); din("w_br_nsa", [512, D]); din("w_mix_out", [D, D])
    din("xa_wq", [D, D]); din("xa_wkv", [D, 2 * D]); din("xa_wo", [D, D])
    din("router_w", [D, 20]); din("router_b_rep", [20])
    din("moe_w1", [16, D, 512]); din("moe_w3", [16, D, 512]); din("moe_w2", [16, 512, D])
    din("par", [128, 1]); din("npar", [128, 1])
    din("posk", [4, S], BF16); din("poskc", [4, 256], BF16); din("posq", [2, 4, NJ * 512], BF16)
    din("eall", [64, S], BF16); din("wmask", [128, 6, 128], BF16); din("cmask", [128, NJ, 2, 128], BF16)
    din("ov", [128, 2, 64], BF16); din("selm", [128, NJ, 64]); din("selc", [128, NJ, 64])
    din("mlmask", [128, 2, 128], BF16)
    y_out = nc.dram_tensor("y", [NJ * 128, D], F32, kind="ExternalOutput").ap()
    dbg = {}
    if debug:
        for nm, shp in debug.items():
            dbg[nm] = nc.dram_tensor("dbg_" + nm, list(shp), F32, kind="ExternalOutput").ap()

    fw = FW(nc)
    pes = ExitStack()
    arena = Arena(nc, pes, 206 * 1024)
    gs = Scope(arena)

    def sb(sc, name, shape, dt, top=False):
        b = arena.alloc(name, shape, dt, top=top)
        sc.bufs.append(b)
        return b

    def pst(es, name, shape, dt):
        return Buf(pes.enter_context(nc.psum_tensor("ps_" + name, list(shape), dt)), name)

    def dve(fn, r=(), w=()):
        return fw.op("dve", fn, reads=r, writes=w)

    def act(fn, r=(), w=()):
        return fw.op("act", fn, reads=r, writes=w)

    def pool(fn, r=(), w=()):
        return fw.op("pool", fn, reads=r, writes=w)

    def pe(fn, r=(), w=()):
        return fw.op("pe", fn, reads=r, writes=w)

    pbs = [pst(gs, "pb%d" % i, [128, 512], F32) for i in range(8)]
    ptbs = [BufView(pbs[6], pbs[6][:, :].bitcast(BF16)), BufView(pbs[7], pbs[7][:, :].bitcast(BF16))]
    pb_rot = Rot(pbs[0:6])
    ptb_rot = Rot(ptbs)

    ident_b = sb(gs, "ident_b", [128, 128], BF16)
    ident_f = sb(gs, "ident_f", [128, 128], F32)
    ones_f = sb(gs, "ones_f", [128, 128], F32)
    tri_f = sb(gs, "tri_f", [128, 128], F32)
    cst = sb(gs, "cst", [128, 4], F32)
    par = sb(gs, "par", [128, 1], F32)
    npar = sb(gs, "npar", [128, 1], F32)
    pool(lambda: G.memset(ident_f[:], 0.0), w=[ident_f])
    pool(lambda: G.affine_select(out=ident_f[:], in_=ident_f[:], pattern=[[-1, 128]], compare_op=ALU.not_equal,
                                 fill=1.0, base=0, channel_multiplier=1), r=[ident_f], w=[ident_f])
    pool(lambda: G.tensor_copy(out=ident_b[:], in_=ident_f[:]), r=[ident_f], w=[ident_b])
    pool(lambda: G.memset(ones_f[:], 1.0), w=[ones_f])
    pool(lambda: G.memset(tri_f[:], 1.0), w=[tri_f])
    pool(lambda: G.affine_select(out=tri_f[:], in_=tri_f[:], pattern=[[1, 128]], compare_op=ALU.is_ge,
                                 fill=0.0, base=0, channel_multiplier=-1), r=[tri_f], w=[tri_f])
    pool(lambda: G.memset(cst[:, 0:1], 1e-6), w=[cst])
    pool(lambda: G.memset(cst[:, 1:2], 1.0), w=[cst])
    pool(lambda: G.memset(cst[:, 2:3], 0.0), w=[cst])
    fw.dma("sp", par[:], dram["par"], writes=[par])
    fw.dma("sp", npar[:], dram["npar"], writes=[npar])

    gb = sb(gs, "gb", [128, D], F32)
    nsc = Scope(arena)
    xs_rot = Rot([sb(nsc, "xs%d" % i, [128, D], F32) for i in range(3)])
    xn_rot = Rot([sb(nsc, "xn%d" % i, [128, D], BF16) for i in range(3)])
    sq_junk = sb(gs, "sq_junk", [128, D], BF16)
    st_rot = Rot([sb(gs, "st%d" % i, [128, 4], F32) for i in range(6)])
    wst_rot = Rot([sb(gs, "wst%d" % i, [128, 1024], F32) for i in range(2)])
    cast_rot = Rot(["act"])

    def blend(eng, out_ap, even_ap, odd_ap, tmp_ap, r, w):
        h = {"dve": V, "pool": G}[eng]
        r = list(r)
        tmpbuf = r.pop()
        fw.op(eng, lambda: h.tensor_scalar(out=tmp_ap, in0=even_ap, scalar1=npar[:, 0:1], scalar2=None, op0=ALU.mult),
              reads=r + [npar], writes=[tmpbuf])
        if eng == "dve":
            fw.op(eng, lambda: h.scalar_tensor_tensor(out=out_ap, in0=odd_ap, scalar=par[:, 0:1], in1=tmp_ap,
                                                      op0=ALU.mult, op1=ALU.add), reads=r + [par, tmpbuf], writes=list(w))
        else:
            fw.op(eng, lambda: h.tensor_scalar(out=out_ap, in0=odd_ap, scalar1=par[:, 0:1], scalar2=None, op0=ALU.mult),
                  reads=r + [par], writes=list(w))
            fw.op(eng, lambda: h.tensor_tensor(out=out_ap, in0=out_ap, in1=tmp_ap, op=ALU.add), reads=[tmpbuf] + list(w), writes=list(w))

    def load_gain(name):
        fw.dma("sp", gb[:], dram[name].partition_broadcast(128), writes=[gb])

    def load_w(dst_ap, src_ap, n_free, dst_bufs, eng=None):
        wst = wst_rot.next()
        stv = wst[:, 0:n_free]
        if len(src_ap.shape) == 3:
            stv = stv.rearrange("p (a n) -> p a n", a=src_ap.shape[1])
        fw.dma("sp", stv, src_ap, writes=[wst])
        e = eng or cast_rot.next()
        if e == "act":
            act(lambda: A.copy(out=dst_ap, in_=stv), r=[wst], w=dst_bufs)
        elif e == "pool":
            pool(lambda: G.tensor_copy(out=dst_ap, in_=stv), r=[wst], w=dst_bufs)
        else:
            dve(lambda: V.tensor_copy(out=dst_ap, in_=stv), r=[wst], w=dst_bufs)

    def load_w_cols(dst, col_dst0, src2d, col0, ncols, kc=KC, key=None):
        step = max(1, 1024 // (kc * 1)) if False else None
        per = max(1, 1024 // kc)
        c = 0
        while c < ncols:
            n = min(per, ncols - c)
            src = src2d[:, col0 + c: col0 + c + n].rearrange("(k p) n -> p k n", p=128)
            load_w(dst[:, :, col_dst0 + c: col_dst0 + c + n], src, kc * n, [(dst, key)] if key is not None else [dst])
            c += n

    def load_x(src_tile_ap):
        xs_buf = xs_rot.next()
        fw.dma("pool", xs_buf[:], src_tile_ap, writes=[xs_buf])
        return (xs_buf[:], xs_buf)

    def rmsnorm_front(src_tile_ap):
        if isinstance(src_tile_ap, tuple):
            xs_ap, xs_buf = src_tile_ap
        else:
            xs_ap, xs_buf = load_x(src_tile_ap)
        st = st_rot.next()
        act(lambda: A.activation(out=sq_junk[:], in_=xs_ap, func=AF.Square, accum_out=st[:, 0:1]), r=[xs_buf], w=[sq_junk, st])
        act(lambda: A.activation(out=st[:, 1:2], in_=st[:, 0:1], func=AF.Ln, scale=1.0 / D, bias=cst[:, 0:1]), r=[st, cst], w=[st])
        act(lambda: A.activation(out=st[:, 2:3], in_=st[:, 1:2], func=AF.Exp, scale=-0.5), r=[st], w=[st])
        xn = xn_rot.next()
        dve(lambda: V.scalar_tensor_tensor(out=xn[:], in0=xs_ap, scalar=st[:, 2:3], in1=gb[:], op0=ALU.mult, op1=ALU.mult),
            r=[xs_buf, st, gb], w=[xn])
        return xn

    def rmsnorm_back(xn, dstT, col0, key=None, evac_eng="act"):
        ptb = ptb_rot.next()
        for k in range(KC):
            pe(lambda: T.transpose(out=ptb[:, k * 128:(k + 1) * 128], in_=xn[:, k * 128:(k + 1) * 128], identity=ident_b[:]),
               r=[xn, ident_b], w=[ptb])
        dst_ap = dstT[:, :, col0:col0 + 128]
        src_ap = ptb[:].rearrange("p (k t) -> p k t", k=KC)
        wl = [(dstT, key)] if key is not None else [dstT]
        if evac_eng == "act":
            act(lambda: A.copy(out=dst_ap, in_=src_ap), r=[ptb], w=wl)
        else:
            dve(lambda: V.tensor_copy(out=dst_ap, in_=src_ap), r=[ptb], w=wl)

    def rmsnorm_T(src_tile_ap, dstT, col0, key=None, evac_eng="act"):
        xn = rmsnorm_front(src_tile_ap)
        rmsnorm_back(xn, dstT, col0, key=key, evac_eng=evac_eng)

    def norm_loop(srcs, dstT, keyfn, la=2):
        q_ = []
        n = len(srcs)
        for t in range(n + la):
            if t < n:
                q_.append(rmsnorm_front(srcs[t]))
            if t >= la:
                tt = t - la
                rmsnorm_back(q_[tt], dstT, tt * 128, key=keyfn(tt), evac_eng="dve")

    def final_wait():
        fw.wait_all_dma("sp")


    def maybe_stop(tag):
        if tag in dbg:
            fw.dma("sp", dbg[tag], cst[:], reads=[cst])
            final_wait()
            pes.close(); fw.close()
            raise _StopBuild(nc)

    def dump(name, src_ap, bufs, dst_ap=None):
        if name in dbg:
            fw.dma("sp", dst_ap if dst_ap is not None else dbg[name], src_ap, reads=bufs)

    ps1 = Scope(arena)
    uT_own = sb(ps1, "uT_own", [128, 4, NJ * 128], BF16, top=True)

    pa = Scope(arena)
    hT_all = sb(pa, "hT_all", [128, KC, S], BF16)
    load_gain("norm_mix")
    xn_q = []
    for t in range(NT + 2):
        if t < NT:
            xn_q.append(rmsnorm_front(x_all[t * 128:(t + 1) * 128, :]))
        if t >= 2:
            tt = t - 2
            rmsnorm_back(xn_q[tt], hT_all, tt * 128, key=tt // 4, evac_eng="dve")

    nsc.close()
    pm = Scope(arena)
    wg = sb(pm, "wg", [128, KC, 8], BF16)
    load_w_cols(wg, 0, w_in, C_MLI, 8)
    gp = pb_rot.next()
    for t in range(NT):
        for k in range(KC):
            pe(lambda: T.matmul(gp[:, t * 8:(t + 1) * 8], lhsT=hT_all[:, k, t * 128:(t + 1) * 128], rhs=wg[:, k, :],
                                start=(k == 0), stop=(k == KC - 1)), r=[(hT_all, t // 4), wg], w=[gp])
    NG = 14
    pgt = Scope(arena)
    gt = [sb(pgt, "gt%d" % i, [128, 128], F32) for i in range(NG)]
    bi_b, bf_b = gt[0], gt[1]
    fw.dma("sp", bi_b[:], dram["bi_rep"].partition_broadcast(128), writes=[bi_b])
    fw.dma("sp", bf_b[:], dram["bf_rep"].partition_broadcast(128), writes=[bf_b])
    gpv = gp[:, 0:256].rearrange("p (t c) -> p t c", c=8)
    ipre, fz, t_a, t_b = gt[2], gt[3], gt[4], gt[5]
    v3 = lambda b_: b_[:].rearrange("p (t h) -> p t h", h=4)
    dve(lambda: V.tensor_tensor(out=v3(ipre), in0=gpv[:, :, 0:4], in1=v3(bi_b), op=ALU.add), r=[gp, bi_b], w=[ipre])
    dve(lambda: V.tensor_tensor(out=v3(fz), in0=gpv[:, :, 4:8], in1=v3(bf_b), op=ALU.add), r=[gp, bf_b], w=[fz])
    dve(lambda: V.tensor_scalar(out=t_a[:], in0=fz[:], scalar1=-1.0, scalar2=None, op0=ALU.mult), r=[fz], w=[t_a])
    dve(lambda: V.tensor_tensor(out=t_a[:], in0=t_a[:], in1=fz[:], op=ALU.max), r=[fz, t_a], w=[t_a])
    act(lambda: A.activation(out=t_a[:], in_=t_a[:], func=AF.Exp, scale=-1.0), r=[t_a], w=[t_a])
    act(lambda: A.activation(out=t_a[:], in_=t_a[:], func=AF.Ln, bias=cst[:, 1:2]), r=[t_a, cst], w=[t_a])
    dve(lambda: V.tensor_scalar_min(out=t_b[:], in0=fz[:], scalar1=0.0), r=[fz], w=[t_b])
    logf = gt[6]
    dve(lambda: V.tensor_sub(out=logf[:], in0=t_b[:], in1=t_a[:]), r=[t_a, t_b], w=[logf])
    pc = pb_rot.next()
    pe(lambda: T.matmul(pc[:, 0:128], lhsT=tri_f[:], rhs=logf[:], start=True, stop=True), r=[tri_f, logf], w=[pc])
    pe(lambda: T.matmul(pc[:, 128:256], lhsT=ones_f[:], rhs=logf[:], start=True, stop=True), r=[ones_f, logf], w=[pc])
    blocal, tot = gt[7], gt[8]
    dve(lambda: V.tensor_copy(out=blocal[:], in_=pc[:, 0:128]), r=[pc], w=[blocal])
    dve(lambda: V.tensor_copy(out=tot[:], in_=pc[:, 128:256]), r=[pc], w=[tot])

    def scan(src, op, tmp1, tmp2):
        cur = src
        bufs = [tmp1, tmp2]
        i = 0
        for d in (1, 2, 4, 8, 16):
            nxt = bufs[i % 2]
            i += 1
            dve(lambda: V.tensor_copy(out=nxt[:, 0:4 * d], in_=cur[:, 0:4 * d]), r=[cur], w=[nxt])
            dve(lambda: V.tensor_tensor(out=nxt[:, 4 * d:128], in0=cur[:, 4 * d:128], in1=cur[:, 0:128 - 4 * d], op=op), r=[cur], w=[nxt])
            cur = nxt
        return cur

    incl = scan(tot, ALU.add, gt[9], gt[10])
    Bg = gt[11]
    dve(lambda: V.tensor_sub(out=Bg[:], in0=incl[:], in1=tot[:]), r=[incl, tot], w=[Bg])
    dve(lambda: V.tensor_add(out=Bg[:], in0=Bg[:], in1=blocal[:]), r=[Bg, blocal], w=[Bg])
    a_t = gt[12]
    dve(lambda: V.tensor_sub(out=a_t[:], in0=ipre[:], in1=Bg[:]), r=[ipre, Bg], w=[a_t])
    pe(lambda: T.transpose(out=pc[:, 256:384], in_=a_t[:], identity=ident_f[:]), r=[a_t, ident_f], w=[pc])
    amax = gt[13]
    dve(lambda: V.tensor_reduce(out=amax[:, 0:1], in_=pc[:, 256:384], axis=AX.X, op=ALU.max), r=[pc], w=[amax])
    dgm = gt[9]
    dve(lambda: V.tensor_scalar(out=dgm[:], in0=ident_f[:], scalar1=amax[:, 0:1], scalar2=None, op0=ALU.mult), r=[ident_f, amax, incl], w=[dgm])
    pe(lambda: T.matmul(pc[:, 384:512], lhsT=ones_f[:], rhs=dgm[:], start=True, stop=True), r=[ones_f, dgm], w=[pc])
    amb = gt[10]
    dve(lambda: V.tensor_scalar_max(out=amb[:], in0=pc[:, 384:512], scalar1=0.0), r=[pc], w=[amb])
    ainc = scan(amb, ALU.max, gt[3], gt[4])
    ref = gt[5]
    v4 = lambda b_: b_[:].rearrange("p (j r h) -> p j r h", r=2, h=4)
    dve(lambda: V.memset(ref[:], 0.0), w=[ref])
    for r_ in range(2):
        dve(lambda: V.tensor_copy(out=v4(ref)[:, 1:16, r_, :], in_=v4(ainc)[:, 0:15, 1, :]), r=[ainc], w=[ref])
    decay = sb(pm, "decay", [128, NJ, 4], F32)
    dve(lambda: V.tensor_sub(out=decay[:], in0=v4(ref)[:, :, 0, :], in1=v4(ainc)[:, :, 1, :]), r=[ref, ainc], w=[decay])
    act(lambda: A.activation(out=decay[:], in_=decay[:], func=AF.Exp), r=[decay], w=[decay])
    ws = sb(pm, "ws", [128, NT, 4], F32)
    thr = sb(pm, "thr", [128, NT, 4], F32)
    wsf = ws[:].rearrange("p t h -> p (t h)")
    thf = thr[:].rearrange("p t h -> p (t h)")
    dve(lambda: V.tensor_sub(out=wsf, in0=a_t[:], in1=ref[:]), r=[a_t, ref], w=[ws])
    act(lambda: A.activation(out=wsf, in_=wsf, func=AF.Exp), r=[ws], w=[ws])
    dve(lambda: V.tensor_scalar(out=wsf, in0=wsf, scalar1=128.0 ** -0.5, scalar2=None, op0=ALU.mult), r=[ws], w=[ws])
    dve(lambda: V.tensor_add(out=thf, in0=Bg[:], in1=ref[:]), r=[Bg, ref], w=[thr])
    act(lambda: A.activation(out=thf, in_=thf, func=AF.Exp, scale=-1.0), r=[thr], w=[thr])
    thr_own = sb(pm, "thr_own", [128, NJ, 4], F32)
    thr4 = thr[:].rearrange("p (j r) h -> p j r h", r=2)
    blend("dve", thr_own[:], thr4[:, :, 0, :], thr4[:, :, 1, :], gt[6][:, 0:64].rearrange("p (j h) -> p j h", h=4), [thr, gt[6]], [thr_own])

    pgt.close()
    og = sb(pm, "og", [128, NJ, 512], BF16)
    pog = Scope(arena)
    wo_b = sb(pog, "wo_b", [128, KC, 512], BF16)
    load_w_cols(wo_b, 0, w_in, C_MLO, 512)
    gain_ml = sb(pog, "gain_ml", [128, 512], F32)
    fw.dma("sp", gain_ml[:], dram["mlstm_norm"].partition_broadcast(128), writes=[gain_ml])
    hown_rot = Rot([sb(pog, "hown%d" % i, [128, KC, 128], BF16) for i in range(2)])
    htmp = sb(pog, "htmp", [128, KC, 128], BF16)
    sig_t = sb(pog, "sig_t", [128, 512], F32)
    def og_blend(J):
        ho = hown_rot.next()
        blend("dve", ho[:], hT_all[:, :, (2 * J) * 128:(2 * J + 1) * 128], hT_all[:, :, (2 * J + 1) * 128:(2 * J + 2) * 128], htmp[:],
              [(hT_all, (2 * J) // 4), htmp], [ho])
        return ho
    ho_next = og_blend(0)
    for J in range(NJ):
        ho = ho_next
        po = pb_rot.next()
        for k in range(KC):
            pe(lambda: T.matmul(po[:, :], lhsT=ho[:, k, :], rhs=wo_b[:, k, :], start=(k == 0), stop=(k == KC - 1)), r=[ho, wo_b], w=[po])
        if J + 1 < NJ:
            ho_next = og_blend(J + 1)
        act(lambda: A.activation(out=sig_t[:], in_=po[:], func=AF.Sigmoid), r=[po], w=[sig_t])
        dve(lambda: V.tensor_mul(out=og[:, J, :], in0=sig_t[:], in1=gain_ml[:]), r=[sig_t, gain_ml], w=[(og, J)])
    pog.close()
    convw = sb(pm, "convw", [128, KC, 4], F32)
    fw.dma("sp", convw[:], conv_qk, writes=[convw])
    mlmask = sb(pm, "mlmask", [128, 2, 128], BF16)
    fw.dma("sp", mlmask[:], dram["mlmask"], writes=[mlmask])
    wq_rot = Rot([sb(pm, "wq%d" % i, [128, KC, 128], BF16) for i in range(2)])
    wk_rot = Rot([sb(pm, "wk%d" % i, [128, KC, 128], BF16) for i in range(2)])
    wv_rot = Rot([sb(pm, "wv%d" % i, [128, KC, 128], BF16) for i in range(2)])
    pre_q = sb(pm, "pre_q", [128, 3 + 512], F32)
    pre_k = sb(pm, "pre_k", [128, 3 + 512], F32)
    acc_rot = Rot([sb(pm, "acc%d" % i, [128, 512], F32) for i in range(2)])
    qsil = sb(pm, "qsil", [128, 512], BF16)
    qtmp = sb(pm, "qtmp", [128, 2, 128], BF16)
    hsets = [dict(QT_own=sb(pm, "QT_own%d" % i, [128, NJ, 128], BF16), KT=sb(pm, "KT%d" % i, [128, S], BF16),
                  Ktok=sb(pm, "Ktok%d" % i, [128, NT, 128], BF16), Vp=sb(pm, "Vp%d" % i, [128, NT, 129], BF16)) for i in range(2)]
    Cst = sb(pm, "Cst", [128, 129], F32)
    Cbf_rot = Rot([sb(pm, "Cbf%d" % i, [128, 129], BF16) for i in range(2)])
    stm_rot = Rot([sb(pm, "stm%d" % i, [128, 2, 128], BF16) for i in range(2)])
    hc_t = sb(pm, "hc_t", [128, 128], F32)
    sm_rot = Rot([sb(pm, "sm%d" % i, [128, 8], F32) for i in range(2)])
    u_rot = Rot([sb(pm, "u%d" % i, [128, 128], BF16) for i in range(2)])
    hc_junk = sb(pm, "hc_junk", [128, 128], BF16)

    def head_items(h):
        hs = hsets[h % 2]
        wq, wk, wv = wq_rot.next(), wk_rot.next(), wv_rot.next()
        items = []

        def ld():
            load_w_cols(wq, 0, w_in, C_MLQ + h * 128, 128)
            load_w_cols(wk, 0, w_in, C_MLK + h * 128, 128)
            load_w_cols(wv, 0, w_in, C_MLV + h * 128, 128)

        def qk_chunk(which, c):
            wsel = wq if which == "q" else wk
            cch = h if which == "q" else 4 + h
            pre = pre_q if which == "q" else pre_k
            st_ = {}

            def A_():
                ps = pb_rot.next()
                for k in range(KC):
                    pe(lambda: T.matmul(ps[:, :], lhsT=wsel[:, k, :], rhs=hT_all[:, k, c * 512:(c + 1) * 512], start=(k == 0), stop=(k == KC - 1)),
                       r=[wsel, (hT_all, c)], w=[ps])
                if c == 0:
                    dve(lambda: V.memset(pre[:, 0:3], 0.0), w=[pre])
                else:
                    dve(lambda: V.tensor_copy(out=pre[:, 0:3], in_=pre[:, 512:515]), r=[pre], w=[pre])
                act(lambda: A.copy(out=pre[:, 3:515], in_=ps[:, :]), r=[ps], w=[pre])
                acc = acc_rot.next()
                st_["acc"] = acc
                act(lambda: A.activation(out=acc[:], in_=ps[:, :], func=AF.Copy, scale=convw[:, cch, 3:4]), r=[ps, convw], w=[acc])

            def B_():
                acc = st_["acc"]
                for j in range(3):
                    dve(lambda: V.scalar_tensor_tensor(out=acc[:], in0=pre[:, j:j + 512], scalar=convw[:, cch, j:j + 1], in1=acc[:],
                                                       op0=ALU.mult, op1=ALU.add), r=[pre, convw, acc], w=[acc])
                if which == "q":
                    qs = qsil_rot.next()
                    st_["qs"] = qs
                    act(lambda: A.activation(out=qs[:], in_=acc[:], func=AF.Silu), r=[acc], w=[qs])
                else:
                    act(lambda: A.activation(out=hs["KT"][:, c * 512:(c + 1) * 512], in_=acc[:], func=AF.Silu), r=[acc], w=[(hs["KT"], c)])

            def C_():
                qs = st_["qs"]
                q4 = qs[:].rearrange("p (j r t) -> p j r t", r=2, t=128)
                blend("dve", hs["QT_own"][:, 2 * c:2 * c + 2, :], q4[:, :, 0, :], q4[:, :, 1, :], qtmp[:], [qs, qtmp], [(hs["QT_own"], c)])
            return [A_, B_, C_] if which == "q" else [A_, B_]

        def ktok_group(c):
            st_ = {}

            def A_():
                ptb = ptb_rot.next()
                st_["ptb"] = ptb
                for i in range(8):
                    t = c * 8 + i
                    pe(lambda: T.transpose(out=ptb[:, i * 128:(i + 1) * 128], in_=hs["KT"][:, t * 128:(t + 1) * 128], identity=ident_b[:]),
                       r=[(hs["KT"], t // 4), ident_b], w=[ptb])

            def B_():
                ptb = st_["ptb"]
                dve(lambda: V.tensor_copy(out=hs["Ktok"][:, c * 8:(c + 1) * 8, :], in_=ptb[:].rearrange("p (i d) -> p i d", i=8)), r=[ptb], w=[(hs["Ktok"], c)])
            return [A_, B_]

        def v_group(c):
            st_ = {}

            def A_():
                ps = pb_rot.next()
                st_["ps"] = ps
                for i in range(4):
                    t = c * 4 + i
                    for k in range(KC):
                        pe(lambda: T.matmul(ps[:, i * 128:(i + 1) * 128], lhsT=hT_all[:, k, t * 128:(t + 1) * 128], rhs=wv[:, k, :],
                                            start=(k == 0), stop=(k == KC - 1)), r=[(hT_all, c), wv], w=[ps])

            def B_():
                ps = st_["ps"]
                wsv = ws[:, c * 4:(c + 1) * 4, h:h + 1]
                dve(lambda: V.tensor_tensor(out=hs["Vp"][:, c * 4:(c + 1) * 4, 0:128], in0=ps[:].rearrange("p (i d) -> p i d", i=4),
                                            in1=wsv.to_broadcast([128, 4, 128]), op=ALU.mult), r=[ps, ws], w=[(hs["Vp"], c)])
                dve(lambda: V.tensor_copy(out=hs["Vp"][:, c * 4:(c + 1) * 4, 128:129], in_=ws[:, c * 4:(c + 1) * 4, h:h + 1]), r=[ws], w=[(hs["Vp"], c)])
            return [A_, B_]

        items.append([ld])
        late = None
        for c in range(8):
            items.append(qk_chunk("q", c))
            items.append(qk_chunk("k", c))
            items.append(v_group(c))
            if late is not None and c % 2 == 0:
                items.append(ktok_group(late))
                late = None
            if c % 2 == 1:
                late = c // 2
        items.append([lambda: None])
        items.append(ktok_group(late))
        return items

    class ItemSched:
        def __init__(self):
            self.queue = []
            self.inflight = []

        def add(self, items):
            self.queue += items

        def step(self, n_new):
            nxt = []
            for it in self.inflight:
                it.pop(0)()
                if it:
                    nxt.append(it)
            self.inflight = nxt
            for _ in range(n_new):
                if self.queue:
                    it = list(self.queue.pop(0))
                    it.pop(0)()
                    if it:
                        self.inflight.append(it)

        def drain(self):
            while self.queue or self.inflight:
                self.step(2)

    qsil_rot = Rot([qsil, sb(pm, "qsil2", [128, 512], BF16)])
    sched = ItemSched()
    u_pending = []
    u_pending_dve = []
    po_rot = Rot([pbs[2], pbs[3]])
    pb_rot = Rot(pbs[4:6])
    sched.add(head_items(0))
    for h in range(4):
        sched.drain()
        if h + 1 < 4:
            sched.add(head_items(h + 1))
        hs = hsets[h % 2]
        QT_own, KT, Ktok, Vp = hs["QT_own"], hs["KT"], hs["Ktok"], hs["Vp"]
        pool(lambda: G.memset(Cst[:], 0.0), w=[Cst])
        Cbf = Cbf_rot.next()
        pool(lambda: G.memset(Cbf[:], 0.0), w=[Cbf])
        for J in range(NJ):
            pst_ = pbs[0]
            for r_ in range(2):
                t = 2 * J + r_
                pe(lambda: T.matmul(pst_[:, r_ * 128:(r_ + 1) * 128], lhsT=KT[:, t * 128:(t + 1) * 128], rhs=QT_own[:, J, :], start=True, stop=True),
                   r=[(KT, t // 4), (QT_own, J // 2)], w=[pst_])
            stm = stm_rot.next()
            dve(lambda: V.tensor_tensor(out=stm[:], in0=pst_[:, 0:256].rearrange("p (r t) -> p r t", r=2), in1=mlmask[:], op=ALU.mult),
                r=[pst_, mlmask], w=[stm])
            Cbf_next = None
            if J < NJ - 1:
                pcs = pbs[1]
                for r_ in range(2):
                    t = 2 * J + r_
                    pe(lambda: T.matmul(pcs[:, 0:129], lhsT=Ktok[:, t, :], rhs=Vp[:, t, :], start=(r_ == 0), stop=(r_ == 1)),
                       r=[(Ktok, t // 8), (Vp, t // 4)], w=[pcs])
                jm = max(J - 1, 0)
                dve(lambda: V.scalar_tensor_tensor(out=Cst[:], in0=Cst[:], scalar=decay[:, jm, h:h + 1], in1=pcs[:, 0:129], op0=ALU.mult, op1=ALU.add),
                    r=[pcs, decay, Cst], w=[Cst])
                Cbf_next = Cbf_rot.next()
                act(lambda: A.activation(out=Cbf_next[:], in_=Cst[:], func=AF.Copy, scale=decay[:, J, h:h + 1]), r=[Cst, decay], w=[Cbf_next])
            sched.step(2)
            po = po_rot.next()
            pe(lambda: T.matmul(po[:, 0:129], lhsT=QT_own[:, J, :], rhs=Cbf[:], start=True, stop=False), r=[(QT_own, J // 2), Cbf], w=[po])
            for r_ in range(2):
                t = 2 * J + r_
                pe(lambda: T.matmul(po[:, 0:129], lhsT=stm[:, r_, :], rhs=Vp[:, t, :], start=False, stop=(r_ == 1)), r=[stm, (Vp, t // 4)], w=[po])
            for f_ in u_pending_dve:
                f_()
            u_pending_dve[:] = []
            while u_pending:
                u_pending.pop(0)()
            sm = sm_rot.next()
            dve(lambda: V.tensor_scalar(out=sm[:, 0:1], in0=po[:, 128:129], scalar1=-1.0, scalar2=thr_own[:, J, h:h + 1], op0=ALU.mult, op1=ALU.max), r=[po, thr_own], w=[sm])
            dve(lambda: V.tensor_tensor(out=sm[:, 1:2], in0=sm[:, 0:1], in1=po[:, 128:129], op=ALU.max), r=[sm, po], w=[sm])
            dve(lambda: V.reciprocal(out=sm[:, 2:3], in_=sm[:, 1:2]), r=[sm], w=[sm])
            dve(lambda: V.tensor_scalar(out=hc_t[:], in0=po[:, 0:128], scalar1=sm[:, 2:3], scalar2=None, op0=ALU.mult), r=[po, sm], w=[hc_t])
            if "h_cell" in dbg:
                dump("h_cell", hc_t[:], [hc_t], dbg["h_cell"][h, J * 128:(J + 1) * 128, :])
            act(lambda: A.activation(out=hc_junk[:], in_=hc_t[:], func=AF.Square, accum_out=sm[:, 3:4]), r=[hc_t], w=[hc_junk, sm])
            act(lambda: A.activation(out=sm[:, 4:5], in_=sm[:, 3:4], func=AF.Ln, scale=1.0 / 128, bias=cst[:, 0:1]), r=[sm, cst], w=[sm])
            act(lambda: A.activation(out=sm[:, 5:6], in_=sm[:, 4:5], func=AF.Exp, scale=-0.5), r=[sm], w=[sm])
            u = u_rot.next()

            def u_dve(u=u, sm=sm, h=h, J=J):
                dve(lambda: V.scalar_tensor_tensor(out=u[:], in0=hc_t[:], scalar=sm[:, 5:6], in1=og[:, J, h * 128:(h + 1) * 128], op0=ALU.mult, op1=ALU.mult),
                    r=[hc_t, sm, (og, J)], w=[u])
            u_pending_dve.append(u_dve)

            def u_fin(u=u, h=h, J=J):
                ptb = ptb_rot.next()
                pe(lambda: T.transpose(out=ptb[:, 0:128], in_=u[:], identity=ident_b[:]), r=[u, ident_b], w=[ptb])
                act(lambda: A.copy(out=uT_own[:, h, J * 128:(J + 1) * 128], in_=ptb[:, 0:128]), r=[ptb], w=[(uT_own, (h, J))])
            u_pending.append(u_fin)
            if Cbf_next is not None:
                Cbf = Cbf_next
    for f_ in u_pending_dve:
        f_()
    while u_pending:
        u_pending.pop(0)()
    pb_rot = Rot(pbs[0:6])
    pm.close()
    if "uT" in dbg:
        uf = sb(pa, "uf_dbg", [128, 4, NJ * 128], F32)
        dve(lambda: V.tensor_copy(out=uf[:], in_=uT_own[:]), r=[uT_own], w=[uf])
        dump("uT", uf[:], [uf], dbg["uT"].rearrange("(h p) t -> p h t", p=128))
        if "oT" not in dbg:
            final_wait()
            pes.close(); fw.close()
            return nc

    pn = Scope(arena)
    KcT = [sb(pn, "KcT%d" % g, [68, 256], BF16) for g in range(2)]
    Vc = [sb(pn, "Vc%d" % g, [128, 2, 64], BF16) for g in range(2)]
    OV = sb(pn, "OV", [128, 2, 64], BF16)
    fw.dma("sp", OV[:], dram["ov"], writes=[OV])
    pcm = Scope(arena)
    cmpT = sb(pcm, "cmpT", [128, S], BF16)
    w1sb = sb(pcm, "w1sb", [128, 32, 128], BF16)
    wcm = sb(pcm, "wcm", [128, KC, 128], BF16)
    w2b = sb(pcm, "w2b", [128, 64], BF16)
    posT = sb(pcm, "posT", [128, 32], BF16)
    posTf = sb(pcm, "posTf", [128, 32], F32)
    b1 = sb(pcm, "b1", [128, 1], F32)
    xb = sb(pcm, "xb", [128, 256], F32)
    tg = sb(pcm, "tg", [128, 256], F32)
    hidb = sb(pcm, "hidb", [128, 256], BF16)
    for which in ("k", "v"):
        w1d = dram["cmp_%s_w1" % which].rearrange("(pos d) h -> d pos h", d=64)
        for q8 in range(4):
            wst = wst_rot.next()
            stv = wst[:, 0:1024].rearrange("p (a n) -> p a n", a=8)
            fw.dma("sp", stv[0:64], w1d[:, q8 * 8:(q8 + 1) * 8, :], writes=[wst])
            fw.dma("sp", stv[64:128], w1d[:, q8 * 8:(q8 + 1) * 8, :], writes=[wst])
            act(lambda: A.copy(out=w1sb[:, q8 * 8:(q8 + 1) * 8, :], in_=stv), r=[wst], w=[w1sb])
        load_w(w2b[:], dram["cmp_%s_w2" % which], 64, [w2b])
        fw.dma("sp", posTf[0:64], dram["cmp_pos_%sT" % which], writes=[posTf])
        fw.dma("sp", posTf[64:128], dram["cmp_pos_%sT" % which], writes=[posTf])
        dve(lambda: V.tensor_copy(out=posT[:], in_=posTf[:]), r=[posTf], w=[posT])
        load_w_cols(wcm, 0, w_in, C_KCMP if which == "k" else C_VCMP, 128)
        for c in range(8):
            ps = pb_rot.next()
            for k in range(KC):
                pe(lambda: T.matmul(ps[:, :], lhsT=wcm[:, k, :], rhs=hT_all[:, k, c * 512:(c + 1) * 512], start=(k == 0), stop=(k == KC - 1)),
                   r=[wcm, (hT_all, c)], w=[ps])
            if c % 2:
                act(lambda: A.copy(out=cmpT[:, c * 512:(c + 1) * 512], in_=ps[:, :]), r=[ps], w=[(cmpT, c)])
            else:
                dve(lambda: V.tensor_copy(out=cmpT[:, c * 512:(c + 1) * 512], in_=ps[:, :]), r=[ps], w=[(cmpT, c)])
        maybe_stop("s0" + which)
        pb1 = pb_rot.next()
        for pos in range(32):
            pe(lambda: T.matmul(pb1[:, 0:1], lhsT=w1sb[0:64, pos, :], rhs=posT[0:64, pos:pos + 1], start=(pos == 0), stop=(pos == 31)),
               r=[w1sb, posT], w=[pb1])
        dve(lambda: V.tensor_copy(out=b1[:], in_=pb1[:, 0:1]), r=[pb1], w=[b1])
        maybe_stop("s1" + which)
        c16 = cmpT[:].rearrange("p (j s) -> p j s", s=16)
        for g in range(2):
            ph = pb_rot.next()
            for pos in range(32):
                rhs = c16[g * 64:(g + 1) * 64, 0:255, pos] if pos < 16 else c16[g * 64:(g + 1) * 64, 1:256, pos - 16]
                pe(lambda: T.matmul(ph[:, 0:255], lhsT=w1sb[g * 64:(g + 1) * 64, pos, :], rhs=rhs, start=(pos == 0), stop=(pos == 31)),
                   r=[w1sb, cmpT], w=[ph])
            act(lambda: A.activation(out=xb[:, 0:255], in_=ph[:, 0:255], func=AF.Identity, bias=b1[:, 0:1]), r=[ph, b1], w=[xb])
            dve(lambda: V.tensor_mul(out=tg[:, 0:255], in0=xb[:, 0:255], in1=xb[:, 0:255]), r=[xb], w=[tg])
            dve(lambda: V.tensor_scalar(out=tg[:, 0:255], in0=tg[:, 0:255], scalar1=0.044715, scalar2=1.0, op0=ALU.mult, op1=ALU.add), r=[tg], w=[tg])
            dve(lambda: V.tensor_mul(out=tg[:, 0:255], in0=tg[:, 0:255], in1=xb[:, 0:255]), r=[tg, xb], w=[tg])
            act(lambda: A.activation(out=tg[:, 0:255], in_=tg[:, 0:255], func=AF.Sigmoid, scale=1.5957691216057308), r=[tg], w=[tg])
            dve(lambda: V.memset(hidb[:], 0.0), w=[hidb])
            dve(lambda: V.tensor_mul(out=hidb[:, 0:255], in0=xb[:, 0:255], in1=tg[:, 0:255]), r=[xb, tg], w=[hidb])
            pk = pb_rot.next()
            if which == "k":
                pe(lambda: T.matmul(pk[0:64, 0:255], lhsT=w2b[:], rhs=hidb[:, 0:255], start=True, stop=True), r=[w2b, hidb], w=[pk])
                dve(lambda: V.memset(KcT[g][0:64, :], 0.0), w=[KcT[g]])
                dve(lambda: V.tensor_copy(out=KcT[g][0:64, 0:255], in_=pk[0:64, 0:255]), r=[pk], w=[KcT[g]])
                fw.dma("sp", KcT[g][64:68, :], dram["poskc"], writes=[KcT[g]])
            else:
                for c in range(2):
                    pe(lambda: T.matmul(pk[:, c * 64:(c + 1) * 64], lhsT=hidb[:, c * 128:(c + 1) * 128], rhs=w2b[:], start=True, stop=True),
                       r=[w2b, hidb], w=[pk])
                dve(lambda: V.tensor_copy(out=Vc[g][:], in_=pk[:, 0:128].rearrange("p (c d) -> p c d", c=2)), r=[pk], w=[Vc[g]])
    maybe_stop("s2")
    pcm.close()
    KTn = {}
    for b_ in ("slc", "win"):
        for g in range(2):
            KTn[(b_, g)] = sb(pn, "KT_%s%d" % (b_, g), [68, S], BF16)
    Vn = {b_: sb(pn, "V_%s" % b_, [128, NT, 2, 65], BF16) for b_ in ("slc", "win")}
    pkv = Scope(arena)
    wkn = sb(pkv, "wkn", [128, KC, 256], BF16)
    wvn = sb(pkv, "wvn", [128, KC, 256], BF16)
    load_w_cols(wkn, 0, w_in, C_KSLC, 128)
    load_w_cols(wkn, 128, w_in, C_KWIN, 128)
    load_w_cols(wvn, 0, w_in, C_VSLC, 128)
    load_w_cols(wvn, 128, w_in, C_VWIN, 128)
    i_ev = 0
    for bi_, b_ in enumerate(("slc", "win")):
        for g in range(2):
            kt_ = KTn[(b_, g)]
            fw.dma("sp", kt_[64:68, :], dram["posk"], writes=[(kt_, "pos")])
            for c in range(8):
                ps = pb_rot.next()
                for k in range(KC):
                    pe(lambda: T.matmul(ps[0:64, :], lhsT=wkn[:, k, bi_ * 128 + g * 64: bi_ * 128 + (g + 1) * 64], rhs=hT_all[:, k, c * 512:(c + 1) * 512],
                                        start=(k == 0), stop=(k == KC - 1)), r=[wkn, (hT_all, c)], w=[ps])
                i_ev += 1
                if i_ev % 2:
                    act(lambda: A.copy(out=kt_[0:64, c * 512:(c + 1) * 512], in_=ps[0:64, :]), r=[ps], w=[(kt_, c)])
                else:
                    dve(lambda: V.tensor_copy(out=kt_[0:64, c * 512:(c + 1) * 512], in_=ps[0:64, :]), r=[ps], w=[(kt_, c)])
    maybe_stop("s3")
    for b_ in ("slc", "win"):
        pool(lambda: G.memset(Vn[b_][:], 1.0), w=[Vn[b_]])
    maybe_stop("s4")
    for t2 in range(NT // 2):
        ps = pb_rot.next()
        for i in range(2):
            t = t2 * 2 + i
            for k in range(KC):
                pe(lambda: T.matmul(ps[:, i * 256:(i + 1) * 256], lhsT=hT_all[:, k, t * 128:(t + 1) * 128], rhs=wvn[:, k, :], start=(k == 0), stop=(k == KC - 1)),
                   r=[(hT_all, t // 4), wvn], w=[ps])
        for i in range(2):
            t = t2 * 2 + i
            for bi_, b_ in enumerate(("slc", "win")):
                src = ps[:, i * 256 + bi_ * 128: i * 256 + (bi_ + 1) * 128].rearrange("p (g d) -> p g d", g=2)
                dve(lambda: V.tensor_copy(out=Vn[b_][:, t, :, 0:64], in_=src), r=[ps], w=[(Vn[b_], t2)])
    maybe_stop("s5")
    pkv.close()
    if "stopB" in dbg:
        uf = sb(pa, "kc_dbg", [128, 128], F32)
        dve(lambda: V.tensor_copy(out=uf[:], in_=Vc[0][:].rearrange("p c d -> p (c d)")), r=[Vc[0]], w=[uf])
        dump("stopB", uf[:], [uf])
        final_wait()
        pes.close(); fw.close()
        return nc
    pa.close()

    oT_own = sb(ps1, "oT_own", [128, 4, NJ * 128], BF16, top=True)
    xs_rot = Rot([sb(gs, "xs%d" % i, [128, D], F32) for i in range(3)])
    xn_rot = Rot([sb(gs, "xn%d" % i, [128, D], BF16) for i in range(3)])
    pq_ = Scope(arena)
    eall = sb(pq_, "eall", [64, S], BF16)
    fw.dma("sp", eall[:], dram["eall"], writes=[eall])
    cmask = sb(pq_, "cmask", [128, NJ, 2, 128], BF16)
    fw.dma("sp", cmask[:], dram["cmask"], writes=[cmask])
    wmask = sb(pq_, "wmask", [128, 6, 128], BF16)
    fw.dma("sp", wmask[:], dram["wmask"], writes=[wmask])
    selm = sb(pq_, "selm", [128, NJ, 64], F32)
    fw.dma("sp", selm[:], dram["selm"], writes=[selm])
    selc = sb(pq_, "selc", [128, NJ, 64], F32)
    fw.dma("sp", selc[:], dram["selc"], writes=[selc])
    wnq = sb(pq_, "wnq", [128, KC, 512], BF16)
    load_w_cols(wnq, 0, w_in, C_NSQ, 512)
    wng = sb(pq_, "wng", [128, KC, 24], BF16)
    load_w_cols(wng, 0, w_in, C_NSG, 24)
    hJ_rot = Rot([sb(pq_, "hJ%d" % i, [128, KC, 128], BF16) for i in range(2)])
    QTg_rot = [Rot([sb(pq_, "QTg%d_%d" % (g, i), [68, 512], BF16) for i in range(2)]) for g in range(2)]
    gsig_rot = Rot([sb(pq_, "gsig%d" % i, [128, 24], F32) for i in range(3)])
    P_rot = Rot([sb(pq_, "P%d" % i, [128, 512], BF16) for i in range(4)])
    selT_g = [sb(pq_, "selT%d" % g, [64, 128], BF16) for g in range(2)]
    cu_sb_g = [sb(pq_, "cu_sb%d" % g, [128, 512], F32) for g in range(2)]
    Msb_rot = Rot([sb(pq_, "Msb%d" % i, [128, NT, 128], BF16) for i in range(2)])
    sc_ = sb(pq_, "sc", [128, 64], F32)
    scw = sb(pq_, "scw", [128, 64], F32)
    psl = sb(pq_, "psl", [128, 64], F32)
    nsb_g = [sb(pq_, "nsb%d" % g, [128, 64], BF16) for g in range(2)]
    m16 = sb(pq_, "m16", [128, 16], F32)
    dd_g = [sb(pq_, "dd%d" % g, [128, 3, 4], F32) for g in range(2)]
    gr = sb(pq_, "gr", [128, 3, 4], F32)
    o_f = sb(pq_, "o_f", [128, 4, 64], F32)
    o_t = sb(pq_, "o_t", [128, 4, 64], F32)
    o_b = sb(pq_, "o_b", [128, 256], BF16)
    S_rot = Rot(pbs[0:3])
    MB = pbs[6]
    CU, OW, OS, PQ = pbs[3], pbs[4], pbs[5], pbs[6]
    PT = ptbs[1]
    load_gain("norm_mix")
    bc4 = lambda ap2: ap2.unsqueeze(1).to_broadcast([128, 4, 128])

    oc_sb_g = [sb(pq_, "oc_sb%d" % g, [128, 256], F32) for g in range(2)]
    ow_sb_g = [sb(pq_, "ow_sb%d" % g, [128, 260], F32) for g in range(2)]
    os_sb = sb(pq_, "os_sb", [128, 260], F32)
    ob_rot = Rot([sb(pq_, "o_b%d" % i, [128, 256], BF16) for i in range(2)])

    def emit_S(tl):
        Sp = S_rot.next()
        tl["Sp"] = Sp
        extra = tl["extra"]
        pe(lambda: T.matmul(Sp[:, :], lhsT=tl["kT"], rhs=tl["QT"][0:68, :], start=True, stop=(len(extra) == 0)), r=tl["kbufs"] + [tl["QT"]], w=[Sp])
        for i, (l_ap, r_ap, bufs) in enumerate(extra):
            pe(lambda: T.matmul(Sp[:, :], lhsT=l_ap, rhs=r_ap, start=False, stop=(i == len(extra) - 1)), r=bufs, w=[Sp])
        Pb = P_rot.next()
        tl["Pb"] = Pb
        act(lambda: A.activation(out=Pb[:], in_=Sp[:, :], func=AF.Exp), r=[Sp], w=[Pb])

    def emit_PV(tl):
        Pb = tl["Pb"]
        if tl["kind"] == "slc":
            Msb = state[("Msb", tl["g"])]
            kt_ = tl["kt"]
            dve(lambda: V.tensor_tensor(out=Pb[:].rearrange("p (h t) -> p h t", h=4), in0=Pb[:].rearrange("p (h t) -> p h t", h=4),
                                        in1=Msb[:, kt_, :].unsqueeze(1).to_broadcast([128, 4, 128]), op=ALU.mult), r=[Pb, (Msb, kt_ // 4)], w=[Pb])
        Oacc, vrhs, vbufs, first, last, also = tl["Oacc"], tl["vrhs"], tl["vbufs"], tl["first"], tl["last"], tl.get("also")
        W = vrhs.shape[-1]
        for hh in range(4):
            pe(lambda: T.matmul(Oacc[0][:, Oacc[1] + hh * W: Oacc[1] + (hh + 1) * W], lhsT=Pb[:, hh * 128:(hh + 1) * 128], rhs=vrhs,
                                start=(first and hh == 0), stop=last, skip_group_check=True),
               r=[Pb] + vbufs, w=[Oacc[0]])
            if also is not None:
                a_ap, a_bufs, a_off = also
                pe(lambda: T.matmul(Oacc[0][:, a_off + hh * 64: a_off + (hh + 1) * 64], lhsT=Pb[:, hh * 128:(hh + 1) * 128], rhs=a_ap,
                                    start=False, stop=last, skip_group_check=True),
                   r=[Pb] + a_bufs, w=[Oacc[0]])

    def prepDMA(J):
        if J not in state["xs"]:
            state["xs"][J] = load_x(x_own[J * 128:(J + 1) * 128, :])
        return state["xs"][J]

    def prepA(J):
        if J not in state["prepA"]:
            state["prepA"][J] = rmsnorm_front(prepDMA(J))
        return state["prepA"][J]

    def prepB1(J):
        if J not in state["hJ"]:
            hJ = hJ_rot.next()
            rmsnorm_back(prepA(J), hJ, 0)
            state["hJ"][J] = hJ
        return state["hJ"][J]

    def prepB2(J):
        if J not in state["gsig"]:
            hJ = prepB1(J)
            gsig = gsig_rot.next()
            for k in range(KC):
                pe(lambda: T.matmul(PQ[:, 0:24], lhsT=hJ[:, k, :], rhs=wng[:, k, :], start=(k == 0), stop=(k == KC - 1)), r=[hJ, wng], w=[PQ])
            act(lambda: A.activation(out=gsig[:], in_=PQ[:, 0:24], func=AF.Sigmoid), r=[PQ], w=[gsig])
            state["gsig"][J] = gsig
        return state["gsig"][J]

    def prepQ(J, g):
        if (J, g) not in state["QT"]:
            hJ = prepB1(J)
            prepB2(J)
            QT = QTg_rot[g].next()
            for hh in range(4):
                hd = g * 4 + hh
                for k in range(KC):
                    pe(lambda: T.matmul(PQ[0:64, hh * 128:(hh + 1) * 128], lhsT=wnq[:, k, hd * 64:(hd + 1) * 64], rhs=hJ[:, k, :], start=(k == 0), stop=(k == KC - 1)),
                       r=[hJ, wnq], w=[PQ])
            dve(lambda: V.tensor_scalar(out=QT[0:64, :], in0=PQ[0:64, :], scalar1=0.125, scalar2=None, op0=ALU.mult), r=[PQ], w=[QT])
            fw.dma("pool", QT[64:68, :], dram["posq"][g, :, J * 512:(J + 1) * 512], writes=[QT])
            state["QT"][(J, g)] = QT
        return state["QT"][(J, g)]

    def prep(J):
        return dict(gsig=prepB2(J), QT=[prepQ(J, 0), prepQ(J, 1)])

    def topk_dve(J, g):
        dd, nsb, oc_sb, cu_sb = dd_g[g], nsb_g[g], oc_sb_g[g], cu_sb_g[g]
        act(lambda: A.copy(out=cu_sb[:], in_=CU[:, :]), r=[CU], w=[cu_sb])
        U3 = cu_sb[:, 256:512].rearrange("p (h b) -> p h b", h=4)
        dve(lambda: V.tensor_reduce(out=dd[:, 0, :], in_=U3, axis=AX.X, op=ALU.add), r=[cu_sb], w=[dd])
        dve(lambda: V.tensor_scalar_max(out=dd[:, 0, :], in0=dd[:, 0, :], scalar1=1e-30), r=[dd], w=[dd])
        dve(lambda: V.reciprocal(out=dd[:, 0, :], in_=dd[:, 0, :]), r=[dd], w=[dd])
        dve(lambda: V.tensor_scalar(out=psl[:], in0=U3[:, 0, :], scalar1=dd[:, 0, 0:1], scalar2=None, op0=ALU.mult), r=[cu_sb, dd], w=[psl])
        for hh in range(1, 4):
            dve(lambda: V.scalar_tensor_tensor(out=psl[:], in0=U3[:, hh, :], scalar=dd[:, 0, hh:hh + 1], in1=psl[:], op0=ALU.mult, op1=ALU.add),
                r=[cu_sb, dd, psl], w=[psl])
        dve(lambda: V.tensor_mul(out=sc_[:], in0=psl[:], in1=selm[:, J, :]), r=[psl, selm], w=[sc_])
        dve(lambda: V.tensor_add(out=sc_[:], in0=sc_[:], in1=selc[:, J, :]), r=[sc_, selc], w=[sc_])
        dve(lambda: V.max(out=m16[:, 0:8], in_=sc_[:]), r=[sc_], w=[m16])
        dve(lambda: V.match_replace(out=scw[:], in_to_replace=m16[:, 0:8], in_values=sc_[:], imm_value=-1e9), r=[sc_, m16], w=[scw])
        dve(lambda: V.max(out=m16[:, 8:16], in_=scw[:]), r=[scw], w=[m16])
        dve(lambda: V.tensor_tensor(out=nsb[:], in0=sc_[:], in1=m16[:, 15:16].to_broadcast([128, 64]), op=ALU.is_ge), r=[sc_, m16], w=[nsb])
        dve(lambda: V.tensor_copy(out=oc_sb[:], in_=cu_sb[:, 0:256]), r=[cu_sb], w=[oc_sb])

    def topk_pe(g):
        nsb, selT = nsb_g[g], selT_g[g]
        pe(lambda: T.transpose(out=PT[0:64, 0:128], in_=nsb[:], identity=ident_b[:]), r=[nsb, ident_b], w=[PT])
        dve(lambda: V.tensor_copy(out=selT[:], in_=PT[0:64, 0:128]), r=[PT], w=[selT])

    def mask_group(J, g, k4):
        if k4 == 0:
            state[("Msb", g)] = Msb_rot.next()
        Msb = state[("Msb", g)]
        selT = selT_g[g]
        nk = 2 * J + 2
        n4 = min(4, nk - k4)
        for i in range(n4):
            kt = k4 + i
            pe(lambda: T.matmul(MB[:, i * 128:(i + 1) * 128], lhsT=eall[:, kt * 128:(kt + 1) * 128], rhs=selT[:], start=True, stop=True),
               r=[eall, selT], w=[MB])
        act(lambda: A.copy(out=Msb[:, k4:k4 + n4, :], in_=MB[:, 0:n4 * 128].rearrange("p (k t) -> p k t", k=n4)), r=[MB], w=[(Msb, k4 // 4)])

    def evac_ow(g):
        act(lambda: A.copy(out=ow_sb_g[g][:], in_=OW[:, 0:260]), r=[OW], w=[ow_sb_g[g]])

    def combine(J, g, gsig):
        dd, oc_sb, ow_sb = dd_g[g], oc_sb_g[g], ow_sb_g[g]
        act(lambda: A.copy(out=os_sb[:], in_=OS[:, 0:260]), r=[OS], w=[os_sb])
        g3 = gsig[:, g * 12:(g + 1) * 12].rearrange("p (h b) -> p h b", b=3)
        OW3 = ow_sb[:].rearrange("p (h w) -> p h w", h=4)
        OS3 = os_sb[:].rearrange("p (h w) -> p h w", h=4)
        OC3 = oc_sb[:].rearrange("p (h w) -> p h w", h=4)
        dve(lambda: V.tensor_copy(out=dd[:, 1, :], in_=OS3[:, :, 64]), r=[os_sb], w=[dd])
        dve(lambda: V.tensor_copy(out=dd[:, 2, :], in_=OW3[:, :, 64]), r=[ow_sb], w=[dd])
        dve(lambda: V.reciprocal(out=dd[:, 1:3, :], in_=dd[:, 1:3, :]), r=[dd], w=[dd])
        dve(lambda: V.tensor_mul(out=gr[:], in0=dd[:], in1=g3.rearrange("p h b -> p b h")), r=[dd, gsig], w=[gr])
        dve(lambda: V.tensor_tensor(out=o_f[:], in0=OC3, in1=gr[:, 0, :].unsqueeze(2).to_broadcast([128, 4, 64]), op=ALU.mult), r=[oc_sb, gr], w=[o_f])
        dve(lambda: V.tensor_tensor(out=o_t[:], in0=OS3[:, :, 0:64], in1=gr[:, 1, :].unsqueeze(2).to_broadcast([128, 4, 64]), op=ALU.mult), r=[os_sb, gr], w=[o_t])
        dve(lambda: V.tensor_add(out=o_f[:], in0=o_f[:], in1=o_t[:]), r=[o_f, o_t], w=[o_f])
        dve(lambda: V.tensor_tensor(out=o_t[:], in0=OW3[:, :, 0:64], in1=gr[:, 2, :].unsqueeze(2).to_broadcast([128, 4, 64]), op=ALU.mult), r=[ow_sb, gr, o_t], w=[o_t])
        ob = ob_rot.next()
        dve(lambda: V.tensor_add(out=ob[:].rearrange("p (h d) -> p h d", h=4), in0=o_f[:], in1=o_t[:]), r=[o_f, o_t], w=[ob])

        def fin():
            for i in range(2):
                pe(lambda: T.transpose(out=PT[:, 128 + i * 128:256 + i * 128], in_=ob[:, i * 128:(i + 1) * 128], identity=ident_b[:]), r=[ob, ident_b], w=[PT])
            act(lambda: A.copy(out=oT_own[:, g * 2:g * 2 + 2, J * 128:(J + 1) * 128], in_=PT[:, 128:384].rearrange("p (i t) -> p i t", i=2)), r=[PT], w=[(oT_own, (g, J))])
        return fin

    seq = []
    state = {"prep": {}, "prepA": {}, "xs": {}, "hJ": {}, "gsig": {}, "QT": {}, "deferred": []}

    def get_prep(J):
        if J not in state["prep"]:
            state["prep"][J] = prep(J)
        return state["prep"][J]

    def add_post(tl, f_):
        tl.setdefault("post", []).append(f_)

    for J in range(NJ):
        cmp_t, win_t, slc_t = [[], []], [[], []], [[], []]
        for g in range(2):
            chunks = [0] if J <= 7 else [0, 1]
            for ci, c in enumerate(chunks):
                cmp_t[g].append(dict(J=J, g=g, kind="cmp", kT=KcT[g][0:68, c * 128:(c + 1) * 128], kbufs=[KcT[g]],
                                     extra_fn=(lambda J=J, c=c: [(ident_b[:], bc4(cmask[:, J, c, :]), [ident_b, cmask])]),
                                     Oacc=(CU, 0), vrhs=Vc[g][:, c, :], vbufs=[Vc[g]], first=(ci == 0), last=(ci == len(chunks) - 1),
                                     also=(OV[:, c, :], [OV], 256)))
            add_post(cmp_t[g][-1], lambda J=J, g=g: topk_dve(J, g))
            wl = [(r_, 2 * J - 4 + r_) for r_ in range(6) if 2 * J - 4 + r_ >= 0]
            kbw = KTn[("win", g)]
            for wi, (r_, kt) in enumerate(wl):
                win_t[g].append(dict(J=J, g=g, kind="win", kT=kbw[0:68, kt * 128:(kt + 1) * 128], kbufs=[(kbw, kt // 4), (kbw, "pos")],
                                     extra_fn=(lambda r_=r_: [(ident_b[:], bc4(wmask[:, r_, :]), [ident_b, wmask])] if r_ in (0, 1, 4, 5) else []),
                                     Oacc=(OW, 0), vrhs=Vn["win"][:, kt, g, :], vbufs=[(Vn["win"], kt // 2)], first=(wi == 0), last=(wi == len(wl) - 1)))
            add_post(win_t[g][-1], lambda g=g: evac_ow(g))
            nk = 2 * J + 2
            kbs = KTn[("slc", g)]
            for kt in range(nk):
                def ex(kt=kt, J=J):
                    e_ = []
                    if kt >= 2 * J:
                        e_.append((ident_b[:], bc4(wmask[:, 4 + kt - 2 * J, :]), [ident_b, wmask]))
                    return e_
                slc_t[g].append(dict(J=J, g=g, kind="slc", kt=kt, kT=kbs[0:68, kt * 128:(kt + 1) * 128], kbufs=[(kbs, kt // 4), (kbs, "pos")], extra_fn=ex,
                                     Oacc=(OS, 0), vrhs=Vn["slc"][:, kt, g, :], vbufs=[(Vn["slc"], kt // 2)], first=(kt == 0), last=(kt == nk - 1)))

            def comb_hook(J=J, g=g):
                state["deferred"].append(combine(J, g, get_prep(J)["gsig"]))
            add_post(slc_t[g][-1], comb_hook)
        nw = len(win_t[0])
        add_post(win_t[0][nw - 1], lambda: topk_pe(0))
        add_post(win_t[1][nw - 1], lambda: topk_pe(1))
        slot_list = cmp_t[0] + cmp_t[1] + win_t[0] + win_t[1] + slc_t[0] + slc_t[1]
        idx0 = slot_list.index(win_t[1][min(1, nw - 1)])
        idx1 = slot_list.index(slc_t[0][min(1, len(slc_t[0]) - 1)])
        for gi, ib in ((0, idx0), (1, idx1)):
            for i, k4 in enumerate(range(0, 2 * J + 2, 4)):
                add_post(slot_list[ib + i], lambda J=J, gi=gi, k4=k4: mask_group(J, gi, k4))

        def flush():
            for f_ in state["deferred"]:
                f_()
            state["deferred"] = []
        add_post(win_t[1][min(2, nw - 1)], flush)
        cmp_t[0][0]["pre"] = [lambda J=J: (get_prep(J), prepA(J + 1) if J + 1 < NJ else None, prepDMA(J + 2) if J + 2 < NJ else None)]
        if J + 1 < NJ:
            nw0 = len(win_t[0])
            add_post(win_t[0][0], lambda J=J: prepB1(J + 1))
            add_post(win_t[0][min(1, nw0 - 1)], lambda J=J: prepB2(J + 1))
            add_post(win_t[0][min(2, nw0 - 1)], lambda J=J: prepQ(J + 1, 0))
            add_post(win_t[0][min(3, nw0 - 1)], lambda J=J: prepQ(J + 1, 1))
        seq += slot_list

    def do_S(tl):
        for f_ in tl.get("pre", []):
            f_()
        tl["QT"] = get_prep(tl["J"])["QT"][tl["g"]]
        tl["extra"] = tl["extra_fn"]()
        emit_S(tl)

    LA = 2
    for i in range(min(LA, len(seq))):
        do_S(seq[i])
    for i in range(len(seq)):
        if i + LA < len(seq):
            do_S(seq[i + LA])
        emit_PV(seq[i])
        for f_ in seq[i].get("post", []):
            f_()
    for f_ in state["deferred"]:
        f_()
    pq_.close()
    pn.close()
    if "oT" in dbg:
        uf = sb(ps1, "of_dbg", [128, 4, NJ * 128], F32)
        dve(lambda: V.tensor_copy(out=uf[:], in_=oT_own[:]), r=[oT_own], w=[uf])
        dump("oT", uf[:], [uf], dbg["oT"].rearrange("(h p) t -> p h t", p=128))
        if "x1" not in dbg:
            final_wait()
            pes.close(); fw.close()
            return nc

    pd = Scope(arena)
    hT_own = sb(pd, "hT_own", [128, KC, NJ * 128], BF16)
    zT = sb(pd, "zT", [128, KC, NJ * 128], BF16)
    wbm = sb(pd, "wbm", [128, 4, D], BF16)
    wbn = sb(pd, "wbn", [128, 4, D], BF16)
    load_w_cols(wbm, 0, dram["w_br_mlstm"], 0, D, kc=4)
    load_w_cols(wbn, 0, dram["w_br_nsa"], 0, D, kc=4)
    load_gain("norm_mix")
    norm_loop([x_own[J * 128:(J + 1) * 128, :] for J in range(NJ)], hT_own, lambda J: J // 4)
    wmg_rot = Rot([sb(pd, "wmg%d" % i, [128, KC, 128], BF16) for i in range(4)])
    sg_rot = Rot([sb(pd, "sg%d" % i, [128, 512], F32) for i in range(4)])
    for cc in range(8):
        wA, wB = wmg_rot.next(), wmg_rot.next()
        load_w_cols(wA, 0, w_in, C_MRG + cc * 128, 128)
        load_w_cols(wB, 0, w_in, C_MRG + 1024 + cc * 128, 128)
        for tc in range(4):
            tsl = slice(tc * 512, (tc + 1) * 512)
            pA, pB, pC, pD = pb_rot.next(), pb_rot.next(), pb_rot.next(), pb_rot.next()
            for k in range(KC):
                pe(lambda: T.matmul(pA[:, :], lhsT=wA[:, k, :], rhs=hT_own[:, k, tsl], start=(k == 0), stop=(k == KC - 1)), r=[wA, (hT_own, tc)], w=[pA])
            for k in range(KC):
                pe(lambda: T.matmul(pB[:, :], lhsT=wB[:, k, :], rhs=hT_own[:, k, tsl], start=(k == 0), stop=(k == KC - 1)), r=[wB, (hT_own, tc)], w=[pB])
            for k in range(4):
                pe(lambda: T.matmul(pC[:, :], lhsT=wbm[:, k, cc * 128:(cc + 1) * 128], rhs=uT_own[:, k, tsl], start=(k == 0), stop=(k == 3)), r=[wbm, uT_own], w=[pC])
            for k in range(4):
                pe(lambda: T.matmul(pD[:, :], lhsT=wbn[:, k, cc * 128:(cc + 1) * 128], rhs=oT_own[:, k, tsl], start=(k == 0), stop=(k == 3)), r=[wbn, oT_own], w=[pD])
            sA, sB = sg_rot.next(), sg_rot.next()
            act(lambda: A.activation(out=sA[:], in_=pA[:, :], func=AF.Sigmoid), r=[pA], w=[sA])
            act(lambda: A.activation(out=sB[:], in_=pB[:, :], func=AF.Sigmoid), r=[pB], w=[sB])
            dve(lambda: V.tensor_mul(out=sA[:], in0=sA[:], in1=pC[:, :]), r=[sA, pC], w=[sA])
            dve(lambda: V.tensor_mul(out=sB[:], in0=sB[:], in1=pD[:, :]), r=[sB, pD], w=[sB])
            dve(lambda: V.tensor_add(out=zT[:, cc, tsl], in0=sA[:], in1=sB[:]), r=[sA, sB], w=[(zT, tc)])
    pd_keep = [zT]
    for b_ in pd.bufs:
        if b_ is not zT:
            arena.release(b_)
    pd.bufs = [zT]
    ps1.close()
    px = Scope(arena)
    xr = sb(px, "xr", [128, NJ, D], F32)
    wmo = sb(pd, "wmo", [128, KC, D], BF16)
    load_w_cols(wmo, 0, dram["w_mix_out"], 0, D)
    for J in range(NJ):
        xs_buf = xs_rot.next()
        fw.dma("sp", xs_buf[:], x_own[J * 128:(J + 1) * 128, :], writes=[xs_buf])
        for hf in range(2):
            po = pb_rot.next()
            for k in range(KC):
                pe(lambda: T.matmul(po[:, :], lhsT=zT[:, k, J * 128:(J + 1) * 128], rhs=wmo[:, k, hf * 512:(hf + 1) * 512], start=(k == 0), stop=(k == KC - 1)),
                   r=[(zT, J // 4), wmo], w=[po])
            dve(lambda: V.tensor_add(out=xr[:, J, hf * 512:(hf + 1) * 512], in0=po[:, :], in1=xs_buf[:, hf * 512:(hf + 1) * 512]), r=[po, xs_buf], w=[(xr, J)])
    pd.close()
    if "x1" in dbg:
        for J in range(NJ):
            dump("x1", xr[:, J, :], [(xr, J)], dbg["x1"][J * 128:(J + 1) * 128, :])
        if "x2" not in dbg:
            final_wait()
            pes.close(); fw.close()
            return nc

    pe_ = Scope(arena)
    ph = Scope(arena)
    qT = sb(pe_, "qT", [128, KC, NJ * 128], BF16)
    hxT = sb(ph, "hxT", [128, KC, NJ * 128], BF16)
    ones_b = sb(pe_, "ones_b", [128, 128], BF16)
    pool(lambda: G.memset(ones_b[:], 1.0), w=[ones_b])
    memT = sb(pe_, "memT", [128, KC, 256], BF16)
    load_gain("norm_mem")
    for t in range(2):
        rmsnorm_T(mem[t * 128:(t + 1) * 128, :], memT, t * 128)
    KxT = sb(pe_, "KxT", [128, KC, 256], BF16)
    Vx = sb(pe_, "Vx", [128, 2, D], BF16)
    wch_rot = Rot([sb(pe_, "wch%d" % i, [128, KC, 128], BF16) for i in range(3)])
    for j in range(8):
        wc = wch_rot.next()
        load_w_cols(wc, 0, dram["xa_wkv"], j * 128, 128)
        ps = pb_rot.next()
        for k in range(KC):
            pe(lambda: T.matmul(ps[:, 0:256], lhsT=wc[:, k, :], rhs=memT[:, k, :], start=(k == 0), stop=(k == KC - 1)), r=[wc, memT], w=[ps])
        dve(lambda: V.tensor_copy(out=KxT[:, j, :], in_=ps[:, 0:256]), r=[ps], w=[KxT])
    for j in range(8):
        wc = wch_rot.next()
        load_w_cols(wc, 0, dram["xa_wkv"], D + j * 128, 128)
        ps = pb_rot.next()
        for mc in range(2):
            for k in range(KC):
                pe(lambda: T.matmul(ps[:, mc * 128:(mc + 1) * 128], lhsT=memT[:, k, mc * 128:(mc + 1) * 128], rhs=wc[:, k, :], start=(k == 0), stop=(k == KC - 1)),
                   r=[wc, memT], w=[ps])
        dve(lambda: V.tensor_copy(out=Vx[:, :, j * 128:(j + 1) * 128], in_=ps[:, 0:256].rearrange("p (m d) -> p m d", m=2)), r=[ps], w=[Vx])
    load_gain("norm_xattn")
    norm_loop([(xr[:, J, :], xr) for J in range(NJ)], hxT, lambda J: J // 4)
    for j in range(8):
        wc = wch_rot.next()
        load_w_cols(wc, 0, dram["xa_wq"], j * 128, 128)
        for tc in range(4):
            ps = pb_rot.next()
            for k in range(KC):
                pe(lambda: T.matmul(ps[:, :], lhsT=wc[:, k, :], rhs=hxT[:, k, tc * 512:(tc + 1) * 512], start=(k == 0), stop=(k == KC - 1)), r=[wc, (hxT, tc)], w=[ps])
            if tc % 2:
                act(lambda: A.copy(out=qT[:, j, tc * 512:(tc + 1) * 512], in_=ps[:, :]), r=[ps], w=[(qT, j)])
            else:
                dve(lambda: V.tensor_copy(out=qT[:, j, tc * 512:(tc + 1) * 512], in_=ps[:, :]), r=[ps], w=[(qT, j)])
    ph.close()
    oTx = sb(pe_, "oTx", [128, KC, NJ * 128], BF16)
    Px_rot = Rot([sb(pe_, "Px%d" % i, [128, 512], BF16) for i in range(4)])
    rdx_rot = Rot([sb(pe_, "rdx%d" % i, [128, 512], F32) for i in range(2)])
    def xa_S(h, tc):
        tsl = slice(tc * 512, (tc + 1) * 512)
        Pm = []
        for mc in range(2):
            Sp = pb_rot.next()
            for c in range(2):
                pe(lambda: T.matmul(Sp[:, :], lhsT=KxT[:, 2 * h + c, mc * 128:(mc + 1) * 128], rhs=qT[:, 2 * h + c, tsl], start=(c == 0), stop=(c == 1)),
                   r=[KxT, (qT, 2 * h + c)], w=[Sp])
            Pb = Px_rot.next()
            act(lambda: A.activation(out=Pb[:], in_=Sp[:, :], func=AF.Exp, scale=1.0 / 16.0), r=[Sp], w=[Pb])
            Pm.append(Pb)
        return Pm

    def xa_P(h, tc, Pm):
        tsl = slice(tc * 512, (tc + 1) * 512)
        pden = pb_rot.next()
        for mc in range(2):
            pe(lambda: T.matmul(pden[:, :], lhsT=ones_b[:], rhs=Pm[mc][:], start=(mc == 0), stop=(mc == 1)), r=[ones_b, Pm[mc]], w=[pden])
        rdx = rdx_rot.next()
        dve(lambda: V.reciprocal(out=rdx[:], in_=pden[:, :]), r=[pden], w=[rdx])
        for c in range(2):
            pov = pb_rot.next()
            for mc in range(2):
                pe(lambda: T.matmul(pov[:, :], lhsT=Vx[:, mc, (2 * h + c) * 128:(2 * h + c + 1) * 128], rhs=Pm[mc][:], start=(mc == 0), stop=(mc == 1)),
                   r=[Vx, Pm[mc]], w=[pov])
            dve(lambda: V.tensor_mul(out=oTx[:, 2 * h + c, tsl], in0=pov[:, :], in1=rdx[:]), r=[pov, rdx], w=[(oTx, tc)])

    units = [(h, tc) for h in range(4) for tc in range(4)]
    pm_next = xa_S(*units[0])
    for i, (h, tc) in enumerate(units):
        pm_cur = pm_next
        if i + 1 < len(units):
            pm_next = xa_S(*units[i + 1])
        xa_P(h, tc, pm_cur)
    wxo = sb(pe_, "wxo", [128, KC, D], BF16)
    load_w_cols(wxo, 0, dram["xa_wo"], 0, D)
    for J in range(NJ):
        for hf in range(2):
            po = pb_rot.next()
            for k in range(KC):
                pe(lambda: T.matmul(po[:, :], lhsT=oTx[:, k, J * 128:(J + 1) * 128], rhs=wxo[:, k, hf * 512:(hf + 1) * 512], start=(k == 0), stop=(k == KC - 1)),
                   r=[(oTx, J // 4), wxo], w=[po])
            dve(lambda: V.tensor_add(out=xr[:, J, hf * 512:(hf + 1) * 512], in0=po[:, :], in1=xr[:, J, hf * 512:(hf + 1) * 512]), r=[po, (xr, J)], w=[(xr, J)])
    pe_.close()
    if "x2" in dbg:
        for J in range(NJ):
            dump("x2", xr[:, J, :], [(xr, J)], dbg["x2"][J * 128:(J + 1) * 128, :])
        if "x3" not in dbg:
            final_wait()
            pes.close(); fw.close()
            return nc

    pf = Scope(arena)
    hmT = sb(pf, "hmT", [128, KC, NJ * 128], BF16)
    load_gain("norm_ffn")
    norm_loop([(xr[:, J, :], xr) for J in range(NJ)], hmT, lambda J: J // 4)
    wr = sb(pf, "wr", [128, KC, 20], BF16)
    load_w_cols(wr, 0, dram["router_w"], 0, 20)
    rb = sb(pf, "rb", [128, 20], F32)
    fw.dma("sp", rb[:], dram["router_b_rep"].partition_broadcast(128), writes=[rb])
    wts = sb(pf, "wts", [128, NJ, 16], F32)
    lg = sb(pf, "lg", [128, 20], F32)
    rt = sb(pf, "rt", [128, 64], F32)
    t44 = sb(pf, "t44", [128, 4, 4], F32)
    def route_tile(J):
        pr = pb_rot.next()
        for k in range(KC):
            pe(lambda: T.matmul(pr[:, 0:20], lhsT=hmT[:, k, J * 128:(J + 1) * 128], rhs=wr[:, k, :], start=(k == 0), stop=(k == KC - 1)), r=[(hmT, J // 4), wr], w=[pr])
        dve(lambda: V.tensor_add(out=lg[:], in0=pr[:, 0:20], in1=rb[:]), r=[pr, rb], w=[lg])
        gmx, ngm, gsum, gval = rt[:, 0:1], rt[:, 1:2], rt[:, 2:3], rt[:, 3:4]
        gm, eg, ein, mk1, e2, mk2, cw = rt[:, 4:8], rt[:, 8:12], rt[:, 12:16], rt[:, 16:20], rt[:, 20:24], rt[:, 24:28], rt[:, 28:32]
        m1, m2, dlt, wa, wb_ = rt[:, 32:33], rt[:, 33:34], rt[:, 34:35], rt[:, 35:36], rt[:, 36:37]
        dve(lambda: V.tensor_reduce(out=gmx, in_=lg[:, 0:4], axis=AX.X, op=ALU.max), r=[lg], w=[rt])
        dve(lambda: V.tensor_tensor(out=gm, in0=lg[:, 0:4], in1=gmx.to_broadcast([128, 4]), op=ALU.is_equal), r=[lg, rt], w=[rt])
        dve(lambda: V.tensor_scalar(out=ngm, in0=gmx, scalar1=-1.0, scalar2=None, op0=ALU.mult), r=[rt], w=[rt])
        act(lambda: A.activation(out=eg, in_=lg[:, 0:4], func=AF.Exp, bias=ngm, accum_out=gsum), r=[lg, rt], w=[rt])
        dve(lambda: V.reciprocal(out=gval, in_=gsum), r=[rt], w=[rt])
        dve(lambda: V.tensor_tensor(out=t44[:], in0=lg[:, 4:20].rearrange("p (g e) -> p g e", g=4), in1=gm.unsqueeze(2).to_broadcast([128, 4, 4]), op=ALU.mult), r=[lg, rt], w=[t44])
        dve(lambda: V.tensor_reduce(out=ein, in_=t44[:].rearrange("p g e -> p e g"), axis=AX.X, op=ALU.add), r=[t44], w=[rt])
        dve(lambda: V.tensor_reduce(out=m1, in_=ein, axis=AX.X, op=ALU.max), r=[rt], w=[rt])
        dve(lambda: V.tensor_tensor(out=mk1, in0=ein, in1=m1.to_broadcast([128, 4]), op=ALU.is_equal), r=[rt], w=[rt])
        dve(lambda: V.scalar_tensor_tensor(out=e2, in0=mk1, scalar=-1e9, in1=ein, op0=ALU.mult, op1=ALU.add), r=[rt], w=[rt])
        dve(lambda: V.tensor_reduce(out=m2, in_=e2, axis=AX.X, op=ALU.max), r=[rt], w=[rt])
        dve(lambda: V.tensor_tensor(out=mk2, in0=e2, in1=m2.to_broadcast([128, 4]), op=ALU.is_equal), r=[rt], w=[rt])
        dve(lambda: V.tensor_sub(out=dlt, in0=m2, in1=m1), r=[rt], w=[rt])
        act(lambda: A.activation(out=dlt, in_=dlt, func=AF.Exp), r=[rt], w=[rt])
        dve(lambda: V.tensor_scalar(out=dlt, in0=dlt, scalar1=1.0, scalar2=None, op0=ALU.add), r=[rt], w=[rt])
        dve(lambda: V.reciprocal(out=dlt, in_=dlt), r=[rt], w=[rt])
        dve(lambda: V.tensor_mul(out=wa, in0=dlt, in1=gval), r=[rt], w=[rt])
        dve(lambda: V.tensor_sub(out=wb_, in0=gval, in1=wa), r=[rt], w=[rt])
        dve(lambda: V.tensor_scalar(out=cw, in0=mk1, scalar1=wa, scalar2=None, op0=ALU.mult), r=[rt], w=[rt])
        dve(lambda: V.scalar_tensor_tensor(out=cw, in0=mk2, scalar=wb_, in1=cw, op0=ALU.mult, op1=ALU.add), r=[rt], w=[rt])
        dve(lambda: V.tensor_tensor(out=wts[:, J, :].rearrange("p (g e) -> p g e", g=4), in0=gm.unsqueeze(2).to_broadcast([128, 4, 4]),
                                    in1=cw.unsqueeze(1).to_broadcast([128, 4, 4]), op=ALU.mult), r=[rt], w=[wts])
    w1_rot = Rot([sb(pf, "w1e%d" % i, [128, KC, 512], BF16) for i in range(2)])
    w3_rot = Rot([sb(pf, "w3e%d" % i, [128, KC, 512], BF16) for i in range(2)])
    w2_rot = Rot([sb(pf, "w2e%d" % i, [128, 4, D], BF16) for i in range(2)])
    actT = sb(pf, "actT", [128, 4, NJ * 128], BF16)
    s1_rot = Rot([sb(pf, "s1_%d" % i, [128, 512], F32) for i in range(2)])
    for e in range(16):
        w1e, w3e, w2e = w1_rot.next(), w3_rot.next(), w2_rot.next()
        load_w_cols(w1e, 0, dram["moe_w1"][e], 0, 512)
        load_w_cols(w3e, 0, dram["moe_w3"][e], 0, 512)
        load_w_cols(w2e, 0, dram["moe_w2"][e], 0, D, kc=4)
        for hc in range(4):
            for tc in range(4):
                tsl = slice(tc * 512, (tc + 1) * 512)
                p1, p3 = pb_rot.next(), pb_rot.next()
                for k in range(KC):
                    pe(lambda: T.matmul(p1[:, :], lhsT=w1e[:, k, hc * 128:(hc + 1) * 128], rhs=hmT[:, k, tsl], start=(k == 0), stop=(k == KC - 1)), r=[w1e, (hmT, tc)], w=[p1])
                for k in range(KC):
                    pe(lambda: T.matmul(p3[:, :], lhsT=w3e[:, k, hc * 128:(hc + 1) * 128], rhs=hmT[:, k, tsl], start=(k == 0), stop=(k == KC - 1)), r=[w3e, (hmT, tc)], w=[p3])
                s1 = s1_rot.next()
                act(lambda: A.activation(out=s1[:], in_=p1[:, :], func=AF.Silu), r=[p1], w=[s1])
                dve(lambda: V.tensor_mul(out=actT[:, hc, tsl], in0=s1[:], in1=p3[:, :]), r=[s1, p3], w=[(actT, tc)])
                if e == 0:
                    route_tile(hc * 4 + tc)
        for J in range(NJ):
            for hf in range(2):
                py = pb_rot.next()
                for hc in range(4):
                    pe(lambda: T.matmul(py[:, :], lhsT=actT[:, hc, J * 128:(J + 1) * 128], rhs=w2e[:, hc, hf * 512:(hf + 1) * 512], start=(hc == 0), stop=(hc == 3)),
                       r=[(actT, J // 4), w2e], w=[py])
                dve(lambda: V.scalar_tensor_tensor(out=xr[:, J, hf * 512:(hf + 1) * 512], in0=py[:, :], scalar=wts[:, J, e:e + 1], in1=xr[:, J, hf * 512:(hf + 1) * 512],
                                                   op0=ALU.mult, op1=ALU.add), r=[py, wts, (xr, J)], w=[(xr, J)])
    pf.close()
    if "x3" in dbg:
        for J in range(NJ):
            dump("x3", xr[:, J, :], [(xr, J)], dbg["x3"][J * 128:(J + 1) * 128, :])

    load_gain("norm_final")
    yo_rot = Rot([sb(px, "yo%d" % i, [128, D], F32) for i in range(2)])
    for J in range(NJ):
        st = st_rot.next()
        act(lambda: A.activation(out=sq_junk[:], in_=xr[:, J, :], func=AF.Square, accum_out=st[:, 0:1]), r=[(xr, J)], w=[sq_junk, st])
        act(lambda: A.activation(out=st[:, 1:2], in_=st[:, 0:1], func=AF.Ln, scale=1.0 / D, bias=cst[:, 0:1]), r=[st, cst], w=[st])
        act(lambda: A.activation(out=st[:, 2:3], in_=st[:, 1:2], func=AF.Exp, scale=-0.5), r=[st], w=[st])
        yo = yo_rot.next()
        dve(lambda: V.scalar_tensor_tensor(out=yo[:], in0=xr[:, J, :], scalar=st[:, 2:3], in1=gb[:], op0=ALU.mult, op1=ALU.mult), r=[(xr, J), st, gb], w=[yo])
        fw.dma("pool", y_out[J * 128:(J + 1) * 128, :], yo[:], reads=[yo])
    final_wait()
    pes.close(); fw.close()
    return nc


_CACHE = {}


def make_in_maps(inputs):
    f = lambda a: np.ascontiguousarray(np.asarray(a, dtype=np.float32))
    x = f(inputs["x"]); mem = f(inputs["mem"])
    shared = {}
    for nm in ("norm_mix", "norm_xattn", "norm_mem", "norm_ffn"):
        shared[nm] = f(inputs[nm][0])
    shared["norm_final"] = f(inputs["norm_final"])
    shared["w_in"] = f(inputs["w_in"][0])
    shared["conv_qk"] = np.ascontiguousarray(f(inputs["conv_qk"][0]).reshape(4, 8, 128).transpose(2, 1, 0))
    shared["bi_rep"] = np.ascontiguousarray(np.tile(f(inputs["b_igate"][0]), 32))
    shared["bf_rep"] = np.ascontiguousarray(np.tile(f(inputs["b_fgate"][0]), 32))
    shared["mlstm_norm"] = f(inputs["mlstm_norm"][0])
    shared["cmp_pos_kT"] = np.ascontiguousarray(f(inputs["cmp_pos_k"][0]).T)
    shared["cmp_pos_vT"] = np.ascontiguousarray(f(inputs["cmp_pos_v"][0]).T)
    for nm in ("cmp_k_w1", "cmp_k_w2", "cmp_v_w1", "cmp_v_w2", "w_br_mlstm", "w_br_nsa", "w_mix_out",
               "xa_wq", "xa_wkv", "xa_wo", "moe_w1", "moe_w3", "moe_w2"):
        shared[nm] = f(inputs[nm][0])
    shared["router_w"] = np.ascontiguousarray(np.concatenate([f(inputs["router_group_w"][0]), f(inputs["router_expert_w"][0])], axis=1))
    shared["router_b_rep"] = np.ascontiguousarray(np.concatenate([f(inputs["router_group_b"][0]), f(inputs["router_expert_b"][0])]))
    hc = [host_consts(0), host_consts(1)]
    maps = []
    for core in range(8):
        b, p = core // 2, core % 2
        m = dict(shared)
        m["x_all"] = x[b]
        m["x_own"] = np.ascontiguousarray(x[b].reshape(NJ, 2, 128, D)[:, p].reshape(NJ * 128, D))
        m["mem"] = mem[b]
        m.update(hc[p])
        maps.append(m)
    return maps


def kernel(**inputs):
    if "nc" not in _CACHE:
        _CACHE["nc"] = build_nc()
    nc = _CACHE["nc"]
    maps = make_in_maps(inputs)
    res = run_bass_kernel_spmd(nc, maps, core_ids=list(range(8)))
    out = np.zeros((4, S, D), np.float32)
    for core in range(8):
        b, p = core // 2, core % 2
        y = np.asarray(res.results[core]["y"]).reshape(NJ, 128, D)
        out[b].reshape(NJ, 2, 128, D)[:, p] = y
    return out
```
